# Optimizing a Trainium2 kernel written in Bass

```python
import math
import jax, jax.numpy as jnp
from jax import lax
import numpy as np

D_MODEL = 1024
BATCH = 8
SEQ = 2048
DEPTH = 2

Q_BLOCK = 128
NORM_EPS = 1e-6
SB_HEADS = 8
SB_HEAD_DIM = 64
MLA_HEADS = 8
MLA_Q_RANK = 768
MLA_KV_RANK = 256
MLA_NOPE_DIM = 64
MLA_ROPE_DIM = 32
MLA_V_DIM = 64
MLA_QK_DIM = MLA_NOPE_DIM + MLA_ROPE_DIM
ROPE_THETA = 10000.0
DIFF_HEADS = 4
DIFF_HEAD_DIM = 64
DIFF_V_DIM = 2 * DIFF_HEAD_DIM
N_BRANCH = 3
BRANCH_WIDTH = 512
D_FF = 2816
N_EXPERTS = 8
TOP_K = 2
EXPERT_D_FF = 2816
N_ADA = 6
SB_COLS = 3 * SB_HEADS * SB_HEAD_DIM
MLA_COLS = MLA_Q_RANK + MLA_KV_RANK + MLA_ROPE_DIM
DIFF_QK_COLS = DIFF_HEADS * 2 * DIFF_HEAD_DIM
DIFF_COLS = 2 * DIFF_QK_COLS + DIFF_HEADS * DIFF_V_DIM
GATE_COLS = N_BRANCH * D_MODEL
IN_COLS = SB_COLS + MLA_COLS + DIFF_COLS + GATE_COLS

kernel_name = 'hybrid_sb_mla_diff_moe_block'


def _rmsnorm(x, g):
    xf = x.astype(jnp.float32)
    y = xf * lax.rsqrt(jnp.mean(xf * xf, axis=-1, keepdims=True) + NORM_EPS)
    return y.astype(x.dtype) * g


def _to_blocks(a):
    b, s = a.shape[:2]
    return jnp.moveaxis(a.reshape(b, s // Q_BLOCK, Q_BLOCK, *a.shape[2:]), 1, 0)


def _from_blocks(a):
    nb, b, qb = a.shape[:3]
    return jnp.moveaxis(a, 0, 1).reshape(b, nb * qb, *a.shape[3:])


def _rope(x, pos):
    half = x.shape[-1] // 2
    inv_freq = ROPE_THETA ** (-jnp.arange(half, dtype=jnp.float32) / half)
    ang = pos.astype(jnp.float32)[..., None] * inv_freq
    ang = ang.reshape(ang.shape[:2] + (1,) * (x.ndim - 3) + (half,))
    cos, sin = jnp.cos(ang), jnp.sin(ang)
    x1 = x[..., :half].astype(jnp.float32)
    x2 = x[..., half:].astype(jnp.float32)
    return jnp.concatenate([x1 * cos - x2 * sin, x1 * sin + x2 * cos], axis=-1).astype(x.dtype)


def _stick_breaking(q, k, v):
    scale = 1.0 / math.sqrt(q.shape[-1])
    key_idx = jnp.arange(k.shape[1])

    def block(args):
        qb, i = args
        z = jnp.einsum('bqhd,bkhd->bhqk', qb, k).astype(jnp.float32) * scale
        q_idx = i * Q_BLOCK + jnp.arange(Q_BLOCK)
        strict = key_idx[None, :] < q_idx[:, None]
        log_1m_beta = jnp.where(strict, jax.nn.log_sigmoid(-z), 0.0)
        later = lax.cumsum(log_1m_beta, axis=3, reverse=True) - log_1m_beta
        w = jnp.where(strict, jnp.exp(jax.nn.log_sigmoid(z) + later), 0.0)
        return jnp.einsum('bhqk,bkhd->bqhd', w.astype(v.dtype), v)

    nb = q.shape[1] // Q_BLOCK
    return _from_blocks(lax.map(block, (_to_blocks(q), jnp.arange(nb))))


def _causal_softmax_attention(q, k, v):
    scale = 1.0 / math.sqrt(q.shape[-1])
    key_idx = jnp.arange(k.shape[1])

    def block(args):
        qb, i = args
        sc = jnp.einsum('bqhd,bkhd->bhqk', qb, k).astype(jnp.float32) * scale
        q_idx = i * Q_BLOCK + jnp.arange(Q_BLOCK)
        causal = key_idx[None, :] <= q_idx[:, None]
        p = jax.nn.softmax(jnp.where(causal, sc, -jnp.inf), axis=-1)
        return jnp.einsum('bhqk,bkhd->bqhd', p.astype(v.dtype), v)

    nb = q.shape[1] // Q_BLOCK
    return _from_blocks(lax.map(block, (_to_blocks(q), jnp.arange(nb))))


def _diff_attention(q, k, v, pos, slopes, lam):
    scale = 1.0 / math.sqrt(q.shape[-1])
    key_idx = jnp.arange(k.shape[1])

    def block(args):
        qb, pq, i = args
        sc = jnp.einsum('bqhmd,bkhmd->bhmqk', qb, k).astype(jnp.float32) * scale
        dist = jnp.abs(pq[:, :, None] - pos[:, None, :]).astype(jnp.float32)
        sc = sc - slopes[None, :, None, None, None] * dist[:, None, None]
        q_idx = i * Q_BLOCK + jnp.arange(Q_BLOCK)
        causal = key_idx[None, :] <= q_idx[:, None]
        p = jax.nn.softmax(jnp.where(causal, sc, -jnp.inf), axis=-1)
        a = p[:, :, 0] - lam * p[:, :, 1]
        return jnp.einsum('bhqk,bkhd->bqhd', a.astype(v.dtype), v)

    nb = q.shape[1] // Q_BLOCK
    return _from_blocks(lax.map(block, (_to_blocks(q), _to_blocks(pos), jnp.arange(nb))))


def _mixer(h, pos, layer, w_in, mla_q_norm, w_mla_uq, mla_kv_norm, w_mla_ukv, mla_q_gain,
           mla_k_gain, diff_q_gain, diff_k_gain, diff_lambda, diff_subln, w_branch, w_out):
    b, s, _ = h.shape
    proj = h @ w_in
    sb, mla, dif, gate_logits = jnp.split(
        proj, [SB_COLS, SB_COLS + MLA_COLS, SB_COLS + MLA_COLS + DIFF_COLS], axis=-1)

    sb = sb.reshape(b, s, 3, SB_HEADS, SB_HEAD_DIM)
    o_sb = _stick_breaking(sb[:, :, 0], sb[:, :, 1], sb[:, :, 2]).reshape(b, s, BRANCH_WIDTH)

    c_q, c_kv, k_rope = jnp.split(mla, [MLA_Q_RANK, MLA_Q_RANK + MLA_KV_RANK], axis=-1)
    q = (_rmsnorm(c_q, mla_q_norm) @ w_mla_uq).reshape(b, s, MLA_HEADS, MLA_QK_DIM)
    kv = (_rmsnorm(c_kv, mla_kv_norm) @ w_mla_ukv).reshape(b, s, MLA_HEADS, MLA_NOPE_DIM + MLA_V_DIM)
    k_rope = jnp.broadcast_to(_rope(k_rope, pos)[:, :, None, :], (b, s, MLA_HEADS, MLA_ROPE_DIM))
    q = jnp.concatenate([q[..., :MLA_NOPE_DIM], _rope(q[..., MLA_NOPE_DIM:], pos)], axis=-1)
    k = jnp.concatenate([kv[..., :MLA_NOPE_DIM], k_rope], axis=-1)
    o_mla = _causal_softmax_attention(_rmsnorm(q, mla_q_gain), _rmsnorm(k, mla_k_gain),
                                      kv[..., MLA_NOPE_DIM:]).reshape(b, s, BRANCH_WIDTH)

    dq, dk, dv = jnp.split(dif, [DIFF_QK_COLS, 2 * DIFF_QK_COLS], axis=-1)
    dq = _rmsnorm(dq.reshape(b, s, DIFF_HEADS, 2, DIFF_HEAD_DIM), diff_q_gain)
    dk = _rmsnorm(dk.reshape(b, s, DIFF_HEADS, 2, DIFF_HEAD_DIM), diff_k_gain)
    dv = dv.reshape(b, s, DIFF_HEADS, DIFF_V_DIM)
    lam_init = 0.8 - 0.6 * math.exp(-0.3 * layer)
    lp = diff_lambda.astype(jnp.float32)
    lam = jnp.exp(jnp.sum(lp[0] * lp[1])) - jnp.exp(jnp.sum(lp[2] * lp[3])) + lam_init
    slopes = 2.0 ** (-8.0 * jnp.arange(1, DIFF_HEADS + 1, dtype=jnp.float32) / DIFF_HEADS)
    o_diff = _diff_attention(dq, dk, dv, pos, slopes, lam)
    o_diff = (_rmsnorm(o_diff, diff_subln) * (1.0 - lam_init)).reshape(b, s, BRANCH_WIDTH)

    branches = jnp.stack([o_sb, o_mla, o_diff], axis=2)
    gates = jax.nn.sigmoid(gate_logits.reshape(b, s, N_BRANCH, D_MODEL))
    merged = jnp.sum(gates * jnp.einsum('bsnc,ncd->bsnd', branches, w_branch), axis=2)
    return merged @ w_out


def _swiglu(h, w_gate, w_up, w_down):
    return (jax.nn.silu(h @ w_gate) * (h @ w_up)) @ w_down


def _moe(h, w_router, w_gate, w_up, w_down):
    logits = (h @ w_router).astype(jnp.float32)
    top_val, top_idx = lax.top_k(logits, TOP_K)
    top_w = jax.nn.softmax(top_val, axis=-1)
    combine = jnp.sum(jax.nn.one_hot(top_idx, N_EXPERTS, dtype=jnp.float32) * top_w[..., None], axis=-2)
    y = jnp.zeros_like(h)
    for e in range(N_EXPERTS):
        y = y + combine[..., e:e + 1].astype(h.dtype) * _swiglu(h, w_gate[e], w_up[e], w_down[e])
    return y


def setup_inputs(seed: int = 0) -> dict:
    key = jax.random.key(seed)
    ks = iter(jax.random.split(key, 40))
    nd = (DEPTH + 1) // 2
    nm = DEPTH // 2

    def nrm(shape, fan_in):
        return jax.random.normal(next(ks), shape, jnp.float32) * fan_in ** -0.5

    def gain(shape):
        return 1.0 + 0.02 * jax.random.normal(next(ks), shape, jnp.float32)

    x = jax.random.normal(next(ks), (BATCH, SEQ, D_MODEL), jnp.float32)
    c = jax.random.normal(next(ks), (BATCH, D_MODEL), jnp.float32)
    offset = jax.random.randint(next(ks), (BATCH, 1), 0, 1024, dtype=jnp.int32)
    positions = offset + jnp.arange(SEQ, dtype=jnp.int32)[None, :]
    return {
        'x': x,
        'c': c,
        'positions': positions,
        'norm1_g': gain((DEPTH, D_MODEL)),
        'norm2_g': gain((DEPTH, D_MODEL)),
        'w_ada': nrm((DEPTH, D_MODEL, N_ADA * D_MODEL), D_MODEL),
        'b_ada': 0.02 * jax.random.normal(next(ks), (DEPTH, N_ADA * D_MODEL), jnp.float32),
        'w_in': nrm((DEPTH, D_MODEL, IN_COLS), D_MODEL),
        'mla_q_norm': gain((DEPTH, MLA_Q_RANK)),
        'w_mla_uq': nrm((DEPTH, MLA_Q_RANK, MLA_HEADS * MLA_QK_DIM), MLA_Q_RANK),
        'mla_kv_norm': gain((DEPTH, MLA_KV_RANK)),
        'w_mla_ukv': nrm((DEPTH, MLA_KV_RANK, MLA_HEADS * (MLA_NOPE_DIM + MLA_V_DIM)), MLA_KV_RANK),
        'mla_q_gain': gain((DEPTH, MLA_QK_DIM)),
        'mla_k_gain': gain((DEPTH, MLA_QK_DIM)),
        'diff_q_gain': gain((DEPTH, DIFF_HEAD_DIM)),
        'diff_k_gain': gain((DEPTH, DIFF_HEAD_DIM)),
        'diff_lambda': 0.1 * jax.random.normal(next(ks), (DEPTH, 4, DIFF_HEAD_DIM), jnp.float32),
        'diff_subln': gain((DEPTH, DIFF_V_DIM)),
        'w_branch': nrm((DEPTH, N_BRANCH, BRANCH_WIDTH, D_MODEL), BRANCH_WIDTH),
        'w_out': nrm((DEPTH, D_MODEL, D_MODEL), D_MODEL),
        'w_ffn_gate': nrm((nd, D_MODEL, D_FF), D_MODEL),
        'w_ffn_up': nrm((nd, D_MODEL, D_FF), D_MODEL),
        'w_ffn_down': nrm((nd, D_FF, D_MODEL), D_FF),
        'w_router': nrm((nm, D_MODEL, N_EXPERTS), D_MODEL),
        'w_exp_gate': nrm((nm, N_EXPERTS, D_MODEL, EXPERT_D_FF), D_MODEL),
        'w_exp_up': nrm((nm, N_EXPERTS, D_MODEL, EXPERT_D_FF), D_MODEL),
        'w_exp_down': nrm((nm, N_EXPERTS, EXPERT_D_FF, D_MODEL), EXPERT_D_FF),
    }


def reference(x, c, positions, norm1_g, norm2_g, w_ada, b_ada, w_in, mla_q_norm, w_mla_uq,
              mla_kv_norm, w_mla_ukv, mla_q_gain, mla_k_gain, diff_q_gain, diff_k_gain,
              diff_lambda, diff_subln, w_branch, w_out, w_ffn_gate, w_ffn_up, w_ffn_down,
              w_router, w_exp_gate, w_exp_up, w_exp_down):
    cond = jax.nn.silu(c)
    for layer in range(DEPTH):
        mod = (cond @ w_ada[layer] + b_ada[layer])[:, None, :]
        sh1, sc1, g1, sh2, sc2, g2 = jnp.split(mod, N_ADA, axis=-1)

        h = _rmsnorm(x, norm1_g[layer]) * (1.0 + sc1) + sh1
        x = x + g1 * _mixer(h, positions, layer, w_in[layer], mla_q_norm[layer], w_mla_uq[layer],
                            mla_kv_norm[layer], w_mla_ukv[layer], mla_q_gain[layer], mla_k_gain[layer],
                            diff_q_gain[layer], diff_k_gain[layer], diff_lambda[layer],
                            diff_subln[layer], w_branch[layer], w_out[layer])

        h = _rmsnorm(x, norm2_g[layer]) * (1.0 + sc2) + sh2
        j = layer // 2
        if layer % 2 == 0:
            f = _swiglu(h, w_ffn_gate[j], w_ffn_up[j], w_ffn_down[j])
        else:
            f = _moe(h, w_router[j], w_exp_gate[j], w_exp_up[j], w_exp_down[j])
        x = x + g2 * f
    return x
```

```python
import math
from contextlib import ExitStack
import numpy as np
import concourse.bass as bass
import concourse.mybir as mybir
from concourse.alu_op_type import AluOpType as ALU
from concourse.bass_utils import run_bass_kernel_spmd

F32 = mybir.dt.float32
BF16 = mybir.dt.bfloat16
I32 = mybir.dt.int32
AF = mybir.ActivationFunctionType
AX = mybir.AxisListType

ENGS = ['pe', 'act', 'dve', 'pool', 'sp']
SAME_ENG_SYNC = True


class Buf:
    __slots__ = ('name', 'w', 'r', 'sbuf', 'g')

    def __init__(self, name, sbuf=True):
        self.name = name
        self.w = None
        self.r = {}
        self.sbuf = sbuf
        self.g = None


class Grp:
    __slots__ = ('name', 'sem', 'count')

    def __init__(self, name):
        self.name = name
        self.sem = None
        self.count = 0


class Sched:
    def __init__(self):
        self.ops = {e: [] for e in ENGS}
        self.seen = {e: {} for e in ENGS}
        self.grps = []
        self.named = {}

    def grp(self, name):
        g = Grp(name)
        self.grps.append(g)
        return g

    def add(self, eng, fn, R=(), W=(), grp=None):
        idx = len(self.ops[eng])
        deps = []
        for b in R:
            if b.w is not None:
                deps.append(b.w)
        for b in W:
            if b.w is not None:
                deps.append(b.w)
            deps.extend(b.r.values())
        waits = {}
        seen = self.seen[eng]
        for d in deps:
            if d[0] == 'c':
                _, e2, i2 = d
                if e2 == eng and (eng == 'pe' or not SAME_ENG_SYNC):
                    continue
                key = ('c', e2)
                val = i2
            else:
                g = d[1]
                key = ('d', g)
                val = g.count
            if seen.get(key, -1) >= val:
                continue
            if waits.get(key, -1) < val:
                waits[key] = val
        for k, v in waits.items():
            seen[k] = v
            if k[0] == 'c':
                self.ops[k[1]][v]['sig'] = True
        if grp is not None:
            grp.count += 1
            tok = ('d', grp, grp.count)
            rkey = grp
        else:
            tok = ('c', eng, idx)
            rkey = eng
        self.ops[eng].append(dict(fn=fn, waits=waits, sig=False, grp=grp))
        for b in R:
            b.r[rkey] = tok
        for b in W:
            b.w = tok
            b.r = {}
        return tok

    def dma(self, q, out, in_, R=(), W=(), grp=None, **kw):
        if grp is None:
            owner = None
            for b in list(W) + list(R):
                if getattr(b, 'sbuf', False):
                    owner = b
                    break
            key = q
            if owner.g is None:
                owner.g = {}
            if key not in owner.g:
                gname = owner.name + '_' + q
                if gname not in self.named:
                    self.named[gname] = self.grp(gname)
                owner.g[key] = self.named[gname]
            grp = owner.g[key]
        return self.add(q, lambda e: e.dma_start(out=out, in_=in_, **kw), R=R, W=W, grp=grp)

    def last_compute(self, e):
        ops = self.ops[e]
        for i in range(len(ops) - 1, -1, -1):
            if ops[i]['fn'] is not None and ops[i]['grp'] is None:
                return i
        return None

    def barrier(self, engs=None):
        lasts = {e: self.last_compute(e) for e in ENGS}
        for e in (engs or ENGS):
            waits = {}
            seen = self.seen[e]
            for e2 in ENGS:
                if e2 == e or lasts[e2] is None:
                    continue
                if seen.get(('c', e2), -1) >= lasts[e2]:
                    continue
                waits[('c', e2)] = lasts[e2]
                self.ops[e2][lasts[e2]]['sig'] = True
            for g in self.grps:
                if g.count > 0 and seen.get(('d', g), -1) < g.count:
                    waits[('d', g)] = g.count
            for k, v in waits.items():
                seen[k] = v
            self.ops[e].append(dict(fn=None, waits=waits, sig=False, grp=None))

    def emit(self, nc, es):
        csem = {e: es.enter_context(nc.semaphore('c_' + e)) for e in ENGS}
        for g in self.grps:
            g.sem = es.enter_context(nc.semaphore('g_' + g.name))
        ordv = {}
        for e in ENGS:
            n = 0
            o = []
            for op in self.ops[e]:
                if op['sig'] and op['grp'] is None:
                    n += 1
                o.append(n)
            ordv[e] = o
        block = es.enter_context(nc.Block())
        ops = self.ops

        def run(e, eng):
            for op in ops[e]:
                for key, val in op['waits'].items():
                    if key[0] == 'c':
                        eng.wait_ge(csem[key[1]], ordv[key[1]][val])
                    else:
                        eng.wait_ge(key[1].sem, 16 * val)
                if op['fn'] is None:
                    continue
                inst = op['fn'](eng)
                if op['grp'] is not None:
                    inst.then_inc(op['grp'].sem, 16)
                elif op['sig']:
                    inst.then_inc(csem[e], 1)

        @block.tensor
        def _(eng):
            run('pe', eng)

        @block.scalar
        def _(eng):
            run('act', eng)

        @block.vector
        def _(eng):
            run('dve', eng)

        @block.gpsimd
        def _(eng):
            run('pool', eng)

        @block.sync
        def _(eng):
            run('sp', eng)


D = 1024
EPS = 1e-6
IN_COLS = 7200
C_SBQ, C_SBK, C_SBV = 0, 512, 1024
C_MLA = 1536
C_DQ, C_DK, C_DV = 2592, 3104, 3616
C_GATE = 4128
DFF = 2816
WNAMES = ['norm1_g', 'norm2_g', 'w_ada', 'b_ada', 'w_in', 'mla_q_norm', 'w_mla_uq', 'mla_kv_norm', 'w_mla_ukv',
          'mla_q_gain', 'mla_k_gain', 'diff_q_gain', 'diff_k_gain', 'diff_lambda', 'diff_subln', 'w_branch',
          'w_out', 'w_ffn_gate', 'w_ffn_up', 'w_ffn_down', 'w_router', 'w_exp_gate', 'w_exp_up', 'w_exp_down']
WSHAPES = {
    'norm1_g': [2, 1024], 'norm2_g': [2, 1024], 'w_ada': [2, 1024, 6144], 'b_ada': [2, 6144],
    'w_in': [2, 1024, 7200], 'mla_q_norm': [2, 768], 'w_mla_uq': [2, 768, 768], 'mla_kv_norm': [2, 256],
    'w_mla_ukv': [2, 256, 1024], 'mla_q_gain': [2, 96], 'mla_k_gain': [2, 96], 'diff_q_gain': [2, 64],
    'diff_k_gain': [2, 64], 'diff_lambda': [2, 4, 64], 'diff_subln': [2, 128], 'w_branch': [2, 3, 512, 1024],
    'w_out': [2, 1024, 1024], 'w_ffn_gate': [1, 1024, 2816], 'w_ffn_up': [1, 1024, 2816],
    'w_ffn_down': [1, 2816, 1024], 'w_router': [1, 1024, 8], 'w_exp_gate': [1, 8, 1024, 2816],
    'w_exp_up': [1, 8, 1024, 2816], 'w_exp_down': [1, 8, 2816, 1024],
}


class Ring:
    def __init__(self, items):
        self.items = items
        self.i = 0

    def next(self):
        it = self.items[self.i % len(self.items)]
        self.i += 1
        return it


def build(SEQ=2048, layers=(0, 1), stop=None, dbg=(), wshapes=None, mcut=99):
    NT = SEQ // 128
    CH = min(512, SEQ)
    NCH = SEQ // CH
    QPC = CH // 128
    nc = bass.Bass("TRN2", target_bir_lowering=False)
    S = Sched()
    es = ExitStack()
    dbg_out = {}

    x_d = nc.dram_tensor("x", [SEQ, D], F32, kind="ExternalInput").ap()
    c_d = nc.dram_tensor("c", [D], F32, kind="ExternalInput").ap()
    pos_d = nc.dram_tensor("positions", [SEQ], I32, kind="ExternalInput").ap()
    Wd = {n: nc.dram_tensor(n, (wshapes or WSHAPES)[n], F32, kind="ExternalInput").ap() for n in WNAMES}
    y_d = nc.dram_tensor("y", [SEQ, D], F32, kind="ExternalOutput").ap()

    def scratch(name, shape, dt=BF16):
        return nc.dram_tensor(name, shape, dt, kind="Internal").ap()
    sbqk_d = scratch("s_sbqk", [8, 128, SEQ]); sbqk_B = [Buf("s_sbqk%d" % i, False) for i in range(8)]
    sbv_d = scratch("s_sbv", [SEQ, 512]); sbv_B = Buf("s_sbv", False)
    mq_d = scratch("s_mq", [8, 96, SEQ]); mq_B = Buf("s_mq", False)
    mk_d = scratch("s_mk", [8, 96, SEQ]); mk_B = Buf("s_mk", False)
    mv_d = scratch("s_mv", [SEQ, 8 * 66]); mv_B = Buf("s_mv", False)
    dq_d = scratch("s_dq", [4, 128, SEQ]); dq_B = Buf("s_dq", False)
    dk_d = scratch("s_dk", [4, 128, SEQ]); dk_B = Buf("s_dk", False)
    dv_d = scratch("s_dv", [SEQ, 4 * 130]); dv_B = Buf("s_dv", False)
    gt_d = scratch("s_gt", [24, 128, SEQ]); gt_B = Buf("s_gt", False)

    uid = [0]

    def T(name, shape, dt, st=None):
        uid[0] += 1
        return (st or es).enter_context(nc.sbuf_tensor("%s_u%d" % (name, uid[0]), shape, dt))

    def act(out, in_, func, R, W, **kw):
        return S.add('act', lambda e: e.activation(out=out, in_=in_, func=func, **kw), R=R, W=W)

    def tt(eng, out, in0, in1, op, R, W):
        return S.add(eng, lambda e: e.tensor_tensor(out=out, in0=in0, in1=in1, op=op), R=R, W=W)

    def ts(eng, out, in0, s1, op0, R, W, s2=None, op1=None):
        if op1 is None:
            return S.add(eng, lambda e: e.tensor_scalar(out=out, in0=in0, scalar1=s1, scalar2=None, op0=op0), R=R, W=W)
        return S.add(eng, lambda e: e.tensor_scalar(out=out, in0=in0, scalar1=s1, scalar2=s2, op0=op0, op1=op1), R=R, W=W)

    def stt(out, in0, scalar, in1, op0, op1, R, W):
        return S.add('dve', lambda e: e.scalar_tensor_tensor(out=out, in0=in0, scalar=scalar, in1=in1, op0=op0, op1=op1), R=R, W=W)

    def mm(out, lhsT, rhs, start, stop, R, W):
        return S.add('pe', lambda e: e.matmul(out, lhsT=lhsT, rhs=rhs, start=start, stop=stop), R=R, W=W)

    def cp(eng, out, in_, R, W):
        if eng == 'act':
            return S.add(eng, lambda e: e.activation(out=out, in_=in_, func=AF.Copy), R=R, W=W)
        return S.add(eng, lambda e: e.tensor_copy(out=out, in_=in_), R=R, W=W)

    def memset(eng, ap, val, W):
        return S.add(eng, lambda e: e.memset(ap, val), W=W)

    def red(out, in_, R, W):
        return S.add('dve', lambda e: e.tensor_reduce(out=out, in_=in_, axis=AX.X, op=ALU.add), R=R, W=W)

    def dump(name, ap, shape, dt=F32):
        if name not in dbg:
            return
        S.barrier()
        d = nc.dram_tensor("dbg_" + name, shape, dt, kind="ExternalOutput").ap()
        dbg_out[name] = d
        S.add('sp', lambda e: e.dma_start(out=d, in_=ap), grp=g_dbg)
        S.barrier()

    g_dbg = S.grp("dbg_sp")
    xs = T("xs", [128, NT, D], F32); xB = [Buf("x%d" % t) for t in range(NT)]
    ident = T("ident", [128, 128], F32); identB = Buf("ident")
    tri_b = T("tri_b", [128, 128], BF16); triB = Buf("tri")
    ones_b = T("ones_b", [128, 128], BF16); onesB = Buf("ones")
    upper_b = T("upper_b", [128, 128], BF16); upperB = Buf("upper")
    pos_tok = T("pos_tok", [128, NT], F32); posB = Buf("pos")
    invf = T("invf", [128, 16], F32); invfB = Buf("invf")
    modT = T("modT", [128, 32], F32); modTB = Buf("modT")
    ABT = T("ABT", [128, 16], F32); ABTB = Buf("ABT")
    g1g2 = T("g1g2", [128, 2, D], F32); g12B = Buf("g1g2")
    comb = T("comb", [128, NT, 8], F32); combB = Buf("comb")
    identb = T("identb", [128, 128], BF16); identbB = Buf("identb")
    ones_f = T("ones_f", [128, 128], F32); onesfB = Buf("ones_f")
    SC = T("SC", [128, NT, 32], F32); SCB = Buf("SC")
    PS = [es.enter_context(nc.psum_tensor("ps%d" % i, [128, 512], F32)) for i in range(8)]
    PB = [Buf("ps%d" % i) for i in range(8)]
    PI = list(zip(PS, PB))

    def tr(out, in_, R, W):
        return S.add('pe', lambda e: e.transpose(out, in_, ident[:in_.shape[0], :in_.shape[0]]), R=list(R) + [identB], W=W)

    def rsqrt(tag, out, in_, scale, R, W, st):
        n = in_.shape[1]
        v = T("rsq_" + tag, [128, n], F32, st); vB = Buf("rsq_" + tag)
        S.add('dve', lambda e: e.tensor_scalar(out=v[:], in0=in_, scalar1=scale, scalar2=EPS, op0=ALU.mult, op1=ALU.add), R=R, W=[vB])
        act(v[:], v[:], AF.Ln, [vB], [vB])
        act(out, v[:], AF.Exp, [vB], W, scale=-0.5)

    g_in = S.grp("in_sp")
    for t in range(NT):
        S.dma('sp', xs[:, t, :], x_d[t * 128:(t + 1) * 128, :], W=[xB[t]], grp=g_in)
    memset('pool', ident[:], 1.0, [identB])
    S.add('pool', lambda e: e.affine_select(out=ident[:], in_=ident[:], pattern=[[-1, 128]], compare_op=ALU.is_equal, fill=0.0, base=0, channel_multiplier=1), R=[identB], W=[identB])
    memset('pool', tri_b[:], 1.0, [triB])
    S.add('pool', lambda e: e.affine_select(out=tri_b[:], in_=tri_b[:], pattern=[[1, 128]], compare_op=ALU.is_ge, fill=0.0, base=0, channel_multiplier=-1), R=[triB], W=[triB])
    memset('pool', ones_b[:], 1.0, [onesB])
    memset('pool', ones_f[:], 1.0, [onesfB])
    memset('pool', upper_b[:], 1.0, [upperB])
    S.add('pool', lambda e: e.affine_select(out=upper_b[:], in_=upper_b[:], pattern=[[-1, 128]], compare_op=ALU.is_gt, fill=0.0, base=0, channel_multiplier=1), R=[upperB], W=[upperB])
    with ExitStack() as st:
        pi = T("pos_i", [128, NT], I32, st); tB = Buf("pos_i")
        S.dma('sp', pi[:], pos_d.rearrange("(t p) -> p t", p=128), W=[tB], grp=g_in, allow_slow_non_contiguous=True)
        cp('dve', pos_tok[:], pi[:], [tB], [posB])
        ii = T("iota_i", [128, 16], I32, st); iB = Buf("iota")
        S.add('pool', lambda e: e.iota(ii[:], pattern=[[1, 16]], base=0, channel_multiplier=0), W=[iB])
        cp('dve', invf[:], ii[:], [iB], [invfB])
        act(invf[:], invf[:], AF.Exp, [invfB], [invfB], scale=-math.log(10000.0) / 16.0)
        cp('dve', identb[:], ident[:], [identB], [identbB])
        TWO_PI_ = 2.0 * math.pi
        angA = T("angA", [128, NT, 16], F32, st); angB = Buf("angA")
        sct = T("sc_tmp", [128, NT * 32], F32, st); sctB = Buf("sc_tmp")
        sci = T("sc_ki", [128, NT * 32], I32, st); sciB = Buf("sc_ki")
        SC2 = SC[:].rearrange("p t s -> p (t s)")
        tt('dve', angA[:], invf[:].unsqueeze(1).broadcast_to([128, NT, 16]), pos_tok[:].unsqueeze(2).broadcast_to([128, NT, 16]), ALU.mult, [invfB, posB], [angB])
        ts('dve', SC[:, :, 0:16], angA[:], math.pi, ALU.add, [angB], [SCB])
        ts('dve', SC[:, :, 16:32], angA[:], 1.5 * math.pi, ALU.add, [angB], [SCB])
        ts('dve', sct[:], SC2, 1.0 / TWO_PI_, ALU.mult, [SCB], [sctB])
        cp('dve', sci[:], sct[:], [sctB], [sciB])
        cp('dve', sct[:], sci[:], [sciB], [sctB])
        stt(SC2, sct[:], -TWO_PI_, SC2, ALU.mult, ALU.add, [sctB, SCB], [SCB])
        ts('dve', sct[:], SC2, 0.0, ALU.is_lt, [SCB], [sctB])
        stt(SC2, sct[:], TWO_PI_, SC2, ALU.mult, ALU.add, [sctB, SCB], [SCB])
        ts('dve', SC2, SC2, -math.pi, ALU.add, [SCB], [SCB])
        ts('dve', SC2, SC2, -math.pi, ALU.max, [SCB], [SCB], s2=math.pi, op1=ALU.min)
        act(SC2, SC2, AF.Sin, [SCB], [SCB])
        S.barrier()

    def layer_body(l):
        lam_init = 0.8 - 0.6 * math.exp(-0.3 * l)
        w_in = Wd['w_in'][l]

        with ExitStack() as st:
            wa_r = Ring([(T("wa%d" % i, [128, 8, 512], F32, st), Buf("wa%d" % i)) for i in range(2)])
            bb_r = Ring([(T("bb%d" % i, [128, 512], F32, st), Buf("bb%d" % i)) for i in range(2)])
            mb_r = Ring([(T("mb%d" % i, [128, 512], F32, st), Buf("mb%d" % i)) for i in range(2)])
            gT = T("gT", [128, 16], F32, st); gTB = Buf("gT")
            condB_t = T("condB", [128, 8, 128], F32, st); condBB = Buf("condB")
            cT = T("cT", [128, 8], F32, st); cB = Buf("cT")
            S.dma('sp', cT[:], c_d.rearrange("(j p) -> p j", p=128), W=[cB], allow_slow_non_contiguous=True)
            act(cT[:], cT[:], AF.Silu, [cB], [cB])
            cp('dve', condB_t[:], cT[:].unsqueeze(2).broadcast_to([128, 8, 128]), [cB], [condBB])
            S.dma('sp', gT[:, 0:8], Wd['norm1_g'][l].rearrange("(j p) -> p j", p=128), W=[gTB], allow_slow_non_contiguous=True)
            S.dma('sp', gT[:, 8:16], Wd['norm2_g'][l].rearrange("(j p) -> p j", p=128), W=[gTB], allow_slow_non_contiguous=True)
            pr = Ring(PI[0:2]); pr2 = Ring(PI[2:4])
            for n in range(12):
                wa, waB = wa_r.next(); bb, bbB = bb_r.next(); mb, mbB = mb_r.next()
                S.dma('sp', wa[:, 0:4, :], Wd['w_ada'][l][0:512, n * 512:(n + 1) * 512].rearrange("(j p) n -> p j n", p=128), W=[waB])
                S.dma('pool', wa[:, 4:8, :], Wd['w_ada'][l][512:1024, n * 512:(n + 1) * 512].rearrange("(j p) n -> p j n", p=128), W=[waB])
                S.dma('sp', bb[:], Wd['b_ada'][l][n * 512:(n + 1) * 512].partition_broadcast(128), W=[bbB])
                ps, psB = pr.next()
                for j in range(8):
                    mm(ps[:, :], condB_t[:, j, :], wa[:, j, :], j == 0, j == 7, [condBB, waB], [psB])
                if n in (4, 5, 10, 11):
                    gi = 0 if n < 6 else 1
                    hf = n % 2
                    tt('dve', g1g2[:, gi, hf * 512:(hf + 1) * 512], ps[:, :], bb[:], ALU.add, [psB, bbB], [g12B])
                else:
                    tt('dve', mb[:], ps[:, :], bb[:], ALU.add, [psB, bbB], [mbB])
                    base = {0: 0, 1: 4, 2: 8, 3: 12, 6: 16, 7: 20, 8: 24, 9: 28}[n]
                    p2, p2B = pr2.next()
                    for q in range(4):
                        tr(p2[:, q * 128:(q + 1) * 128], mb[:, q * 128:(q + 1) * 128], [mbB], [p2B])
                    cp('act' if n % 2 else 'dve', modT[:, base:base + 4], p2[:, :].rearrange("p (q t) -> p q t", t=128)[:, :, 0], [p2B], [modTB])
            stt(ABT[:, 0:8], modT[:, 8:16], 1.0, gT[:, 0:8], ALU.add, ALU.mult, [modTB, gTB], [ABTB])
            stt(ABT[:, 8:16], modT[:, 24:32], 1.0, gT[:, 8:16], ALU.add, ALU.mult, [modTB, gTB], [ABTB])
            S.barrier()
        dump("modT%d" % l, modT[:], [128, 32])
        dump("ABT%d" % l, ABT[:], [128, 16])
        dump("g1g2_%d" % l, g1g2[:].rearrange("p a d -> p (a d)"), [128, 2 * D])
        if stop == 'A0':
            return False

        def norm_phase(which, hT, hB, st, router=None):
            aoff = 0 if which == 1 else 8
            boff = 0 if which == 1 else 16
            junk_r = Ring([(T("nj%d" % i, [128, D], BF16, st), Buf("nj%d" % i)) for i in range(2)])
            xn_r = Ring([(T("xn%d" % i, [128, D], F32, st), Buf("xn%d" % i)) for i in range(2)])
            ss_r = Ring([(T("nss%d" % i, [128, 2], F32, st), Buf("nss%d" % i)) for i in range(3)])
            pr = Ring([(PI[0], PI[1]), (PI[2], PI[3])])
            def n0(t):
                junk, jB = junk_r.next(); xn, xnB = xn_r.next(); ss, ssB = ss_r.next()
                act(junk[:], xs[:, t, :], AF.Square, [xB[t]], [jB, ssB], accum_out=ss[:, 0:1])
                ts('dve', ss[:, 1:2], ss[:, 0:1], 1.0 / D, ALU.mult, [ssB], [ssB], s2=EPS, op1=ALU.add)
                act(ss[:, 1:2], ss[:, 1:2], AF.Ln, [ssB], [ssB])
                act(ss[:, 1:2], ss[:, 1:2], AF.Exp, [ssB], [ssB], scale=-0.5)
                ts('dve', xn[:], xs[:, t, :], ss[:, 1:2], ALU.mult, [xB[t], ssB], [xnB])
                return xn, xnB

            def n1(t, xn, xnB):
                (pa, paB), (pb, pbB) = pr.next()
                for j in range(8):
                    p, pB_ = (pa, paB) if j < 4 else (pb, pbB)
                    tr(p[:, (j % 4) * 128:(j % 4 + 1) * 128], xn[:, j * 128:(j + 1) * 128], [xnB], [pB_])
                for j in range(8):
                    p, pB_ = (pa, paB) if j < 4 else (pb, pbB)
                    src = p[:, (j % 4) * 128:(j % 4 + 1) * 128]
                    dst = hT[:, j, t * 128:(t + 1) * 128]
                    if j % 2 == 0:
                        act(dst, src, AF.Identity, [pB_, ABTB, modTB], [hB[t]], scale=ABT[:, aoff + j:aoff + j + 1], bias=modT[:, boff + j:boff + j + 1])
                    else:
                        ts('dve', dst, src, ABT[:, aoff + j:aoff + j + 1], ALU.mult, [pB_, ABTB, modTB], [hB[t]], s2=modT[:, boff + j:boff + j + 1], op1=ALU.add)
                if router is not None:
                    router(t, pa, paB, pb, pbB, aoff, boff)

            pend = {}
            for k in range(NT + 1):
                if k < NT:
                    pend[k] = n0(k)
                if k >= 1:
                    n1(k - 1, *pend.pop(k - 1))

        def load_w(tile, buf, src2d, r0, nrow_chunks, c0, ncols):
            S.dma('pool', tile[:, 0:nrow_chunks, 0:ncols],
                  src2d[r0:r0 + 128 * nrow_chunks, c0:c0 + ncols].rearrange("(j p) n -> p j n", p=128), W=[buf])

        def proj_tok(ps, psB, hT, hB, W, WB, c0, ncols, t):
            for j in range(8):
                mm(ps[:, 0:ncols], hT[:, j, t * 128:(t + 1) * 128], W[:, j, c0:c0 + ncols], j == 0, j == 7, [hB[t], WB], [psB])

        def proj_T(ps, psB, hT, hB, W, WB, c0, ncols, c):
            R = [hB[t] for t in range(c * QPC, (c + 1) * QPC)] + [WB]
            for j in range(8):
                mm(ps[0:ncols, 0:CH], W[:, j, c0:c0 + ncols], hT[:, j, c * CH:(c + 1) * CH], j == 0, j == 7, R, [psB])

        stA = ExitStack()
        hT = T("hT", [128, 8, SEQ], BF16, stA); hB = [Buf("h%d" % t) for t in range(NT)]
        with ExitStack() as st:
            norm_phase(1, hT, hB, st)
            S.barrier()
        dump("hT%d" % l, hT[:].rearrange("p j t -> p (j t)"), [128, 8 * SEQ], BF16)
        if stop == 'A1':
            stA.close()
            return False

        with ExitStack() as st:
            w_r = Ring([(T("wsb%d" % i, [128, 8, 512], BF16, st), Buf("wsb%d" % i)) for i in range(2)])
            stg_r = Ring([(T("sbst%d" % i, [128, SEQ], BF16, st), Buf("sbst%d" % i)) for i in range(3)])
            stv_r = Ring([(T("sbsv%d" % i, [128, 512], BF16, st), Buf("sbsv%d" % i)) for i in range(3)])
            pr = Ring(PI)
            for part in range(3):
                W, WB = w_r.next()
                load_w(W, WB, w_in, 0, 8, part * 512, 512)
                if part < 2:
                    for m in range(4):
                        stg, stgB = stg_r.next()
                        for c in range(NCH):
                            ps, psB = pr.next()
                            proj_T(ps, psB, hT, hB, W, WB, m * 128, 128, c)
                            cp('act' if c % 2 else 'dve', stg[:, c * CH:(c + 1) * CH], ps[:, 0:CH], [psB], [stgB])
                        S.dma('sp', sbqk_d[part * 4 + m], stg[:], R=[stgB], W=[sbqk_B[part * 4 + m]])
                else:
                    for t in range(NT):
                        stv, stvB = stv_r.next()
                        ps, psB = pr.next()
                        proj_tok(ps, psB, hT, hB, W, WB, 0, 512, t)
                        cp('act' if t % 2 else 'dve', stv[:], ps[:, :], [psB], [stvB])
                        S.dma('sp', sbv_d[t * 128:(t + 1) * 128, :], stv[:], R=[stvB], W=[sbv_B])
            S.barrier()
        if stop == 'A2SB':
            stA.close()
            return False
        with ExitStack() as st:
            Wm = T("wmla", [128, 8, 1056], BF16, st); WmB = Buf("wmla")
            for i in range(3):
                c0 = i * 512
                ncol = min(512, 1056 - c0)
                S.dma('pool', Wm[:, :, c0:c0 + ncol], w_in[:, C_MLA + c0:C_MLA + c0 + ncol].rearrange("(j p) n -> p j n", p=128), W=[WmB])
            Wuq = T("wuq", [128, 6, 768], BF16, st); WuqB = Buf("wuq")
            load_w(Wuq, WuqB, Wd['w_mla_uq'][l], 0, 6, 0, 768)
            Wukv = T("wukv", [128, 2, 1024], BF16, st); WukvB = Buf("wukv")
            load_w(Wukv, WukvB, Wd['w_mla_ukv'][l], 0, 2, 0, 1024)
            nrm = T("mnrm", [128, 8], F32, st); nrmB = Buf("mnrm")
            S.dma('sp', nrm[:, 0:6], Wd['mla_q_norm'][l].rearrange("(j p) -> p j", p=128), W=[nrmB], allow_slow_non_contiguous=True)
            S.dma('sp', nrm[:, 6:8], Wd['mla_kv_norm'][l].rearrange("(j p) -> p j", p=128), W=[nrmB], allow_slow_non_contiguous=True)
            for j in range(6):
                ts('dve', Wuq[:, j, :], Wuq[:, j, :], nrm[:, j:j + 1], ALU.mult, [WuqB, nrmB], [WuqB])
            for j in range(2):
                ts('dve', Wukv[:, j, :], Wukv[:, j, :], nrm[:, 6 + j:7 + j], ALU.mult, [WukvB, nrmB], [WukvB])
            gq = T("mgq", [128, 3], F32, st); gqB = Buf("mgq")
            S.dma('sp', gq[0:96, 0:1], Wd['mla_q_gain'][l].rearrange("(p o) -> p o", o=1), W=[gqB])
            S.dma('sp', gq[0:96, 1:2], Wd['mla_k_gain'][l].rearrange("(p o) -> p o", o=1), W=[gqB])
            stt(gq[0:96, 2:3], gq[0:96, 0:1], 1.0 / math.sqrt(96.0), gq[0:96, 1:2], ALU.mult, ALU.mult, [gqB], [gqB])
            pr = Ring(PI)
            NR = 2
            junk_r = Ring([(T("mj%d" % i, [128, 512], BF16, st), Buf("mj%d" % i)) for i in range(NR)])
            ss_r = Ring([(T("mss%d" % i, [128, 8], F32, st), Buf("mss%d" % i)) for i in range(NR)])
            cf_r = Ring([(T("mcf%d" % i, [128, 1024], F32, st), Buf("mcf%d" % i)) for i in range(NR)])
            cT_r = Ring([(T("mcT%d" % i, [128, 8, 128], BF16, st), Buf("mcT%d" % i)) for i in range(NR)])
            qf_r = Ring([(T("mqf%d" % i, [128, 768], F32, st), Buf("mqf%d" % i)) for i in range(NR)])
            kf_r = Ring([(T("mkf%d" % i, [128, 768], F32, st), Buf("mkf%d" % i)) for i in range(NR)])
            vb_r = Ring([(T("mvb%d" % i, [128, 8, 66], BF16, st), Buf("mvb%d" % i)) for i in range(NR)])
            rp_r = Ring([(T("mrp%d" % i, [128, 8, 64], F32, st), Buf("mrp%d" % i)) for i in range(NR)])
            sm_r = Ring([(T("msm%d" % i, [128, 192], F32, st), Buf("msm%d" % i)) for i in range(NR)])
            sq_r = Ring([(T("msq%d" % i, [128, 768], F32, st), Buf("msq%d" % i)) for i in range(NR)])
            rq_r = Ring([(T("mrq%d" % i, [128, 32], F32, st), Buf("mrq%d" % i)) for i in range(NR)])
            stq_r = Ring([(T("mstq%d" % i, [128, 8, 128], BF16, st), Buf("mstq%d" % i)) for i in range(NR)])
            stk_r = Ring([(T("mstk%d" % i, [128, 8, 128], BF16, st), Buf("mstk%d" % i)) for i in range(NR)])
            for it in vb_r.items:
                memset('pool', it[0][:, :, 64:66], 1.0, [it[1]])
            prA = Ring(PI[0:3]); prB = Ring(PI[3:8])
            ST = {}

            def sA_pe(t):
                d = ST.setdefault(t, {})
                pa, paB = prA.next(); pb, pbB = prA.next(); pc, pcB = prA.next()
                d['pa'] = (pa, paB); d['pb'] = (pb, pbB); d['pc'] = (pc, pcB)
                proj_tok(pa, paB, hT, hB, Wm, WmB, 0, 512, t)
                proj_tok(pb, pbB, hT, hB, Wm, WmB, 512, 512, t)
                proj_tok(pc, pcB, hT, hB, Wm, WmB, 1024, 32, t)
                junk, jB = junk_r.next(); ss, ssB = ss_r.next()
                d['ss'] = (ss, ssB)
                act(junk[:, 0:512], pa[:, :], AF.Square, [paB], [jB, ssB], accum_out=ss[:, 0:1])
                act(junk[:, 0:256], pb[:, 0:256], AF.Square, [pbB], [jB, ssB], accum_out=ss[:, 1:2])
                act(junk[:, 0:256], pb[:, 256:512], AF.Square, [pbB], [jB, ssB], accum_out=ss[:, 2:3])

            def sA_dve(t):
                d = ST[t]
                pa, paB = d['pa']; pb, pbB = d['pb']; pc, pcB = d['pc']; ss, ssB = d['ss']
                cf, cfB = cf_r.next(); sm, smB = sm_r.next()
                d['cf'] = (cf, cfB); d['sm'] = (sm, smB)
                cp('dve', cf[:, 0:512], pa[:, :], [paB], [cfB])
                cp('dve', cf[:, 512:1024], pb[:, :], [pbB], [cfB])
                tt('dve', ss[:, 0:1], ss[:, 0:1], ss[:, 1:2], ALU.add, [ssB], [ssB])
                ts('dve', ss[:, 4:5], ss[:, 0:1], 1.0 / 768, ALU.mult, [ssB], [ssB], s2=EPS, op1=ALU.add)
                ts('dve', ss[:, 5:6], ss[:, 2:3], 1.0 / 256, ALU.mult, [ssB], [ssB], s2=EPS, op1=ALU.add)
                act(ss[:, 4:6], ss[:, 4:6], AF.Ln, [ssB], [ssB])
                act(ss[:, 4:6], ss[:, 4:6], AF.Exp, [ssB], [ssB], scale=-0.5)
                sinv = SC[:, t, 0:16]; cosv = SC[:, t, 16:32]
                k1 = pc[:, 0:16]; k2 = pc[:, 16:32]
                tt('dve', sm[:, 48:64], k1, cosv, ALU.mult, [pcB, SCB], [smB])
                tt('dve', sm[:, 64:80], k2, sinv, ALU.mult, [pcB, SCB], [smB])
                tt('dve', sm[:, 80:96], k1, sinv, ALU.mult, [pcB, SCB], [smB])
                tt('dve', sm[:, 96:112], k2, cosv, ALU.mult, [pcB, SCB], [smB])
                tt('dve', sm[:, 112:128], sm[:, 48:64], sm[:, 64:80], ALU.subtract, [smB], [smB])
                tt('dve', sm[:, 128:144], sm[:, 80:96], sm[:, 96:112], ALU.add, [smB], [smB])

            def sB(t):
                d = ST[t]
                cf, cfB = d['cf']; ss, ssB = d['ss']
                cT, cTB = cT_r.next()
                pt1, pt1B = prB.next(); pt2, pt2B = prB.next()
                for j in range(8):
                    p, pB_ = (pt1, pt1B) if j < 4 else (pt2, pt2B)
                    tr(p[:, (j % 4) * 128:(j % 4 + 1) * 128], cf[:, j * 128:(j + 1) * 128], [cfB], [pB_])
                cp('act', cT[:, 0:4, :].rearrange("p j t -> p (j t)"), pt1[:, :], [pt1B], [cTB])
                cp('dve', cT[:, 4:8, :].rearrange("p j t -> p (j t)"), pt2[:, :], [pt2B], [cTB])
                pq = [prB.next(), prB.next()]; pkv = [prB.next(), prB.next()]
                for half in range(2):
                    for j in range(6):
                        mm(pq[half][0][:, 0:384], cT[:, j, :], Wuq[:, j, half * 384:(half + 1) * 384], j == 0, j == 5, [cTB, WuqB], [pq[half][1]])
                for half in range(2):
                    for j in range(2):
                        mm(pkv[half][0][:, 0:512], cT[:, 6 + j, :], Wukv[:, j, half * 512:(half + 1) * 512], j == 0, j == 1, [cTB, WukvB], [pkv[half][1]])
                qf, qfB = qf_r.next(); kf, kfB = kf_r.next(); vb, vbB = vb_r.next()
                d['qf'] = (qf, qfB); d['kf'] = (kf, kfB); d['vb'] = (vb, vbB)
                k3 = kf[:].rearrange("p (h d) -> p h d", d=96)
                for half in range(2):
                    ts('dve', qf[:, half * 384:(half + 1) * 384], pq[half][0][:, 0:384], ss[:, 4:5], ALU.mult, [pq[half][1], ssB], [qfB])
                    src3 = pkv[half][0][:, :].rearrange("p (h e) -> p h e", e=128)
                    ts('dve', k3[:, half * 4:(half + 1) * 4, 0:64], src3[:, :, 0:64], ss[:, 5:6], ALU.mult, [pkv[half][1], ssB], [kfB])
                    ts('dve', vb[:, half * 4:(half + 1) * 4, 0:64], src3[:, :, 64:128], ss[:, 5:6], ALU.mult, [pkv[half][1], ssB], [vbB])

            def sC_dve(t):
                d = ST[t]
                qf, qfB = d['qf']; kf, kfB = d['kf']; sm, smB = d['sm']
                q3 = qf[:].rearrange("p (h d) -> p h d", d=96); k3 = kf[:].rearrange("p (h d) -> p h d", d=96)
                rp, rpB = rp_r.next()
                sinv = SC[:, t, 0:16]; cosv = SC[:, t, 16:32]
                sinB3 = sinv.unsqueeze(1).broadcast_to([128, 8, 16]); cosB3 = cosv.unsqueeze(1).broadcast_to([128, 8, 16])
                q1 = q3[:, :, 64:80]; q2 = q3[:, :, 80:96]
                tt('dve', rp[:, :, 0:16], q1, cosB3, ALU.mult, [qfB, SCB], [rpB])
                tt('dve', rp[:, :, 16:32], q2, sinB3, ALU.mult, [qfB, SCB], [rpB])
                tt('dve', rp[:, :, 32:48], q1, sinB3, ALU.mult, [qfB, SCB], [rpB])
                tt('dve', rp[:, :, 48:64], q2, cosB3, ALU.mult, [qfB, SCB], [rpB])
                tt('dve', q1, rp[:, :, 0:16], rp[:, :, 16:32], ALU.subtract, [rpB], [qfB])
                tt('dve', q2, rp[:, :, 32:48], rp[:, :, 48:64], ALU.add, [rpB], [qfB])
                cp('dve', k3[:, :, 64:96], sm[:, 112:144].unsqueeze(1).broadcast_to([128, 8, 32]), [smB], [kfB])
                sq, sqB = sq_r.next(); rq, rqB = rq_r.next()
                tt('dve', sq[:], qf[:], qf[:], ALU.mult, [qfB], [sqB])
                red(rq[:, 0:8], sq[:].rearrange("p (h d) -> p h d", d=96), [sqB], [rqB])
                tt('pool', sq[:], kf[:], kf[:], ALU.mult, [kfB, rqB], [sqB])
                red(rq[:, 8:16], sq[:].rearrange("p (h d) -> p h d", d=96), [sqB], [rqB])
                ts('dve', rq[:, 16:32], rq[:, 0:16], 1.0 / 96, ALU.mult, [rqB], [rqB], s2=EPS, op1=ALU.add)
                act(rq[:, 16:32], rq[:, 16:32], AF.Ln, [rqB], [rqB])
                act(rq[:, 16:32], rq[:, 16:32], AF.Exp, [rqB], [rqB], scale=-0.5)
                tt('dve', q3, q3, rq[:, 16:24].unsqueeze(2).broadcast_to([128, 8, 96]), ALU.mult, [qfB, rqB], [qfB])
                tt('dve', k3, k3, rq[:, 24:32].unsqueeze(2).broadcast_to([128, 8, 96]), ALU.mult, [kfB, rqB], [kfB])

            def sC_rest(t):
                d = ST.pop(t)
                qf, qfB = d['qf']; kf, kfB = d['kf']; vb, vbB = d['vb']
                q3 = qf[:].rearrange("p (h d) -> p h d", d=96); k3 = kf[:].rearrange("p (h d) -> p h d", d=96)
                stq, stqB = stq_r.next(); stk, stkB = stk_r.next()
                for (src3_, srcB, stg, stgB, isq) in ((q3, qfB, stq, stqB, True), (k3, kfB, stk, stkB, False)):
                    for half in range(2):
                        p, pB_ = prB.next()
                        for hh in range(4):
                            tr(p[0:96, hh * 128:(hh + 1) * 128], src3_[:, half * 4 + hh, :], [srcB], [pB_])
                        src = p[0:96, :]
                        dst = stg[0:96, half * 4:(half + 1) * 4, :].rearrange("p h t -> p (h t)")
                        if isq:
                            act(dst, src, AF.Identity, [pB_, gqB], [stgB], scale=gq[0:96, 2:3])
                        else:
                            cp('dve', dst, src, [pB_], [stgB])
                S.dma('sp', mq_d[:, :, t * 128:(t + 1) * 128].rearrange("h p t -> p h t"), stq[0:96, :, :], R=[stqB], W=[mq_B])
                S.dma('sp', mk_d[:, :, t * 128:(t + 1) * 128].rearrange("h p t -> p h t"), stk[0:96, :, :], R=[stkB], W=[mk_B])
                S.dma('sp', mv_d[t * 128:(t + 1) * 128, :], vb[:].rearrange("p h e -> p (h e)"), R=[vbB], W=[mv_B])

            sA_pe(0); sA_dve(0)
            for k in range(NT):
                sB(k)
                if k + 1 < NT:
                    sA_pe(k + 1)
                sC_dve(k)
                if k + 1 < NT:
                    sA_dve(k + 1)
                sC_rest(k)
            S.barrier()

        if stop == 'A2MLA':
            stA.close()
            return False
        with ExitStack() as st:
            W3 = [(T("wdf%d" % i, [128, 8, 512], BF16, st), Buf("wdf%d" % i)) for i in range(3)]
            for i, c0 in enumerate((C_DQ, C_DK, C_DV)):
                load_w(W3[i][0], W3[i][1], w_in, 0, 8, c0, 512)
            gd = T("dgd", [128, 3], F32, st); gdB = Buf("dgd")
            for half in range(2):
                S.dma('sp', gd[half * 64:(half + 1) * 64, 0:1], Wd['diff_q_gain'][l].rearrange("(p o) -> p o", o=1), W=[gdB])
                S.dma('sp', gd[half * 64:(half + 1) * 64, 1:2], Wd['diff_k_gain'][l].rearrange("(p o) -> p o", o=1), W=[gdB])
            stt(gd[:, 2:3], gd[:, 0:1], 1.0 / 8.0, gd[:, 1:2], ALU.mult, ALU.mult, [gdB], [gdB])
            pr = Ring(PI)
            NR = 2
            sq_r = Ring([(T("dsq%d" % i, [128, 512], F32, st), Buf("dsq%d" % i)) for i in range(NR)])
            rr_r = Ring([(T("drr%d" % i, [128, 32], F32, st), Buf("drr%d" % i)) for i in range(NR)])
            qn_r = Ring([(T("dqn%d" % i, [128, 512], F32, st), Buf("dqn%d" % i)) for i in range(NR)])
            kn_r = Ring([(T("dkn%d" % i, [128, 512], F32, st), Buf("dkn%d" % i)) for i in range(NR)])
            stq_r = Ring([(T("dstq%d" % i, [128, 4, 128], BF16, st), Buf("dstq%d" % i)) for i in range(NR)])
            stk_r = Ring([(T("dstk%d" % i, [128, 4, 128], BF16, st), Buf("dstk%d" % i)) for i in range(NR)])
            vb_r = Ring([(T("dvb%d" % i, [128, 4, 130], BF16, st), Buf("dvb%d" % i)) for i in range(NR)])
            for it in vb_r.items:
                memset('pool', it[0][:, :, 128:130], 1.0, [it[1]])
            prA = Ring(PI[0:3]); prB = Ring(PI[3:8])
            DS = {}

            def d0(t):
                pq, pqB = prA.next(); pk, pkB = prA.next(); pv, pvB = prA.next()
                proj_tok(pq, pqB, hT, hB, W3[0][0], W3[0][1], 0, 512, t)
                proj_tok(pk, pkB, hT, hB, W3[1][0], W3[1][1], 0, 512, t)
                proj_tok(pv, pvB, hT, hB, W3[2][0], W3[2][1], 0, 512, t)
                rr, rrB = rr_r.next()
                for (p, pB_, off) in ((pq, pqB, 0), (pk, pkB, 8)):
                    sq, sqB = sq_r.next()
                    act(sq[:], p[:, :], AF.Square, [pB_], [sqB])
                    red(rr[:, off:off + 8], sq[:].rearrange("p (g d) -> p g d", d=64), [sqB], [rrB])
                ts('dve', rr[:, 16:32], rr[:, 0:16], 1.0 / 64, ALU.mult, [rrB], [rrB], s2=EPS, op1=ALU.add)
                act(rr[:, 16:32], rr[:, 16:32], AF.Ln, [rrB], [rrB])
                act(rr[:, 16:32], rr[:, 16:32], AF.Exp, [rrB], [rrB], scale=-0.5)
                qn, qnB = qn_r.next(); kn, knB = kn_r.next(); vb, vbB = vb_r.next()
                tt('dve', qn[:].rearrange("p (g d) -> p g d", d=64), pq[:, :].rearrange("p (g d) -> p g d", d=64),
                   rr[:, 16:24].unsqueeze(2).broadcast_to([128, 8, 64]), ALU.mult, [pqB, rrB], [qnB])
                tt('dve', kn[:].rearrange("p (g d) -> p g d", d=64), pk[:, :].rearrange("p (g d) -> p g d", d=64),
                   rr[:, 24:32].unsqueeze(2).broadcast_to([128, 8, 64]), ALU.mult, [pkB, rrB], [knB])
                cp('dve', vb[:, :, 0:128], pv[:, :].rearrange("p (h e) -> p h e", e=128), [pvB], [vbB])
                DS[t] = (qn, qnB, kn, knB, vb, vbB)

            def d1(t):
                qn, qnB, kn, knB, vb, vbB = DS.pop(t)
                stq, stqB = stq_r.next(); stk, stkB = stk_r.next()
                p, pB_ = prB.next()
                for g in range(4):
                    tr(p[:, g * 128:(g + 1) * 128], qn[:, g * 128:(g + 1) * 128], [qnB], [pB_])
                act(stq[:].rearrange("p g t -> p (g t)"), p[:, :], AF.Identity, [pB_, gdB], [stqB], scale=gd[:, 2:3])
                p, pB_ = prB.next()
                for g in range(4):
                    tr(p[:, g * 128:(g + 1) * 128], kn[:, g * 128:(g + 1) * 128], [knB], [pB_])
                cp('dve', stk[:].rearrange("p g t -> p (g t)"), p[:, :], [pB_], [stkB])
                S.dma('sp', dq_d[:, :, t * 128:(t + 1) * 128].rearrange("g p t -> p g t"), stq[:], R=[stqB], W=[dq_B])
                S.dma('sp', dk_d[:, :, t * 128:(t + 1) * 128].rearrange("g p t -> p g t"), stk[:], R=[stkB], W=[dk_B])
                S.dma('sp', dv_d[t * 128:(t + 1) * 128, :], vb[:].rearrange("p h e -> p (h e)"), R=[vbB], W=[dv_B])

            d0(0)
            for k in range(NT):
                if k + 1 < NT:
                    d0(k + 1)
                d1(k)
            S.barrier()

        if stop == 'A2DIFF':
            stA.close()
            return False
        with ExitStack() as st:
            w_r = Ring([(T("wgt%d" % i, [128, 8, 512], BF16, st), Buf("wgt%d" % i)) for i in range(2)])
            stg_r = Ring([(T("gst%d" % i, [128, SEQ], BF16, st), Buf("gst%d" % i)) for i in range(3)])
            pr = Ring(PI)
            for part in range(6):
                W, WB = w_r.next()
                load_w(W, WB, w_in, 0, 8, C_GATE + part * 512, 512)
                for m in range(4):
                    stg, stgB = stg_r.next()
                    for c in range(NCH):
                        ps, psB = pr.next()
                        proj_T(ps, psB, hT, hB, W, WB, m * 128, 128, c)
                        act(stg[:, c * CH:(c + 1) * CH], ps[:, 0:CH], AF.Sigmoid, [psB], [stgB])
                    S.dma('sp', gt_d[part * 4 + m], stg[:], R=[stgB], W=[gt_B])
            S.barrier()

        if stop == 'A2G':
            stA.close()
            return False
        stA.close()
        stB = ExitStack()
        oT = [T("oT%d" % n, [128, 4, SEQ], BF16, stB) for n in range(3)]
        oTB = [[Buf("oT%d_%d" % (n, k)) for k in range(4)] for n in range(3)]

        def sb_attention():
            with ExitStack() as st:
                NB = 2
                msk_sb = T("msk_sb", [128, 4, 512], BF16, st); mskB = Buf("msk")
                memset('pool', msk_sb[:], 1.0, [mskB])
                for r in range(4):
                    def f(e, r=r):
                        return e.affine_select(out=msk_sb[:, r, :], in_=msk_sb[:, r, :], pattern=[[1, 512]], compare_op=ALU.is_gt, fill=0.0, base=-128 * r, channel_multiplier=-1)
                    S.add('pool', f, R=[mskB], W=[mskB])
                mneg = T("mneg_sb", [128, 4, 512], BF16, st); mnegB = Buf("mneg")
                ts('dve', mneg[:].rearrange("p r t -> p (r t)"), msk_sb[:].rearrange("p r t -> p (r t)"), 240.0, ALU.mult, [mskB], [mnegB], s2=-240.0, op1=ALU.add)
                qz_r = Ring([([T("sbq%d_%d" % (i, z), [128, SEQ], BF16, st) for z in range(2)], Buf("sbq%d" % i)) for i in range(NB)])
                k_r = Ring([(T("sbk%d" % i, [128, SEQ], BF16, st), Buf("sbk%d" % i)) for i in range(NB)])
                v_r = Ring([(T("sbvp%d" % i, [128, NT, 2, 128], BF16, st), Buf("sbvp%d" % i)) for i in range(NB)])
                e_r = Ring([(T("sbe%d" % i, [128, 512], F32, st), Buf("sbe%d" % i)) for i in range(2)])
                sp_r = Ring([(T("sbsp%d" % i, [128, 512], F32, st), Buf("sbsp%d" % i)) for i in range(4)])
                tm_r = Ring([(T("sbtm%d" % i, [128, 512], F32, st), Buf("sbtm%d" % i)) for i in range(2)])
                pb_r = Ring([(T("sbpb%d" % i, [128, 512], BF16, st), Buf("sbpb%d" % i)) for i in range(3)])
                wb_r = Ring([(T("sbwb%d" % i, [128, 512], BF16, st), Buf("sbwb%d" % i)) for i in range(3)])
                ra_r = Ring([(T("sbra%d" % i, [128, 512], BF16, st), Buf("sbra%d" % i)) for i in range(3)])
                pz_r = Ring(PI[0:3]); pc_r = Ring(PI[3:6]); po_r = Ring(PI[6:8])
                scale = 1.0 / 8.0
                heads = {}

                def load_hp(hp):
                    qz, qB = qz_r.next(); kt, kB = k_r.next(); vp, vB = v_r.next()
                    memset('pool', qz[0][64:128, :], 0.0, [qB]); memset('pool', qz[1][0:64, :], 0.0, [qB])
                    S.dma('sp', qz[0][0:64, :], sbqk_d[hp][0:64, :], R=[sbqk_B[hp]], W=[qB])
                    S.dma('sp', qz[1][64:128, :], sbqk_d[hp][64:128, :], R=[sbqk_B[hp]], W=[qB])
                    S.dma('sp', kt[:], sbqk_d[4 + hp], R=[sbqk_B[4 + hp]], W=[kB])
                    memset('pool', vp[:, :, 0, 64:128], 0.0, [vB]); memset('pool', vp[:, :, 1, 0:64], 0.0, [vB])
                    S.dma('sp', vp[:, :, 0, 0:64], sbv_d[:, hp * 128:hp * 128 + 64].rearrange("(t p) d -> p t d", p=128), R=[sbv_B], W=[vB])
                    S.dma('sp', vp[:, :, 1, 64:128], sbv_d[:, hp * 128 + 64:hp * 128 + 128].rearrange("(t p) d -> p t d", p=128), R=[sbv_B], W=[vB])
                    heads[hp] = (qz, qB, kt, kB, vp, vB)

                items = []
                for hp in range(4):
                    for z in range(2):
                        for c in range(NCH):
                            nblk = (c + 1) * QPC
                            grp_ = {}
                            for bi, sb in enumerate(range(nblk - 1, -1, -1)):
                                items.append(dict(hp=hp, z=z, c=c, sb=sb, bi=bi, nblk=nblk, g=grp_, idx=len(items)))
                first_idx = {}
                for it in items:
                    first_idx.setdefault(it['hp'], it['idx'])

                def s0(x):
                    if x['idx'] == 0:
                        load_hp(0)
                    if x['idx'] == first_idx[x['hp']] + 3 and x['hp'] + 1 < 4:
                        load_hp(x['hp'] + 1)
                    qz, qB, kt, kB, vp, vB = heads[x['hp']]
                    c, sb, z = x['c'], x['sb'], x['z']
                    x['pz'], x['pzB'] = pz_r.next()
                    e_, eB = e_r.next()
                    x['sp'], x['spB'] = sp_r.next()
                    diag = sb >= c * QPC
                    mm(x['pz'][:, 0:CH], kt[:, sb * 128:(sb + 1) * 128], qz[z][:, c * CH:(c + 1) * CH], True, not diag, [kB, qB], [x['pzB']])
                    if diag:
                        wd_ = 128 * (sb - c * QPC + 1)
                        mm(x['pz'][:, 0:wd_], identb[:], mneg[:, sb - c * QPC, 0:wd_], False, True, [identbB, mnegB], [x['pzB']])
                    act(e_[:, 0:CH], x['pz'][:, 0:CH], AF.Exp, [x['pzB']], [eB], scale=-scale)
                    act(x['sp'][:, 0:CH], e_[:, 0:CH], AF.Ln, [eB], [x['spB']], bias=1.0)

                def s1(x):
                    c, sb, bi, g = x['c'], x['sb'], x['bi'], x['g']
                    diag = sb >= c * QPC
                    r = sb - c * QPC
                    x['pb'], x['pbB'] = pb_r.next()
                    x['pc'], x['pcB'] = pc_r.next()
                    pb, pbB, pc, pcB = x['pb'], x['pbB'], x['pc'], x['pcB']
                    stt(pb[:, 0:CH], x['pz'][:, 0:CH], scale, x['sp'][:, 0:CH], ALU.mult, ALU.add, [x['pzB'], x['spB']], [pbB])
                    if diag:
                        wd_ = 128 * (r + 1)
                        tt('dve', pb[:, 0:wd_], pb[:, 0:wd_], msk_sb[:, r, 0:wd_], ALU.mult, [pbB, mskB], [pbB])
                    mm(pc[:, 0:CH], upper_b[:], pb[:, 0:CH], True, bi == 0, [upperB, pbB], [pcB])
                    if bi > 0:
                        ra, raB = g['ra']
                        mm(pc[:, 0:CH], ones_b[:], ra[:, 0:CH], False, True, [onesB, raB], [pcB])
                    if bi < x['nblk'] - 1:
                        ran, ranB = ra_r.next()
                        if bi == 0:
                            cp('pool', ran[:, 0:CH], pb[:, 0:CH], [pbB], [ranB])
                        else:
                            ra, raB = g['ra']
                            tt('pool', ran[:, 0:CH], ra[:, 0:CH], pb[:, 0:CH], ALU.add, [raB, pbB], [ranB])
                        g['ra'] = (ran, ranB)

                def s2(x):
                    c, sb = x['c'], x['sb']
                    diag = sb >= c * QPC
                    r = sb - c * QPC
                    tm, tmB = tm_r.next()
                    x['wb'], x['wbB'] = wb_r.next()
                    tt('dve', tm[:, 0:CH], x['pc'][:, 0:CH], x['sp'][:, 0:CH], ALU.add, [x['pcB'], x['spB']], [tmB])
                    act(x['wb'][:, 0:CH], tm[:, 0:CH], AF.Exp, [tmB], [x['wbB']], scale=-1.0)

                def s3(x):
                    qz, qB, kt, kB, vp, vB = heads[x['hp']]
                    c, sb, z, bi, g, hp = x['c'], x['sb'], x['z'], x['bi'], x['g'], x['hp']
                    if bi == 0:
                        g['po'] = po_r.next()
                    po, poB = g['po']
                    mm(po[:, 0:CH], vp[:, sb, z, :], x['wb'][:, 0:CH], bi == 0, bi == x['nblk'] - 1, [vB, x['wbB']], [poB])
                    if bi == x['nblk'] - 1:
                        po_ = 64 * z
                        cp('act', oT[0][po_:po_ + 64, hp, c * CH:(c + 1) * CH], po[po_:po_ + 64, 0:CH], [poB], [oTB[0][hp]])

                stages = [s0, s1, s2, s3]
                for k in range(len(items) + len(stages) - 1):
                    for j, stg_f in enumerate(stages):
                        ii = k - j
                        if 0 <= ii < len(items):
                            stg_f(items[ii])
                S.barrier()
        sb_attention()
        dump("o_sbT%d" % l, oT[0][:].rearrange("p j t -> p (j t)"), [128, 4 * SEQ], BF16)
        if stop == 'BSB':
            stB.close()
            return False

        with ExitStack() as st:
            NB = 2
            q_r = Ring([(T("mq%d" % i, [128, SEQ], BF16, st), Buf("mq%d" % i)) for i in range(NB)])
            k_r = Ring([(T("mk%d" % i, [128, SEQ], BF16, st), Buf("mk%d" % i)) for i in range(NB)])
            v_r = [Ring([(T("mv%d_%d" % (z, i), [128, NT, 128], BF16, st), Buf("mv%d_%d" % (z, i))) for i in range(1)]) for z in range(2)]
            for z in range(2):
                for it in v_r[z].items:
                    memset('pool', it[0][:].rearrange("p t e -> p (t e)"), 0.0, [it[1]])
                    if z == 0:
                        memset('pool', it[0][:, :, 64:65], 1.0, [it[1]])
                    else:
                        memset('pool', it[0][:, :, 0:1], 1.0, [it[1]])
            E_r = Ring([(T("mE%d" % i, [128, 512], BF16, st), Buf("mE%d" % i)) for i in range(4)])
            rd_r = Ring([(T("mrd%d" % i, [128, 512], F32, st), Buf("mrd%d" % i)) for i in range(2)])
            pz_r = Ring([PI[0], PI[1], PI[6]]); pa_r = Ring([PI[2], PI[3], PI[4]]); pb_r = Ring([PI[5], PI[7]])
            heads = {}

            def load_h(h):
                z = h % 2
                qT, qB = q_r.next(); kT, kB = k_r.next(); vh, vB = v_r[z].next()
                S.dma('sp', qT[0:96, :], mq_d[h], R=[mq_B], W=[qB])
                S.dma('sp', kT[0:96, :], mk_d[h], R=[mk_B], W=[kB])
                c0 = 0 if z == 0 else 64
                S.dma('sp', vh[:, :, c0:c0 + 64], mv_d[:, h * 66:h * 66 + 64].rearrange("(t p) e -> p t e", p=128), R=[mv_B], W=[vB])
                heads[h] = (qT, qB, kT, kB, vh, vB)

            items = []
            for h in range(8):
                for c in range(NCH):
                    n_sb = (c + 1) * QPC
                    g_ = {}
                    for sb in range(n_sb):
                        items.append(dict(h=h, c=c, sb=sb, last=(sb == n_sb - 1), g=g_, idx=len(items)))
            first_idx = {}
            for it in items:
                first_idx.setdefault(it['h'], it['idx'])

            def s0(x):
                h, c, sb = x['h'], x['c'], x['sb']
                if x['idx'] == 0:
                    load_h(0)
                if x['idx'] == first_idx[h] + 2 and h + 1 < 8:
                    load_h(h + 1)
                qT, qB, kT, kB, vh, vB = heads[h]
                q0 = max(0, sb - c * QPC); f0 = q0 * 128
                x['pz'], x['pzB'] = pz_r.next()
                mm(x['pz'][:, f0:CH], kT[0:96, sb * 128:(sb + 1) * 128], qT[0:96, c * CH + f0:(c + 1) * CH], True, True, [kB, qB], [x['pzB']])

            def s1(x):
                c, sb = x['c'], x['sb']
                q0 = max(0, sb - c * QPC); f0 = q0 * 128
                x['E'], x['EB'] = E_r.next()
                E, EB = x['E'], x['EB']
                act(E[:, f0:CH], x['pz'][:, f0:CH], AF.Exp, [x['pzB']], [EB])
                if sb >= c * QPC:
                    tt('pool', E[:, f0:f0 + 128], E[:, f0:f0 + 128], tri_b[:], ALU.mult, [EB, triB], [EB])

            def s2(x):
                h, c, sb, g = x['h'], x['c'], x['sb'], x['g']
                hp, z = h // 2, h % 2
                qT, qB, kT, kB, vh, vB = heads[h]
                q0 = max(0, sb - c * QPC); f0 = q0 * 128
                E, EB = x['E'], x['EB']
                if sb == 0:
                    g['acc'] = pa_r.next()
                acc, accB = g['acc']
                M_ = 65 if z == 0 else 128
                mm(acc[0:M_, f0:CH], vh[:, sb, 0:M_], E[:, f0:CH], sb == 0, x['last'], [EB, vB], [accB])
                if not x['last']:
                    return
                rd, rdB = rd_r.next(); bc, bcB = pb_r.next()
                dp = 64 if z == 0 else 0
                o0 = 0 if z == 0 else 64
                S.add('dve', (lambda o_, i_: (lambda e: e.reciprocal(out=o_, in_=i_)))(rd[dp:dp + 1, 0:CH], acc[dp:dp + 1, 0:CH]), R=[accB], W=[rdB])
                mm(bc[0:128, 0:CH], ones_f[dp:dp + 1, 0:128], rd[dp:dp + 1, 0:CH], True, True, [onesfB, rdB], [bcB])
                cp('act', rd[o0:o0 + 64, 0:CH], bc[o0:o0 + 64, 0:CH], [bcB], [rdB])
                tt('dve', oT[1][o0:o0 + 64, hp, c * CH:(c + 1) * CH], acc[o0:o0 + 64, 0:CH], rd[o0:o0 + 64, 0:CH], ALU.mult, [accB, rdB], [oTB[1][hp]])

            stages = [s0, s1, s2]
            for k in range(len(items) + len(stages) - 1):
                for j, stg_f in enumerate(stages):
                    ii = k - j
                    if 0 <= ii < len(items):
                        stg_f(items[ii])
            S.barrier()
        dump("o_mlaT%d" % l, oT[1][:].rearrange("p j t -> p (j t)"), [128, 4 * SEQ], BF16)
        if stop == 'BMLA':
            stB.close()
            return False

        with ExitStack() as st:
            posrow = T("posrow", [128, SEQ], F32, st); prwB = Buf("posrow")
            with ExitStack() as st2:
                pri = T("posrow_i", [128, SEQ], I32, st2); priB = Buf("posrow_i")
                S.dma('sp', pri[:], pos_d.partition_broadcast(128), W=[priB])
                cp('dve', posrow[:], pri[:], [priB], [prwB])
                S.barrier()
            negpos = T("negpos", [128, NT], F32, st)
            ts('dve', negpos[:], pos_tok[:], -1.0, ALU.mult, [posB], [prwB])
            lp = T("dlp", [128, 256], F32, st); lpB = Buf("dlp")
            lam = T("dlam", [128, 4], F32, st); lamB = Buf("dlam")
            prd = T("dprd", [128, 128], F32, st); prdB = Buf("dprd")
            S.dma('sp', lp[:], Wd['diff_lambda'][l].rearrange("a d -> (a d)").partition_broadcast(128), W=[lpB])
            tt('dve', prd[:, 0:64], lp[:, 0:64], lp[:, 64:128], ALU.mult, [lpB], [prdB])
            tt('dve', prd[:, 64:128], lp[:, 128:192], lp[:, 192:256], ALU.mult, [lpB], [prdB])
            red(lam[:, 0:2], prd[:].rearrange("p (a d) -> p a d", d=64), [prdB], [lamB])
            act(lam[:, 0:2], lam[:, 0:2], AF.Exp, [lamB], [lamB])
            tt('dve', lam[:, 2:3], lam[:, 1:2], lam[:, 0:1], ALU.subtract, [lamB], [lamB])
            ts('dve', lam[:, 3:4], lam[:, 2:3], -lam_init, ALU.add, [lamB], [lamB])
            sg = T("dsg", [128, 2], F32, st); sgB = Buf("dsg")
            S.dma('sp', sg[:, 0:1], Wd['diff_subln'][l].rearrange("(p o) -> p o", o=1), W=[sgB])
            ts('dve', sg[:, 1:2], sg[:, 0:1], 1.0 - lam_init, ALU.mult, [sgB], [sgB])
            NB = 2
            qz_r = Ring([([T("dq%d_%d" % (i, z), [128, SEQ], BF16, st) for z in range(2)], Buf("dq%d" % i)) for i in range(NB)])
            k_r = Ring([(T("dk%d" % i, [128, SEQ], BF16, st), Buf("dk%d" % i)) for i in range(NB)])
            v_r = Ring([(T("dv%d" % i, [128, NT, 130], BF16, st), Buf("dv%d" % i)) for i in range(NB)])
            E_r = Ring([(T("dE%d" % i, [128, 512], BF16, st), Buf("dE%d" % i)) for i in range(4)])
            dt_r = Ring([(T("ddt%d" % i, [128, 512], F32, st), Buf("ddt%d" % i)) for i in range(3)])
            zz_r = Ring([(T("dzz%d" % i, [128, 512], F32, st), Buf("dzz%d" % i)) for i in range(3)])
            o1_r = Ring([(T("do1%d" % i, [128, QPC, 128], F32, st), Buf("do1%d" % i)) for i in range(2)])
            od_r = Ring([(T("dod%d" % i, [128, 128], F32, st), Buf("dod%d" % i)) for i in range(6)])
            on_r = Ring([(T("don%d" % i, [128, 128], F32, st), Buf("don%d" % i)) for i in range(3)])
            jk_r = Ring([(T("djk%d" % i, [128, 128], BF16, st), Buf("djk%d" % i)) for i in range(2)])
            rc_r = Ring([(T("drc%d" % i, [128, 4], F32, st), Buf("drc%d" % i)) for i in range(8)])
            pz_r = Ring([PI[0], PI[1], PI[6]]); pt_r = Ring(PI[7:8])
            heads = {}

            def load_h(h):
                qz, qB = qz_r.next(); kp, kB = k_r.next(); vh, vB = v_r.next()
                memset('pool', qz[0][64:128, :], 0.0, [qB]); memset('pool', qz[1][0:64, :], 0.0, [qB])
                S.dma('sp', qz[0][0:64, :], dq_d[h][0:64, :], R=[dq_B], W=[qB])
                S.dma('sp', qz[1][64:128, :], dq_d[h][64:128, :], R=[dq_B], W=[qB])
                S.dma('sp', kp[:], dk_d[h], R=[dk_B], W=[kB])
                S.dma('sp', vh[:], dv_d[:, h * 130:(h + 1) * 130].rearrange("(t p) e -> p t e", p=128), R=[dv_B], W=[vB])
                heads[h] = (qz, qB, kp, kB, vh, vB)

            CD = 256; QD = 2; NCD = SEQ // CD
            items = []
            for h in range(4):
                for c in range(NCD):
                    n_sb = (c + 1) * QD
                    for sb in range(n_sb):
                        items.append(dict(h=h, c=c, sb=sb, last=(sb == n_sb - 1), idx=len(items)))
            first_idx = {}
            for it in items:
                first_idx.setdefault(it['h'], it['idx'])

            def s0(x):
                h, c, sb = x['h'], x['c'], x['sb']
                if x['idx'] == 0:
                    load_h(0)
                if x['idx'] == first_idx[h] + 2 and h + 1 < 4:
                    load_h(h + 1)
                qz, qB, kp, kB, vh, vB = heads[h]
                q0 = max(0, sb - c * QD); f0 = q0 * 128
                x['pz'], x['pzB'] = pz_r.next()
                x['dt'], x['dtB'] = dt_r.next()
                for m in range(2):
                    mm(x['pz'][:, m * CD + f0:(m + 1) * CD], kp[:, sb * 128:(sb + 1) * 128], qz[m][:, c * CD + f0:(c + 1) * CD], True, True, [kB, qB], [x['pzB']])
                act(x['dt'][:, f0:CD], posrow[:, c * CD + f0:(c + 1) * CD], AF.Abs, [prwB], [x['dtB']], bias=negpos[:, sb:sb + 1])

            def s1(x):
                h, c, sb = x['h'], x['c'], x['sb']
                slope = 2.0 ** (-8.0 * (h + 1) / 4.0)
                q0 = max(0, sb - c * QD); f0 = q0 * 128
                zz, zzB = zz_r.next()
                x['E'], x['EB'] = E_r.next()
                E, EB = x['E'], x['EB']
                for m in range(2):
                    stt(zz[:, m * CD + f0:(m + 1) * CD], x['dt'][:, f0:CD], -slope, x['pz'][:, m * CD + f0:(m + 1) * CD], ALU.mult, ALU.add, [x['dtB'], x['pzB']], [zzB])
                if f0 == 0:
                    act(E[:, 0:2 * CD], zz[:, 0:2 * CD], AF.Exp, [zzB], [EB])
                else:
                    for m in range(2):
                        act(E[:, m * CD + f0:(m + 1) * CD], zz[:, m * CD + f0:(m + 1) * CD], AF.Exp, [zzB], [EB])
                if sb >= c * QD:
                    for m in range(2):
                        tt('pool', E[:, m * CD + f0:m * CD + f0 + 128], E[:, m * CD + f0:m * CD + f0 + 128], tri_b[:], ALU.mult, [EB, triB], [EB])

            def s2(x):
                h, c, sb = x['h'], x['c'], x['sb']
                qz, qB, kp, kB, vh, vB = heads[h]
                q0 = max(0, sb - c * QD)
                E, EB = x['E'], x['EB']
                for q in range(q0, QD):
                    qb = c * QD + q
                    for m in range(2):
                        bi_ = 2 + q * 2 + m
                        mm(PS[bi_][:, 0:130], E[:, m * CD + q * 128:m * CD + (q + 1) * 128], vh[:, sb, :], sb == 0, sb == qb, [EB, vB], [PB[bi_]])
                if not x['last']:
                    return
                o1, o1B = o1_r.next()
                fin = []
                for q in range(QD):
                    qb = c * QD + q
                    b0 = 2 + q * 2; b1 = b0 + 1
                    rc, rcB = rc_r.next()
                    S.add('dve', (lambda o_, i_: (lambda e: e.reciprocal(out=o_, in_=i_)))(rc[:, 0:1], PS[b0][:, 128:129]), R=[PB[b0]], W=[rcB])
                    S.add('dve', (lambda o_, i_: (lambda e: e.reciprocal(out=o_, in_=i_)))(rc[:, 1:2], PS[b1][:, 128:129]), R=[PB[b1]], W=[rcB])
                    ts('dve', o1[:, q, :], PS[b0][:, 0:128], rc[:, 0:1], ALU.mult, [PB[b0], rcB], [o1B])
                    od, odB = od_r.next(); jk, jkB = jk_r.next()
                    tt('dve', rc[:, 1:2], rc[:, 1:2], lam[:, 3:4], ALU.mult, [rcB, lamB], [rcB])
                    stt(od[:], PS[b1][:, 0:128], rc[:, 1:2], o1[:, q, :], ALU.mult, ALU.add, [PB[b1], rcB, o1B], [odB])
                    act(jk[:], od[:], AF.Square, [odB], [jkB, rcB], accum_out=rc[:, 2:3])
                    fin.append((qb, rc, rcB, od, odB))

                def p1(fin=fin):
                    for (qb, rc, rcB, od, odB) in fin:
                        ts('dve', rc[:, 3:4], rc[:, 2:3], 1.0 / 128, ALU.mult, [rcB], [rcB], s2=EPS, op1=ALU.add)
                        act(rc[:, 3:4], rc[:, 3:4], AF.Ln, [rcB], [rcB])
                        act(rc[:, 3:4], rc[:, 3:4], AF.Exp, [rcB], [rcB], scale=-0.5)

                def p2(fin=fin, h=h):
                    for (qb, rc, rcB, od, odB) in fin:
                        on, onB = on_r.next()
                        ts('dve', on[:], od[:], rc[:, 3:4], ALU.mult, [odB, rcB], [onB])
                        pt, ptB = pt_r.next()
                        tr(pt[:, 0:128], on[:], [onB], [ptB])
                        act(oT[2][:, h, qb * 128:(qb + 1) * 128], pt[:, 0:128], AF.Identity, [ptB, sgB], [oTB[2][h]], scale=sg[:, 1:2])
                deferred.append((cur_step[0] + 1, p1)); deferred.append((cur_step[0] + 2, p2))

            deferred = []
            cur_step = [0]
            stages = [s0, s1, s2]
            for k in range(len(items) + len(stages) - 1):
                cur_step[0] = k
                for j, stg_f in enumerate(stages):
                    ii = k - j
                    if 0 <= ii < len(items):
                        stg_f(items[ii])
                due = [d_ for d_ in deferred if d_[0] <= k]
                deferred[:] = [d_ for d_ in deferred if d_[0] > k]
                for _, fn_ in due:
                    fn_()
            for _, fn_ in deferred:
                fn_()
            S.barrier()
        dump("o_diffT%d" % l, oT[2][:].rearrange("p j t -> p (j t)"), [128, 4 * SEQ], BF16)
        if stop == 'BDIFF':
            stB.close()
            return False

        with ExitStack() as st:
            wbr = T("wbr", [128, 12, D], BF16, st); wbrB = Buf("wbr")
            wbsrc = Wd['w_branch'][l].rearrange("n k d -> (n k) d")
            for i in range(3):
                S.dma('pool', wbr[:, i * 4:(i + 1) * 4, :], wbsrc[i * 512:(i + 1) * 512, :].rearrange("(j p) n -> p j n", p=128), W=[wbrB])
            wo = T("wo", [128, 8, D], BF16, st); woB = Buf("wo")
            for i in range(2):
                S.dma('pool', wo[:, i * 4:(i + 1) * 4, :], Wd['w_out'][l][i * 512:(i + 1) * 512, :].rearrange("(j p) n -> p j n", p=128), W=[woB])
            for j in range(8):
                tt('pool' if j % 2 else 'dve', wo[:, j, :], wo[:, j, :], g1g2[:, 0, :], ALU.mult, [woB, g12B], [woB])
            gts_r = Ring([(T("gts%d" % i, [128, 3, CH], BF16, st), Buf("gts%d" % i)) for i in range(3)])
            M_r = Ring([(T("mM%d" % i, [128, 8, CH], BF16, st), Buf("mM%d" % i)) for i in range(2)])
            tn_r = Ring([(T("mtn%d" % i, [128, 512], F32, st), Buf("mtn%d" % i)) for i in range(6)])
            gt_v = gt_d.rearrange("(n j) p t -> j p n t", n=3)
            pr = Ring(PI)
            for c in range(NCH):
                M, MB = M_r.next()
                for j in range(8):
                    gts, gtsB = gts_r.next()
                    S.dma('sp', gts[:], gt_v[j][:, :, c * CH:(c + 1) * CH], R=[gt_B], W=[gtsB])
                    tn = []
                    for n in range(3):
                        py, pyB = pr.next()
                        for kc in range(4):
                            mm(py[:, 0:CH], wbr[:, n * 4 + kc, j * 128:(j + 1) * 128], oT[n][:, kc, c * CH:(c + 1) * CH], kc == 0, kc == 3, [wbrB, oTB[n][kc]], [pyB])
                        t_, tB_ = tn_r.next()
                        tt('dve', t_[:, 0:CH], py[:, 0:CH], gts[:, n, :], ALU.mult, [pyB, gtsB], [tB_])
                        tn.append((t_, tB_))
                    tt('dve', tn[0][0][:, 0:CH], tn[0][0][:, 0:CH], tn[1][0][:, 0:CH], ALU.add, [tn[0][1], tn[1][1]], [tn[0][1]])
                    tt('pool', M[:, j, :], tn[0][0][:, 0:CH], tn[2][0][:, 0:CH], ALU.add, [tn[0][1], tn[2][1]], [MB])
                for q in range(QPC):
                    t = c * QPC + q
                    for half in range(2):
                        px, pxB = pr.next()
                        for j in range(8):
                            mm(px[:, :], M[:, j, q * 128:(q + 1) * 128], wo[:, j, half * 512:(half + 1) * 512], j == 0, j == 7, [MB, woB], [pxB])
                        tt('dve', xs[:, t, half * 512:(half + 1) * 512], px[:, :], xs[:, t, half * 512:(half + 1) * 512], ALU.add, [pxB, xB[t]], [xB[t]])
            S.barrier()
        stB.close()
        dump("xmid%d" % l, xs[:].rearrange("p t d -> p (t d)"), [128, NT * D])
        if stop == 'C':
            return False
        stA = ExitStack()
        hT = T("hT2", [128, 8, SEQ], BF16, stA)

        moe = (l % 2 == 1)
        jl = l // 2
        with ExitStack() as st:
            router = None
            if moe:
                wr = T("wr", [128, 8, 8], F32, st); wrB = Buf("wr")
                wr2 = T("wr2", [128, 8, 8], F32, st); wr2B = Buf("wr2")
                bbc = T("bbc", [128, 8, 128], F32, st); bbcB = Buf("bbc")
                brB_t = T("brB", [128, 8], F32, st); brBB = Buf("brB")
                S.dma('sp', wr[:], Wd['w_router'][jl].rearrange("(j p) e -> p j e", p=128), W=[wrB])
                tt('dve', wr2[:], wr[:], ABT[:, 8:16].unsqueeze(2).broadcast_to([128, 8, 8]), ALU.mult, [wrB, ABTB], [wr2B])
                cp('dve', bbc[:], modT[:, 16:24].unsqueeze(2).broadcast_to([128, 8, 128]), [modTB], [bbcB])
                pl, plB = PI[4]
                for j in range(8):
                    mm(pl[:, 0:8], bbc[:, j, :], wr[:, j, :], j == 0, j == 7, [bbcB, wrB], [plB])
                cp('dve', brB_t[:], pl[:, 0:8], [plB], [brBB])
                xt_r = Ring([(T("rxt%d" % i, [128, 8, 128], F32, st), Buf("rxt%d" % i)) for i in range(2)])
                rt_r = Ring([(T("rrt%d" % i, [128, 48], F32, st), Buf("rrt%d" % i)) for i in range(2)])
                prr = Ring(PI[4:6])

                def router(t, pa, paB, pb, pbB, aoff, boff):
                    xt, xtB = xt_r.next(); rt, rtB = rt_r.next()
                    cp('dve', xt[:, 0:4, :].rearrange("p j t -> p (j t)"), pa[:, :], [paB], [xtB])
                    cp('act', xt[:, 4:8, :].rearrange("p j t -> p (j t)"), pb[:, :], [pbB], [xtB])
                    pl_, plB_ = prr.next()
                    for j in range(8):
                        mm(pl_[:, 0:8], xt[:, j, :], wr2[:, j, :], j == 0, j == 7, [xtB, wr2B], [plB_])
                    lg = rt[:, 0:8]; mx = rt[:, 8:16]; mk = rt[:, 16:24]; ex = rt[:, 24:32]
                    tt('dve', lg, pl_[:, 0:8], brB_t[:], ALU.add, [plB_, brBB], [rtB])
                    S.add('dve', (lambda o_, i_: (lambda e: e.max(out=o_, in_=i_)))(mx, lg), R=[rtB], W=[rtB])
                    ts('dve', mk, lg, rt[:, 9:10], ALU.is_ge, [rtB], [rtB])
                    ts('dve', rt[:, 32:33], rt[:, 8:9], -1.0, ALU.mult, [rtB], [rtB])
                    act(ex, lg, AF.Exp, [rtB], [rtB], bias=rt[:, 32:33])
                    tt('dve', ex, ex, mk, ALU.mult, [rtB], [rtB])
                    red(rt[:, 33:34], ex, [rtB], [rtB])
                    S.add('dve', (lambda o_, i_: (lambda e: e.reciprocal(out=o_, in_=i_)))(rt[:, 34:35], rt[:, 33:34]), R=[rtB], W=[rtB])
                    ts('dve', comb[:, t, :], ex, rt[:, 34:35], ALU.mult, [rtB], [combB])
            norm_phase(2, hT, hB, st, router)
            S.barrier()
        dump("h2T%d" % l, hT[:].rearrange("p j t -> p (j t)"), [128, 8 * SEQ], BF16)
        dump("comb%d" % l, comb[:].rearrange("p t e -> p (t e)"), [128, NT * 8])

        with ExitStack() as st:
            if moe:
                units = [(Wd['w_exp_gate'][jl][e], Wd['w_exp_up'][jl][e], Wd['w_exp_down'][jl][e], e) for e in range(8)]
            else:
                units = [(Wd['w_ffn_gate'][jl], Wd['w_ffn_up'][jl], Wd['w_ffn_down'][jl], None)]
            groups = [(0, 4), (4, 4), (8, 4), (12, 4), (16, 4), (20, 2)]
            wg_r = Ring([(T("fwg%d" % i, [128, 8, 512], BF16, st), Buf("fwg%d" % i)) for i in range(2)])
            wu_r = Ring([(T("fwu%d" % i, [128, 8, 512], BF16, st), Buf("fwu%d" % i)) for i in range(2)])
            wd_r = Ring([(T("fwd%d" % i, [128, 4, D], BF16, st), Buf("fwd%d" % i)) for i in range(2)])
            aT_r = Ring([(T("faT%d" % i, [128, 4, SEQ], BF16, st), Buf("faT%d" % i)) for i in range(2)])
            sg_r = Ring([(T("fsg%d" % i, [128, 512], F32, st), Buf("fsg%d" % i)) for i in range(3)])
            pr = Ring(PI)
            for (wg2, wu2, wd2, e) in units:
                for (f0, nf) in groups:
                    wg, wgB = wg_r.next(); wu, wuB = wu_r.next(); wd_, wdB = wd_r.next(); aT, aTB = aT_r.next()
                    load_w(wg, wgB, wg2, 0, 8, f0 * 128, nf * 128)
                    load_w(wu, wuB, wu2, 0, 8, f0 * 128, nf * 128)
                    S.dma('pool', wd_[:, 0:nf, :], wd2[f0 * 128:(f0 + nf) * 128, :].rearrange("(j p) n -> p j n", p=128), W=[wdB])
                    for fc in range(nf):
                        tt('pool', wd_[:, fc, :], wd_[:, fc, :], g1g2[:, 1, :], ALU.mult, [wdB, g12B], [wdB])
                    for fc in range(nf):
                        for c in range(NCH):
                            pg, pgB = pr.next(); pu, puB = pr.next()
                            proj_T(pg, pgB, hT, hB, wg, wgB, fc * 128, 128, c)
                            proj_T(pu, puB, hT, hB, wu, wuB, fc * 128, 128, c)
                            sg_, sgB_ = sg_r.next()
                            act(sg_[:, 0:CH], pg[:, 0:CH], AF.Silu, [pgB], [sgB_])
                            tt('dve', aT[:, fc, c * CH:(c + 1) * CH], sg_[:, 0:CH], pu[:, 0:CH], ALU.mult, [sgB_, puB], [aTB])
                    for t in range(NT):
                        for half in range(2):
                            px, pxB = pr.next()
                            for fc in range(nf):
                                mm(px[:, :], aT[:, fc, t * 128:(t + 1) * 128], wd_[:, fc, half * 512:(half + 1) * 512], fc == 0, fc == nf - 1, [aTB, wdB], [pxB])
                            xsl = xs[:, t, half * 512:(half + 1) * 512]
                            if e is None:
                                tt('dve', xsl, px[:, :], xsl, ALU.add, [pxB, xB[t]], [xB[t]])
                            else:
                                stt(xsl, px[:, :], comb[:, t, e:e + 1], xsl, ALU.mult, ALU.add, [pxB, combB, xB[t]], [xB[t]])
            S.barrier()
        stA.close()
        dump("xout%d" % l, xs[:].rearrange("p t d -> p (t d)"), [128, NT * D])

        return True

    for l in layers:
        if not layer_body(l):
            break

    g_out = S.grp("out_sp")
    S.barrier()
    for t in range(NT):
        S.dma('sp', y_d[t * 128:(t + 1) * 128, :], xs[:, t, :], R=[xB[t]], grp=g_out)
    S.barrier(['sp'])
    S.emit(nc, es)
    es.close()
    global LAST_S
    LAST_S = S
    return nc


LAST_S = None


_NC_CACHE = {}


def kernel(**inputs):
    B = 8
    SEQ = 2048
    if 'nc' not in _NC_CACHE:
        _NC_CACHE['nc'] = build(SEQ=SEQ, layers=(0, 1))
    nc = _NC_CACHE['nc']
    x = np.ascontiguousarray(inputs['x'], dtype=np.float32)
    c = np.ascontiguousarray(inputs['c'], dtype=np.float32)
    pos = np.ascontiguousarray(inputs['positions'], dtype=np.int32)
    wts = {n: np.ascontiguousarray(inputs[n], dtype=np.float32) for n in WNAMES}
    in_maps = []
    for b in range(B):
        m = {'x': x[b], 'c': c[b], 'positions': pos[b]}
        m.update(wts)
        in_maps.append(m)
    res = run_bass_kernel_spmd(nc, in_maps, core_ids=list(range(B)))
    return np.stack([np.asarray(r['y'], dtype=np.float32) for r in res.results], axis=0)
```

```python
import math
from contextlib import ExitStack
import numpy as np
import concourse.bass as bass
import concourse.mybir as mybir
from concourse.alu_op_type import AluOpType as ALU
from concourse.bass_utils import run_bass_kernel_spmd

F32 = mybir.dt.float32
BF16 = mybir.dt.bfloat16
I32 = mybir.dt.int32
AF = mybir.ActivationFunctionType
AX = mybir.AxisListType

ENGS = ['pe', 'act', 'dve', 'pool', 'sp']
SAME_ENG_SYNC = True


class Buf:
    __slots__ = ('name', 'w', 'r', 'sbuf', 'g')

    def __init__(self, name, sbuf=True):
        self.name = name
        self.w = None
        self.r = {}
        self.sbuf = sbuf
        self.g = None


class Grp:
    __slots__ = ('name', 'sem', 'count')

    def __init__(self, name):
        self.name = name
        self.sem = None
        self.count = 0


class Sched:
    def __init__(self):
        self.ops = {e: [] for e in ENGS}
        self.seen = {e: {} for e in ENGS}
        self.grps = []
        self.named = {}

    def grp(self, name):
        g = Grp(name)
        self.grps.append(g)
        return g

    def add(self, eng, fn, R=(), W=(), grp=None):
        idx = len(self.ops[eng])
        deps = []
        for b in R:
            if b.w is not None:
                deps.append(b.w)
        for b in W:
            if b.w is not None:
                deps.append(b.w)
            deps.extend(b.r.values())
        waits = {}
        seen = self.seen[eng]
        for d in deps:
            if d[0] == 'c':
                _, e2, i2 = d
                if e2 == eng and (eng == 'pe' or not SAME_ENG_SYNC):
                    continue
                key = ('c', e2)
                val = i2
            else:
                g = d[1]
                key = ('d', g)
                val = g.count
            if seen.get(key, -1) >= val:
                continue
            if waits.get(key, -1) < val:
                waits[key] = val
        for k, v in waits.items():
            seen[k] = v
            if k[0] == 'c':
                self.ops[k[1]][v]['sig'] = True
        if grp is not None:
            grp.count += 1
            tok = ('d', grp, grp.count)
            rkey = grp
        else:
            tok = ('c', eng, idx)
            rkey = eng
        self.ops[eng].append(dict(fn=fn, waits=waits, sig=False, grp=grp))
        for b in R:
            b.r[rkey] = tok
        for b in W:
            b.w = tok
            b.r = {}
        return tok

    def dma(self, q, out, in_, R=(), W=(), grp=None, **kw):
        if grp is None:
            owner = None
            for b in list(W) + list(R):
                if getattr(b, 'sbuf', False):
                    owner = b
                    break
            key = q
            if owner.g is None:
                owner.g = {}
            if key not in owner.g:
                gname = owner.name + '_' + q
                if gname not in self.named:
                    self.named[gname] = self.grp(gname)
                owner.g[key] = self.named[gname]
            grp = owner.g[key]
        return self.add(q, lambda e: e.dma_start(out=out, in_=in_, **kw), R=R, W=W, grp=grp)

    def last_compute(self, e):
        ops = self.ops[e]
        for i in range(len(ops) - 1, -1, -1):
            if ops[i]['fn'] is not None and ops[i]['grp'] is None:
                return i
        return None

    def barrier(self, engs=None):
        lasts = {e: self.last_compute(e) for e in ENGS}
        for e in (engs or ENGS):
            waits = {}
            seen = self.seen[e]
            for e2 in ENGS:
                if e2 == e or lasts[e2] is None:
                    continue
                if seen.get(('c', e2), -1) >= lasts[e2]:
                    continue
                waits[('c', e2)] = lasts[e2]
                self.ops[e2][lasts[e2]]['sig'] = True
            for g in self.grps:
                if g.count > 0 and seen.get(('d', g), -1) < g.count:
                    waits[('d', g)] = g.count
            for k, v in waits.items():
                seen[k] = v
            self.ops[e].append(dict(fn=None, waits=waits, sig=False, grp=None))

    def emit(self, nc, es):
        csem = {e: es.enter_context(nc.semaphore('c_' + e)) for e in ENGS}
        for g in self.grps:
            g.sem = es.enter_context(nc.semaphore('g_' + g.name))
        ordv = {}
        for e in ENGS:
            n = 0
            o = []
            for op in self.ops[e]:
                if op['sig'] and op['grp'] is None:
                    n += 1
                o.append(n)
            ordv[e] = o
        block = es.enter_context(nc.Block())
        ops = self.ops

        def run(e, eng):
            for op in ops[e]:
                for key, val in op['waits'].items():
                    if key[0] == 'c':
                        eng.wait_ge(csem[key[1]], ordv[key[1]][val])
                    else:
                        eng.wait_ge(key[1].sem, 16 * val)
                if op['fn'] is None:
                    continue
                inst = op['fn'](eng)
                if op['grp'] is not None:
                    inst.then_inc(op['grp'].sem, 16)
                elif op['sig']:
                    inst.then_inc(csem[e], 1)

        @block.tensor
        def _(eng):
            run('pe', eng)

        @block.scalar
        def _(eng):
            run('act', eng)

        @block.vector
        def _(eng):
            run('dve', eng)

        @block.gpsimd
        def _(eng):
            run('pool', eng)

        @block.sync
        def _(eng):
            run('sp', eng)


D = 1024
EPS = 1e-6
IN_COLS = 7200
C_SBQ, C_SBK, C_SBV = 0, 512, 1024
C_MLA = 1536
C_DQ, C_DK, C_DV = 2592, 3104, 3616
C_GATE = 4128
DFF = 2816
WNAMES = ['norm1_g', 'norm2_g', 'w_ada', 'b_ada', 'w_in', 'mla_q_norm', 'w_mla_uq', 'mla_kv_norm', 'w_mla_ukv',
          'mla_q_gain', 'mla_k_gain', 'diff_q_gain', 'diff_k_gain', 'diff_lambda', 'diff_subln', 'w_branch',
          'w_out', 'w_ffn_gate', 'w_ffn_up', 'w_ffn_down', 'w_router', 'w_exp_gate', 'w_exp_up', 'w_exp_down']
WSHAPES = {
    'norm1_g': [2, 1024], 'norm2_g': [2, 1024], 'w_ada': [2, 1024, 6144], 'b_ada': [2, 6144],
    'w_in': [2, 1024, 7200], 'mla_q_norm': [2, 768], 'w_mla_uq': [2, 768, 768], 'mla_kv_norm': [2, 256],
    'w_mla_ukv': [2, 256, 1024], 'mla_q_gain': [2, 96], 'mla_k_gain': [2, 96], 'diff_q_gain': [2, 64],
    'diff_k_gain': [2, 64], 'diff_lambda': [2, 4, 64], 'diff_subln': [2, 128], 'w_branch': [2, 3, 512, 1024],
    'w_out': [2, 1024, 1024], 'w_ffn_gate': [1, 1024, 2816], 'w_ffn_up': [1, 1024, 2816],
    'w_ffn_down': [1, 2816, 1024], 'w_router': [1, 1024, 8], 'w_exp_gate': [1, 8, 1024, 2816],
    'w_exp_up': [1, 8, 1024, 2816], 'w_exp_down': [1, 8, 2816, 1024],
}


class Ring:
    def __init__(self, items):
        self.items = items
        self.i = 0

    def next(self):
        it = self.items[self.i % len(self.items)]
        self.i += 1
        return it


def build(SEQ=2048, layers=(0, 1), stop=None, dbg=(), wshapes=None, mcut=99):
    NT = SEQ // 128
    CH = min(512, SEQ)
    NCH = SEQ // CH
    QPC = CH // 128
    nc = bass.Bass("TRN2", target_bir_lowering=False)
    S = Sched()
    es = ExitStack()
    dbg_out = {}

    x_d = nc.dram_tensor("x", [SEQ, D], F32, kind="ExternalInput").ap()
    c_d = nc.dram_tensor("c", [D], F32, kind="ExternalInput").ap()
    pos_d = nc.dram_tensor("positions", [SEQ], I32, kind="ExternalInput").ap()
    Wd = {n: nc.dram_tensor(n, (wshapes or WSHAPES)[n], F32, kind="ExternalInput").ap() for n in WNAMES}
    y_d = nc.dram_tensor("y", [SEQ, D], F32, kind="ExternalOutput").ap()

    def scratch(name, shape, dt=BF16):
        return nc.dram_tensor(name, shape, dt, kind="Internal").ap()
    sbqk_d = scratch("s_sbqk", [8, 128, SEQ]); sbqk_B = [Buf("s_sbqk%d" % i, False) for i in range(8)]
    sbv_d = scratch("s_sbv", [SEQ, 512]); sbv_B = Buf("s_sbv", False)
    mq_d = scratch("s_mq", [8, 96, SEQ]); mq_B = Buf("s_mq", False)
    mk_d = scratch("s_mk", [8, 96, SEQ]); mk_B = Buf("s_mk", False)
    mv_d = scratch("s_mv", [SEQ, 8 * 66]); mv_B = Buf("s_mv", False)
    dq_d = scratch("s_dq", [4, 128, SEQ]); dq_B = Buf("s_dq", False)
    dk_d = scratch("s_dk", [4, 128, SEQ]); dk_B = Buf("s_dk", False)
    dv_d = scratch("s_dv", [SEQ, 4 * 130]); dv_B = Buf("s_dv", False)
    gt_d = scratch("s_gt", [24, 128, SEQ]); gt_B = Buf("s_gt", False)

    uid = [0]

    def T(name, shape, dt, st=None):
        uid[0] += 1
        return (st or es).enter_context(nc.sbuf_tensor("%s_u%d" % (name, uid[0]), shape, dt))

    def act(out, in_, func, R, W, **kw):
        return S.add('act', lambda e: e.activation(out=out, in_=in_, func=func, **kw), R=R, W=W)

    def tt(eng, out, in0, in1, op, R, W):
        return S.add(eng, lambda e: e.tensor_tensor(out=out, in0=in0, in1=in1, op=op), R=R, W=W)

    def ts(eng, out, in0, s1, op0, R, W, s2=None, op1=None):
        if op1 is None:
            return S.add(eng, lambda e: e.tensor_scalar(out=out, in0=in0, scalar1=s1, scalar2=None, op0=op0), R=R, W=W)
        return S.add(eng, lambda e: e.tensor_scalar(out=out, in0=in0, scalar1=s1, scalar2=s2, op0=op0, op1=op1), R=R, W=W)

    def stt(out, in0, scalar, in1, op0, op1, R, W):
        return S.add('dve', lambda e: e.scalar_tensor_tensor(out=out, in0=in0, scalar=scalar, in1=in1, op0=op0, op1=op1), R=R, W=W)

    def mm(out, lhsT, rhs, start, stop, R, W):
        return S.add('pe', lambda e: e.matmul(out, lhsT=lhsT, rhs=rhs, start=start, stop=stop), R=R, W=W)

    def cp(eng, out, in_, R, W):
        if eng == 'act':
            return S.add(eng, lambda e: e.activation(out=out, in_=in_, func=AF.Copy), R=R, W=W)
        return S.add(eng, lambda e: e.tensor_copy(out=out, in_=in_), R=R, W=W)

    def memset(eng, ap, val, W):
        return S.add(eng, lambda e: e.memset(ap, val), W=W)

    def red(out, in_, R, W):
        return S.add('dve', lambda e: e.tensor_reduce(out=out, in_=in_, axis=AX.X, op=ALU.add), R=R, W=W)

    def dump(name, ap, shape, dt=F32):
        if name not in dbg:
            return
        S.barrier()
        d = nc.dram_tensor("dbg_" + name, shape, dt, kind="ExternalOutput").ap()
        dbg_out[name] = d
        S.add('sp', lambda e: e.dma_start(out=d, in_=ap), grp=g_dbg)
        S.barrier()

    g_dbg = S.grp("dbg_sp")
    xs = T("xs", [128, NT, D], F32); xB = [Buf("x%d" % t) for t in range(NT)]
    ident = T("ident", [128, 128], F32); identB = Buf("ident")
    tri_b = T("tri_b", [128, 128], BF16); triB = Buf("tri")
    ones_b = T("ones_b", [128, 128], BF16); onesB = Buf("ones")
    upper_b = T("upper_b", [128, 128], BF16); upperB = Buf("upper")
    pos_tok = T("pos_tok", [128, NT], F32); posB = Buf("pos")
    invf = T("invf", [128, 16], F32); invfB = Buf("invf")
    modT = T("modT", [128, 32], F32); modTB = Buf("modT")
    ABT = T("ABT", [128, 16], F32); ABTB = Buf("ABT")
    g1g2 = T("g1g2", [128, 2, D], F32); g12B = Buf("g1g2")
    comb = T("comb", [128, NT, 8], F32); combB = Buf("comb")
    identb = T("identb", [128, 128], BF16); identbB = Buf("identb")
    ones_f = T("ones_f", [128, 128], F32); onesfB = Buf("ones_f")
    SC = T("SC", [128, NT, 32], F32); SCB = Buf("SC")
    PS = [es.enter_context(nc.psum_tensor("ps%d" % i, [128, 512], F32)) for i in range(8)]
    PB = [Buf("ps%d" % i) for i in range(8)]
    PI = list(zip(PS, PB))

    def tr(out, in_, R, W):
        return S.add('pe', lambda e: e.transpose(out, in_, ident[:in_.shape[0], :in_.shape[0]]), R=list(R) + [identB], W=W)

    def rsqrt(tag, out, in_, scale, R, W, st):
        n = in_.shape[1]
        v = T("rsq_" + tag, [128, n], F32, st); vB = Buf("rsq_" + tag)
        S.add('dve', lambda e: e.tensor_scalar(out=v[:], in0=in_, scalar1=scale, scalar2=EPS, op0=ALU.mult, op1=ALU.add), R=R, W=[vB])
        act(v[:], v[:], AF.Ln, [vB], [vB])
        act(out, v[:], AF.Exp, [vB], W, scale=-0.5)

    g_in = S.grp("in_sp")
    for t in range(NT):
        S.dma('sp', xs[:, t, :], x_d[t * 128:(t + 1) * 128, :], W=[xB[t]], grp=g_in)
    memset('pool', ident[:], 1.0, [identB])
    S.add('pool', lambda e: e.affine_select(out=ident[:], in_=ident[:], pattern=[[-1, 128]], compare_op=ALU.is_equal, fill=0.0, base=0, channel_multiplier=1), R=[identB], W=[identB])
    memset('pool', tri_b[:], 1.0, [triB])
    S.add('pool', lambda e: e.affine_select(out=tri_b[:], in_=tri_b[:], pattern=[[1, 128]], compare_op=ALU.is_ge, fill=0.0, base=0, channel_multiplier=-1), R=[triB], W=[triB])
    memset('pool', ones_b[:], 1.0, [onesB])
    memset('pool', ones_f[:], 1.0, [onesfB])
    memset('pool', upper_b[:], 1.0, [upperB])
    S.add('pool', lambda e: e.affine_select(out=upper_b[:], in_=upper_b[:], pattern=[[-1, 128]], compare_op=ALU.is_gt, fill=0.0, base=0, channel_multiplier=1), R=[upperB], W=[upperB])
    with ExitStack() as st:
        pi = T("pos_i", [128, NT], I32, st); tB = Buf("pos_i")
        S.dma('sp', pi[:], pos_d.rearrange("(t p) -> p t", p=128), W=[tB], grp=g_in, allow_slow_non_contiguous=True)
        cp('dve', pos_tok[:], pi[:], [tB], [posB])
        ii = T("iota_i", [128, 16], I32, st); iB = Buf("iota")
        S.add('pool', lambda e: e.iota(ii[:], pattern=[[1, 16]], base=0, channel_multiplier=0), W=[iB])
        cp('dve', invf[:], ii[:], [iB], [invfB])
        act(invf[:], invf[:], AF.Exp, [invfB], [invfB], scale=-math.log(10000.0) / 16.0)
        cp('dve', identb[:], ident[:], [identB], [identbB])
        TWO_PI_ = 2.0 * math.pi
        angA = T("angA", [128, NT, 16], F32, st); angB = Buf("angA")
        sct = T("sc_tmp", [128, NT * 32], F32, st); sctB = Buf("sc_tmp")
        sci = T("sc_ki", [128, NT * 32], I32, st); sciB = Buf("sc_ki")
        SC2 = SC[:].rearrange("p t s -> p (t s)")
        tt('dve', angA[:], invf[:].unsqueeze(1).broadcast_to([128, NT, 16]), pos_tok[:].unsqueeze(2).broadcast_to([128, NT, 16]), ALU.mult, [invfB, posB], [angB])
        ts('dve', SC[:, :, 0:16], angA[:], math.pi, ALU.add, [angB], [SCB])
        ts('dve', SC[:, :, 16:32], angA[:], 1.5 * math.pi, ALU.add, [angB], [SCB])
        ts('dve', sct[:], SC2, 1.0 / TWO_PI_, ALU.mult, [SCB], [sctB])
        cp('dve', sci[:], sct[:], [sctB], [sciB])
        cp('dve', sct[:], sci[:], [sciB], [sctB])
        stt(SC2, sct[:], -TWO_PI_, SC2, ALU.mult, ALU.add, [sctB, SCB], [SCB])
        ts('dve', sct[:], SC2, 0.0, ALU.is_lt, [SCB], [sctB])
        stt(SC2, sct[:], TWO_PI_, SC2, ALU.mult, ALU.add, [sctB, SCB], [SCB])
        ts('dve', SC2, SC2, -math.pi, ALU.add, [SCB], [SCB])
        ts('dve', SC2, SC2, -math.pi, ALU.max, [SCB], [SCB], s2=math.pi, op1=ALU.min)
        act(SC2, SC2, AF.Sin, [SCB], [SCB])
        S.barrier()

    def layer_body(l):
        lam_init = 0.8 - 0.6 * math.exp(-0.3 * l)
        w_in = Wd['w_in'][l]

        with ExitStack() as st:
            wa_r = Ring([(T("wa%d" % i, [128, 8, 512], F32, st), Buf("wa%d" % i)) for i in range(2)])
            bb_r = Ring([(T("bb%d" % i, [128, 512], F32, st), Buf("bb%d" % i)) for i in range(2)])
            mb_r = Ring([(T("mb%d" % i, [128, 512], F32, st), Buf("mb%d" % i)) for i in range(2)])
            gT = T("gT", [128, 16], F32, st); gTB = Buf("gT")
            condB_t = T("condB", [128, 8, 128], F32, st); condBB = Buf("condB")
            cT = T("cT", [128, 8], F32, st); cB = Buf("cT")
            S.dma('sp', cT[:], c_d.rearrange("(j p) -> p j", p=128), W=[cB], allow_slow_non_contiguous=True)
            act(cT[:], cT[:], AF.Silu, [cB], [cB])
            cp('dve', condB_t[:], cT[:].unsqueeze(2).broadcast_to([128, 8, 128]), [cB], [condBB])
            S.dma('sp', gT[:, 0:8], Wd['norm1_g'][l].rearrange("(j p) -> p j", p=128), W=[gTB], allow_slow_non_contiguous=True)
            S.dma('sp', gT[:, 8:16], Wd['norm2_g'][l].rearrange("(j p) -> p j", p=128), W=[gTB], allow_slow_non_contiguous=True)
            pr = Ring(PI[0:2]); pr2 = Ring(PI[2:4])
            for n in range(12):
                wa, waB = wa_r.next(); bb, bbB = bb_r.next(); mb, mbB = mb_r.next()
                S.dma('sp', wa[:, 0:4, :], Wd['w_ada'][l][0:512, n * 512:(n + 1) * 512].rearrange("(j p) n -> p j n", p=128), W=[waB])
                S.dma('pool', wa[:, 4:8, :], Wd['w_ada'][l][512:1024, n * 512:(n + 1) * 512].rearrange("(j p) n -> p j n", p=128), W=[waB])
                S.dma('sp', bb[:], Wd['b_ada'][l][n * 512:(n + 1) * 512].partition_broadcast(128), W=[bbB])
                ps, psB = pr.next()
                for j in range(8):
                    mm(ps[:, :], condB_t[:, j, :], wa[:, j, :], j == 0, j == 7, [condBB, waB], [psB])
                if n in (4, 5, 10, 11):
                    gi = 0 if n < 6 else 1
                    hf = n % 2
                    tt('dve', g1g2[:, gi, hf * 512:(hf + 1) * 512], ps[:, :], bb[:], ALU.add, [psB, bbB], [g12B])
                else:
                    tt('dve', mb[:], ps[:, :], bb[:], ALU.add, [psB, bbB], [mbB])
                    base = {0: 0, 1: 4, 2: 8, 3: 12, 6: 16, 7: 20, 8: 24, 9: 28}[n]
                    p2, p2B = pr2.next()
                    for q in range(4):
                        tr(p2[:, q * 128:(q + 1) * 128], mb[:, q * 128:(q + 1) * 128], [mbB], [p2B])
                    cp('act' if n % 2 else 'dve', modT[:, base:base + 4], p2[:, :].rearrange("p (q t) -> p q t", t=128)[:, :, 0], [p2B], [modTB])
            stt(ABT[:, 0:8], modT[:, 8:16], 1.0, gT[:, 0:8], ALU.add, ALU.mult, [modTB, gTB], [ABTB])
            stt(ABT[:, 8:16], modT[:, 24:32], 1.0, gT[:, 8:16], ALU.add, ALU.mult, [modTB, gTB], [ABTB])
            S.barrier()
        dump("modT%d" % l, modT[:], [128, 32])
        dump("ABT%d" % l, ABT[:], [128, 16])
        dump("g1g2_%d" % l, g1g2[:].rearrange("p a d -> p (a d)"), [128, 2 * D])
        if stop == 'A0':
            return False

        def norm_phase(which, hT, hB, st, router=None):
            aoff = 0 if which == 1 else 8
            boff = 0 if which == 1 else 16
            junk_r = Ring([(T("nj%d" % i, [128, D], BF16, st), Buf("nj%d" % i)) for i in range(2)])
            xn_r = Ring([(T("xn%d" % i, [128, D], F32, st), Buf("xn%d" % i)) for i in range(2)])
            ss_r = Ring([(T("nss%d" % i, [128, 2], F32, st), Buf("nss%d" % i)) for i in range(3)])
            pr = Ring([(PI[0], PI[1]), (PI[2], PI[3])])
            def n0(t):
                junk, jB = junk_r.next(); xn, xnB = xn_r.next(); ss, ssB = ss_r.next()
                act(junk[:], xs[:, t, :], AF.Square, [xB[t]], [jB, ssB], accum_out=ss[:, 0:1])
                ts('dve', ss[:, 1:2], ss[:, 0:1], 1.0 / D, ALU.mult, [ssB], [ssB], s2=EPS, op1=ALU.add)
                act(ss[:, 1:2], ss[:, 1:2], AF.Ln, [ssB], [ssB])
                act(ss[:, 1:2], ss[:, 1:2], AF.Exp, [ssB], [ssB], scale=-0.5)
                ts('dve', xn[:], xs[:, t, :], ss[:, 1:2], ALU.mult, [xB[t], ssB], [xnB])
                return xn, xnB

            def n1(t, xn, xnB):
                (pa, paB), (pb, pbB) = pr.next()
                for j in range(8):
                    p, pB_ = (pa, paB) if j < 4 else (pb, pbB)
                    tr(p[:, (j % 4) * 128:(j % 4 + 1) * 128], xn[:, j * 128:(j + 1) * 128], [xnB], [pB_])
                for j in range(8):
                    p, pB_ = (pa, paB) if j < 4 else (pb, pbB)
                    src = p[:, (j % 4) * 128:(j % 4 + 1) * 128]
                    dst = hT[:, j, t * 128:(t + 1) * 128]
                    if j % 2 == 0:
                        act(dst, src, AF.Identity, [pB_, ABTB, modTB], [hB[t]], scale=ABT[:, aoff + j:aoff + j + 1], bias=modT[:, boff + j:boff + j + 1])
                    else:
                        ts('dve', dst, src, ABT[:, aoff + j:aoff + j + 1], ALU.mult, [pB_, ABTB, modTB], [hB[t]], s2=modT[:, boff + j:boff + j + 1], op1=ALU.add)
                if router is not None:
                    router(t, pa, paB, pb, pbB, aoff, boff)

            pend = {}
            for k in range(NT + 1):
                if k < NT:
                    pend[k] = n0(k)
                if k >= 1:
                    n1(k - 1, *pend.pop(k - 1))

        def load_w(tile, buf, src2d, r0, nrow_chunks, c0, ncols):
            S.dma('pool', tile[:, 0:nrow_chunks, 0:ncols],
                  src2d[r0:r0 + 128 * nrow_chunks, c0:c0 + ncols].rearrange("(j p) n -> p j n", p=128), W=[buf])

        def proj_tok(ps, psB, hT, hB, W, WB, c0, ncols, t):
            for j in range(8):
                mm(ps[:, 0:ncols], hT[:, j, t * 128:(t + 1) * 128], W[:, j, c0:c0 + ncols], j == 0, j == 7, [hB[t], WB], [psB])

        def proj_T(ps, psB, hT, hB, W, WB, c0, ncols, c):
            R = [hB[t] for t in range(c * QPC, (c + 1) * QPC)] + [WB]
            for j in range(8):
                mm(ps[0:ncols, 0:CH], W[:, j, c0:c0 + ncols], hT[:, j, c * CH:(c + 1) * CH], j == 0, j == 7, R, [psB])

        stA = ExitStack()
        hT = T("hT", [128, 8, SEQ], BF16, stA); hB = [Buf("h%d" % t) for t in range(NT)]
        with ExitStack() as st:
            norm_phase(1, hT, hB, st)
            S.barrier()
        dump("hT%d" % l, hT[:].rearrange("p j t -> p (j t)"), [128, 8 * SEQ], BF16)
        if stop == 'A1':
            stA.close()
            return False

        with ExitStack() as st:
            w_r = Ring([(T("wsb%d" % i, [128, 8, 512], BF16, st), Buf("wsb%d" % i)) for i in range(2)])
            stg_r = Ring([(T("sbst%d" % i, [128, SEQ], BF16, st), Buf("sbst%d" % i)) for i in range(3)])
            stv_r = Ring([(T("sbsv%d" % i, [128, 512], BF16, st), Buf("sbsv%d" % i)) for i in range(3)])
            pr = Ring(PI)
            for part in range(3):
                W, WB = w_r.next()
                load_w(W, WB, w_in, 0, 8, part * 512, 512)
                if part < 2:
                    for m in range(4):
                        stg, stgB = stg_r.next()
                        for c in range(NCH):
                            ps, psB = pr.next()
                            proj_T(ps, psB, hT, hB, W, WB, m * 128, 128, c)
                            cp('act' if c % 2 else 'dve', stg[:, c * CH:(c + 1) * CH], ps[:, 0:CH], [psB], [stgB])
                        S.dma('sp', sbqk_d[part * 4 + m], stg[:], R=[stgB], W=[sbqk_B[part * 4 + m]])
                else:
                    for t in range(NT):
                        stv, stvB = stv_r.next()
                        ps, psB = pr.next()
                        proj_tok(ps, psB, hT, hB, W, WB, 0, 512, t)
                        cp('act' if t % 2 else 'dve', stv[:], ps[:, :], [psB], [stvB])
                        S.dma('sp', sbv_d[t * 128:(t + 1) * 128, :], stv[:], R=[stvB], W=[sbv_B])
            S.barrier()
        if stop == 'A2SB':
            stA.close()
            return False
        with ExitStack() as st:
            Wm = T("wmla", [128, 8, 1056], BF16, st); WmB = Buf("wmla")
            for i in range(3):
                c0 = i * 512
                ncol = min(512, 1056 - c0)
                S.dma('pool', Wm[:, :, c0:c0 + ncol], w_in[:, C_MLA + c0:C_MLA + c0 + ncol].rearrange("(j p) n -> p j n", p=128), W=[WmB])
            Wuq = T("wuq", [128, 6, 768], BF16, st); WuqB = Buf("wuq")
            load_w(Wuq, WuqB, Wd['w_mla_uq'][l], 0, 6, 0, 768)
            Wukv = T("wukv", [128, 2, 1024], BF16, st); WukvB = Buf("wukv")
            load_w(Wukv, WukvB, Wd['w_mla_ukv'][l], 0, 2, 0, 1024)
            nrm = T("mnrm", [128, 8], F32, st); nrmB = Buf("mnrm")
            S.dma('sp', nrm[:, 0:6], Wd['mla_q_norm'][l].rearrange("(j p) -> p j", p=128), W=[nrmB], allow_slow_non_contiguous=True)
            S.dma('sp', nrm[:, 6:8], Wd['mla_kv_norm'][l].rearrange("(j p) -> p j", p=128), W=[nrmB], allow_slow_non_contiguous=True)
            for j in range(6):
                ts('dve', Wuq[:, j, :], Wuq[:, j, :], nrm[:, j:j + 1], ALU.mult, [WuqB, nrmB], [WuqB])
            for j in range(2):
                ts('dve', Wukv[:, j, :], Wukv[:, j, :], nrm[:, 6 + j:7 + j], ALU.mult, [WukvB, nrmB], [WukvB])
            gq = T("mgq", [128, 3], F32, st); gqB = Buf("mgq")
            S.dma('sp', gq[0:96, 0:1], Wd['mla_q_gain'][l].rearrange("(p o) -> p o", o=1), W=[gqB])
            S.dma('sp', gq[0:96, 1:2], Wd['mla_k_gain'][l].rearrange("(p o) -> p o", o=1), W=[gqB])
            stt(gq[0:96, 2:3], gq[0:96, 0:1], 1.0 / math.sqrt(96.0), gq[0:96, 1:2], ALU.mult, ALU.mult, [gqB], [gqB])
            pr = Ring(PI)
            NR = 2
            junk_r = Ring([(T("mj%d" % i, [128, 512], BF16, st), Buf("mj%d" % i)) for i in range(NR)])
            ss_r = Ring([(T("mss%d" % i, [128, 8], F32, st), Buf("mss%d" % i)) for i in range(NR)])
            cf_r = Ring([(T("mcf%d" % i, [128, 1024], F32, st), Buf("mcf%d" % i)) for i in range(NR)])
            cT_r = Ring([(T("mcT%d" % i, [128, 8, 128], BF16, st), Buf("mcT%d" % i)) for i in range(NR)])
            qf_r = Ring([(T("mqf%d" % i, [128, 768], F32, st), Buf("mqf%d" % i)) for i in range(NR)])
            kf_r = Ring([(T("mkf%d" % i, [128, 768], F32, st), Buf("mkf%d" % i)) for i in range(NR)])
            vb_r = Ring([(T("mvb%d" % i, [128, 8, 66], BF16, st), Buf("mvb%d" % i)) for i in range(NR)])
            rp_r = Ring([(T("mrp%d" % i, [128, 8, 64], F32, st), Buf("mrp%d" % i)) for i in range(NR)])
            sm_r = Ring([(T("msm%d" % i, [128, 192], F32, st), Buf("msm%d" % i)) for i in range(NR)])
            sq_r = Ring([(T("msq%d" % i, [128, 768], F32, st), Buf("msq%d" % i)) for i in range(NR)])
            rq_r = Ring([(T("mrq%d" % i, [128, 32], F32, st), Buf("mrq%d" % i)) for i in range(NR)])
            stq_r = Ring([(T("mstq%d" % i, [128, 8, 128], BF16, st), Buf("mstq%d" % i)) for i in range(NR)])
            stk_r = Ring([(T("mstk%d" % i, [128, 8, 128], BF16, st), Buf("mstk%d" % i)) for i in range(NR)])
            for it in vb_r.items:
                memset('pool', it[0][:, :, 64:66], 1.0, [it[1]])
            prA = Ring(PI[0:3]); prB = Ring(PI[3:8])
            ST = {}

            def sA_pe(t):
                d = ST.setdefault(t, {})
                pa, paB = prA.next(); pb, pbB = prA.next(); pc, pcB = prA.next()
                d['pa'] = (pa, paB); d['pb'] = (pb, pbB); d['pc'] = (pc, pcB)
                proj_tok(pa, paB, hT, hB, Wm, WmB, 0, 512, t)
                proj_tok(pb, pbB, hT, hB, Wm, WmB, 512, 512, t)
                proj_tok(pc, pcB, hT, hB, Wm, WmB, 1024, 32, t)
                junk, jB = junk_r.next(); ss, ssB = ss_r.next()
                d['ss'] = (ss, ssB)
                act(junk[:, 0:512], pa[:, :], AF.Square, [paB], [jB, ssB], accum_out=ss[:, 0:1])
                act(junk[:, 0:256], pb[:, 0:256], AF.Square, [pbB], [jB, ssB], accum_out=ss[:, 1:2])
                act(junk[:, 0:256], pb[:, 256:512], AF.Square, [pbB], [jB, ssB], accum_out=ss[:, 2:3])

            def sA_dve(t):
                d = ST[t]
                pa, paB = d['pa']; pb, pbB = d['pb']; pc, pcB = d['pc']; ss, ssB = d['ss']
                cf, cfB = cf_r.next(); sm, smB = sm_r.next()
                d['cf'] = (cf, cfB); d['sm'] = (sm, smB)
                cp('dve', cf[:, 0:512], pa[:, :], [paB], [cfB])
                cp('dve', cf[:, 512:1024], pb[:, :], [pbB], [cfB])
                tt('dve', ss[:, 0:1], ss[:, 0:1], ss[:, 1:2], ALU.add, [ssB], [ssB])
                ts('dve', ss[:, 4:5], ss[:, 0:1], 1.0 / 768, ALU.mult, [ssB], [ssB], s2=EPS, op1=ALU.add)
                ts('dve', ss[:, 5:6], ss[:, 2:3], 1.0 / 256, ALU.mult, [ssB], [ssB], s2=EPS, op1=ALU.add)
                act(ss[:, 4:6], ss[:, 4:6], AF.Ln, [ssB], [ssB])
                act(ss[:, 4:6], ss[:, 4:6], AF.Exp, [ssB], [ssB], scale=-0.5)
                sinv = SC[:, t, 0:16]; cosv = SC[:, t, 16:32]
                k1 = pc[:, 0:16]; k2 = pc[:, 16:32]
                tt('dve', sm[:, 48:64], k1, cosv, ALU.mult, [pcB, SCB], [smB])
                tt('dve', sm[:, 64:80], k2, sinv, ALU.mult, [pcB, SCB], [smB])
                tt('dve', sm[:, 80:96], k1, sinv, ALU.mult, [pcB, SCB], [smB])
                tt('dve', sm[:, 96:112], k2, cosv, ALU.mult, [pcB, SCB], [smB])
                tt('dve', sm[:, 112:128], sm[:, 48:64], sm[:, 64:80], ALU.subtract, [smB], [smB])
                tt('dve', sm[:, 128:144], sm[:, 80:96], sm[:, 96:112], ALU.add, [smB], [smB])

            def sB(t):
                d = ST[t]
                cf, cfB = d['cf']; ss, ssB = d['ss']
                cT, cTB = cT_r.next()
                pt1, pt1B = prB.next(); pt2, pt2B = prB.next()
                for j in range(8):
                    p, pB_ = (pt1, pt1B) if j < 4 else (pt2, pt2B)
                    tr(p[:, (j % 4) * 128:(j % 4 + 1) * 128], cf[:, j * 128:(j + 1) * 128], [cfB], [pB_])
                cp('act', cT[:, 0:4, :].rearrange("p j t -> p (j t)"), pt1[:, :], [pt1B], [cTB])
                cp('dve', cT[:, 4:8, :].rearrange("p j t -> p (j t)"), pt2[:, :], [pt2B], [cTB])
                pq = [prB.next(), prB.next()]; pkv = [prB.next(), prB.next()]
                for half in range(2):
                    for j in range(6):
                        mm(pq[half][0][:, 0:384], cT[:, j, :], Wuq[:, j, half * 384:(half + 1) * 384], j == 0, j == 5, [cTB, WuqB], [pq[half][1]])
                for half in range(2):
                    for j in range(2):
                        mm(pkv[half][0][:, 0:512], cT[:, 6 + j, :], Wukv[:, j, half * 512:(half + 1) * 512], j == 0, j == 1, [cTB, WukvB], [pkv[half][1]])
                qf, qfB = qf_r.next(); kf, kfB = kf_r.next(); vb, vbB = vb_r.next()
                d['qf'] = (qf, qfB); d['kf'] = (kf, kfB); d['vb'] = (vb, vbB)
                k3 = kf[:].rearrange("p (h d) -> p h d", d=96)
                for half in range(2):
                    ts('dve', qf[:, half * 384:(half + 1) * 384], pq[half][0][:, 0:384], ss[:, 4:5], ALU.mult, [pq[half][1], ssB], [qfB])
                    src3 = pkv[half][0][:, :].rearrange("p (h e) -> p h e", e=128)
                    ts('dve', k3[:, half * 4:(half + 1) * 4, 0:64], src3[:, :, 0:64], ss[:, 5:6], ALU.mult, [pkv[half][1], ssB], [kfB])
                    ts('dve', vb[:, half * 4:(half + 1) * 4, 0:64], src3[:, :, 64:128], ss[:, 5:6], ALU.mult, [pkv[half][1], ssB], [vbB])

            def sC_dve(t):
                d = ST[t]
                qf, qfB = d['qf']; kf, kfB = d['kf']; sm, smB = d['sm']
                q3 = qf[:].rearrange("p (h d) -> p h d", d=96); k3 = kf[:].rearrange("p (h d) -> p h d", d=96)
                rp, rpB = rp_r.next()
                sinv = SC[:, t, 0:16]; cosv = SC[:, t, 16:32]
                sinB3 = sinv.unsqueeze(1).broadcast_to([128, 8, 16]); cosB3 = cosv.unsqueeze(1).broadcast_to([128, 8, 16])
                q1 = q3[:, :, 64:80]; q2 = q3[:, :, 80:96]
                tt('dve', rp[:, :, 0:16], q1, cosB3, ALU.mult, [qfB, SCB], [rpB])
                tt('dve', rp[:, :, 16:32], q2, sinB3, ALU.mult, [qfB, SCB], [rpB])
                tt('dve', rp[:, :, 32:48], q1, sinB3, ALU.mult, [qfB, SCB], [rpB])
                tt('dve', rp[:, :, 48:64], q2, cosB3, ALU.mult, [qfB, SCB], [rpB])
                tt('dve', q1, rp[:, :, 0:16], rp[:, :, 16:32], ALU.subtract, [rpB], [qfB])
                tt('dve', q2, rp[:, :, 32:48], rp[:, :, 48:64], ALU.add, [rpB], [qfB])
                cp('dve', k3[:, :, 64:96], sm[:, 112:144].unsqueeze(1).broadcast_to([128, 8, 32]), [smB], [kfB])
                sq, sqB = sq_r.next(); rq, rqB = rq_r.next()
                tt('dve', sq[:], qf[:], qf[:], ALU.mult, [qfB], [sqB])
                red(rq[:, 0:8], sq[:].rearrange("p (h d) -> p h d", d=96), [sqB], [rqB])
                tt('pool', sq[:], kf[:], kf[:], ALU.mult, [kfB, rqB], [sqB])
                red(rq[:, 8:16], sq[:].rearrange("p (h d) -> p h d", d=96), [sqB], [rqB])
                ts('dve', rq[:, 16:32], rq[:, 0:16], 1.0 / 96, ALU.mult, [rqB], [rqB], s2=EPS, op1=ALU.add)
                act(rq[:, 16:32], rq[:, 16:32], AF.Ln, [rqB], [rqB])
                act(rq[:, 16:32], rq[:, 16:32], AF.Exp, [rqB], [rqB], scale=-0.5)
                tt('dve', q3, q3, rq[:, 16:24].unsqueeze(2).broadcast_to([128, 8, 96]), ALU.mult, [qfB, rqB], [qfB])
                tt('dve', k3, k3, rq[:, 24:32].unsqueeze(2).broadcast_to([128, 8, 96]), ALU.mult, [kfB, rqB], [kfB])

            def sC_rest(t):
                d = ST.pop(t)
                qf, qfB = d['qf']; kf, kfB = d['kf']; vb, vbB = d['vb']
                q3 = qf[:].rearrange("p (h d) -> p h d", d=96); k3 = kf[:].rearrange("p (h d) -> p h d", d=96)
                stq, stqB = stq_r.next(); stk, stkB = stk_r.next()
                for (src3_, srcB, stg, stgB, isq) in ((q3, qfB, stq, stqB, True), (k3, kfB, stk, stkB, False)):
                    for half in range(2):
                        p, pB_ = prB.next()
                        for hh in range(4):
                            tr(p[0:96, hh * 128:(hh + 1) * 128], src3_[:, half * 4 + hh, :], [srcB], [pB_])
                        src = p[0:96, :]
                        dst = stg[0:96, half * 4:(half + 1) * 4, :].rearrange("p h t -> p (h t)")
                        if isq:
                            act(dst, src, AF.Identity, [pB_, gqB], [stgB], scale=gq[0:96, 2:3])
                        else:
                            cp('dve', dst, src, [pB_], [stgB])
                S.dma('sp', mq_d[:, :, t * 128:(t + 1) * 128].rearrange("h p t -> p h t"), stq[0:96, :, :], R=[stqB], W=[mq_B])
                S.dma('sp', mk_d[:, :, t * 128:(t + 1) * 128].rearrange("h p t -> p h t"), stk[0:96, :, :], R=[stkB], W=[mk_B])
                S.dma('sp', mv_d[t * 128:(t + 1) * 128, :], vb[:].rearrange("p h e -> p (h e)"), R=[vbB], W=[mv_B])

            sA_pe(0); sA_dve(0)
            for k in range(NT):
                sB(k)
                if k + 1 < NT:
                    sA_pe(k + 1)
                sC_dve(k)
                if k + 1 < NT:
                    sA_dve(k + 1)
                sC_rest(k)
            S.barrier()

        if stop == 'A2MLA':
            stA.close()
            return False
        with ExitStack() as st:
            W3 = [(T("wdf%d" % i, [128, 8, 512], BF16, st), Buf("wdf%d" % i)) for i in range(3)]
            for i, c0 in enumerate((C_DQ, C_DK, C_DV)):
                load_w(W3[i][0], W3[i][1], w_in, 0, 8, c0, 512)
            gd = T("dgd", [128, 3], F32, st); gdB = Buf("dgd")
            for half in range(2):
                S.dma('sp', gd[half * 64:(half + 1) * 64, 0:1], Wd['diff_q_gain'][l].rearrange("(p o) -> p o", o=1), W=[gdB])
                S.dma('sp', gd[half * 64:(half + 1) * 64, 1:2], Wd['diff_k_gain'][l].rearrange("(p o) -> p o", o=1), W=[gdB])
            stt(gd[:, 2:3], gd[:, 0:1], 1.0 / 8.0, gd[:, 1:2], ALU.mult, ALU.mult, [gdB], [gdB])
            pr = Ring(PI)
            NR = 2
            sq_r = Ring([(T("dsq%d" % i, [128, 512], F32, st), Buf("dsq%d" % i)) for i in range(NR)])
            rr_r = Ring([(T("drr%d" % i, [128, 32], F32, st), Buf("drr%d" % i)) for i in range(NR)])
            qn_r = Ring([(T("dqn%d" % i, [128, 512], F32, st), Buf("dqn%d" % i)) for i in range(NR)])
            kn_r = Ring([(T("dkn%d" % i, [128, 512], F32, st), Buf("dkn%d" % i)) for i in range(NR)])
            stq_r = Ring([(T("dstq%d" % i, [128, 4, 128], BF16, st), Buf("dstq%d" % i)) for i in range(NR)])
            stk_r = Ring([(T("dstk%d" % i, [128, 4, 128], BF16, st), Buf("dstk%d" % i)) for i in range(NR)])
            vb_r = Ring([(T("dvb%d" % i, [128, 4, 130], BF16, st), Buf("dvb%d" % i)) for i in range(NR)])
            for it in vb_r.items:
                memset('pool', it[0][:, :, 128:130], 1.0, [it[1]])
            prA = Ring(PI[0:3]); prB = Ring(PI[3:8])
            DS = {}

            def d0(t):
                pq, pqB = prA.next(); pk, pkB = prA.next(); pv, pvB = prA.next()
                proj_tok(pq, pqB, hT, hB, W3[0][0], W3[0][1], 0, 512, t)
                proj_tok(pk, pkB, hT, hB, W3[1][0], W3[1][1], 0, 512, t)
                proj_tok(pv, pvB, hT, hB, W3[2][0], W3[2][1], 0, 512, t)
                rr, rrB = rr_r.next()
                for (p, pB_, off) in ((pq, pqB, 0), (pk, pkB, 8)):
                    sq, sqB = sq_r.next()
                    act(sq[:], p[:, :], AF.Square, [pB_], [sqB])
                    red(rr[:, off:off + 8], sq[:].rearrange("p (g d) -> p g d", d=64), [sqB], [rrB])
                ts('dve', rr[:, 16:32], rr[:, 0:16], 1.0 / 64, ALU.mult, [rrB], [rrB], s2=EPS, op1=ALU.add)
                act(rr[:, 16:32], rr[:, 16:32], AF.Ln, [rrB], [rrB])
                act(rr[:, 16:32], rr[:, 16:32], AF.Exp, [rrB], [rrB], scale=-0.5)
                qn, qnB = qn_r.next(); kn, knB = kn_r.next(); vb, vbB = vb_r.next()
                tt('dve', qn[:].rearrange("p (g d) -> p g d", d=64), pq[:, :].rearrange("p (g d) -> p g d", d=64),
                   rr[:, 16:24].unsqueeze(2).broadcast_to([128, 8, 64]), ALU.mult, [pqB, rrB], [qnB])
                tt('dve', kn[:].rearrange("p (g d) -> p g d", d=64), pk[:, :].rearrange("p (g d) -> p g d", d=64),
                   rr[:, 24:32].unsqueeze(2).broadcast_to([128, 8, 64]), ALU.mult, [pkB, rrB], [knB])
                cp('dve', vb[:, :, 0:128], pv[:, :].rearrange("p (h e) -> p h e", e=128), [pvB], [vbB])
                DS[t] = (qn, qnB, kn, knB, vb, vbB)

            def d1(t):
                qn, qnB, kn, knB, vb, vbB = DS.pop(t)
                stq, stqB = stq_r.next(); stk, stkB = stk_r.next()
                p, pB_ = prB.next()
                for g in range(4):
                    tr(p[:, g * 128:(g + 1) * 128], qn[:, g * 128:(g + 1) * 128], [qnB], [pB_])
                act(stq[:].rearrange("p g t -> p (g t)"), p[:, :], AF.Identity, [pB_, gdB], [stqB], scale=gd[:, 2:3])
                p, pB_ = prB.next()
                for g in range(4):
                    tr(p[:, g * 128:(g + 1) * 128], kn[:, g * 128:(g + 1) * 128], [knB], [pB_])
                cp('dve', stk[:].rearrange("p g t -> p (g t)"), p[:, :], [pB_], [stkB])
                S.dma('sp', dq_d[:, :, t * 128:(t + 1) * 128].rearrange("g p t -> p g t"), stq[:], R=[stqB], W=[dq_B])
                S.dma('sp', dk_d[:, :, t * 128:(t + 1) * 128].rearrange("g p t -> p g t"), stk[:], R=[stkB], W=[dk_B])
                S.dma('sp', dv_d[t * 128:(t + 1) * 128, :], vb[:].rearrange("p h e -> p (h e)"), R=[vbB], W=[dv_B])

            d0(0)
            for k in range(NT):
                if k + 1 < NT:
                    d0(k + 1)
                d1(k)
            S.barrier()

        if stop == 'A2DIFF':
            stA.close()
            return False
        with ExitStack() as st:
            w_r = Ring([(T("wgt%d" % i, [128, 8, 512], BF16, st), Buf("wgt%d" % i)) for i in range(2)])
            stg_r = Ring([(T("gst%d" % i, [128, SEQ], BF16, st), Buf("gst%d" % i)) for i in range(3)])
            pr = Ring(PI)
            for part in range(6):
                W, WB = w_r.next()
                load_w(W, WB, w_in, 0, 8, C_GATE + part * 512, 512)
                for m in range(4):
                    stg, stgB = stg_r.next()
                    for c in range(NCH):
                        ps, psB = pr.next()
                        proj_T(ps, psB, hT, hB, W, WB, m * 128, 128, c)
                        act(stg[:, c * CH:(c + 1) * CH], ps[:, 0:CH], AF.Sigmoid, [psB], [stgB])
                    S.dma('sp', gt_d[part * 4 + m], stg[:], R=[stgB], W=[gt_B])
            S.barrier()

        if stop == 'A2G':
            stA.close()
            return False
        stA.close()
        stB = ExitStack()
        oT = [T("oT%d" % n, [128, 4, SEQ], BF16, stB) for n in range(3)]
        oTB = [[Buf("oT%d_%d" % (n, k)) for k in range(4)] for n in range(3)]

        def sb_attention():
            with ExitStack() as st:
                NB = 2
                msk_sb = T("msk_sb", [128, 4, 512], BF16, st); mskB = Buf("msk")
                memset('pool', msk_sb[:], 1.0, [mskB])
                for r in range(4):
                    def f(e, r=r):
                        return e.affine_select(out=msk_sb[:, r, :], in_=msk_sb[:, r, :], pattern=[[1, 512]], compare_op=ALU.is_gt, fill=0.0, base=-128 * r, channel_multiplier=-1)
                    S.add('pool', f, R=[mskB], W=[mskB])
                mneg = T("mneg_sb", [128, 4, 512], BF16, st); mnegB = Buf("mneg")
                ts('dve', mneg[:].rearrange("p r t -> p (r t)"), msk_sb[:].rearrange("p r t -> p (r t)"), 240.0, ALU.mult, [mskB], [mnegB], s2=-240.0, op1=ALU.add)
                qz_r = Ring([([T("sbq%d_%d" % (i, z), [128, SEQ], BF16, st) for z in range(2)], Buf("sbq%d" % i)) for i in range(NB)])
                k_r = Ring([(T("sbk%d" % i, [128, SEQ], BF16, st), Buf("sbk%d" % i)) for i in range(NB)])
                v_r = Ring([(T("sbvp%d" % i, [128, NT, 2, 128], BF16, st), Buf("sbvp%d" % i)) for i in range(NB)])
                e_r = Ring([(T("sbe%d" % i, [128, 512], F32, st), Buf("sbe%d" % i)) for i in range(2)])
                sp_r = Ring([(T("sbsp%d" % i, [128, 512], F32, st), Buf("sbsp%d" % i)) for i in range(4)])
                tm_r = Ring([(T("sbtm%d" % i, [128, 512], F32, st), Buf("sbtm%d" % i)) for i in range(2)])
                pb_r = Ring([(T("sbpb%d" % i, [128, 512], BF16, st), Buf("sbpb%d" % i)) for i in range(3)])
                wb_r = Ring([(T("sbwb%d" % i, [128, 512], BF16, st), Buf("sbwb%d" % i)) for i in range(3)])
                ra_r = Ring([(T("sbra%d" % i, [128, 512], BF16, st), Buf("sbra%d" % i)) for i in range(3)])
                pz_r = Ring(PI[0:3]); pc_r = Ring(PI[3:6]); po_r = Ring(PI[6:8])
                scale = 1.0 / 8.0
                heads = {}

                def load_hp(hp):
                    qz, qB = qz_r.next(); kt, kB = k_r.next(); vp, vB = v_r.next()
                    memset('pool', qz[0][64:128, :], 0.0, [qB]); memset('pool', qz[1][0:64, :], 0.0, [qB])
                    S.dma('sp', qz[0][0:64, :], sbqk_d[hp][0:64, :], R=[sbqk_B[hp]], W=[qB])
                    S.dma('sp', qz[1][64:128, :], sbqk_d[hp][64:128, :], R=[sbqk_B[hp]], W=[qB])
                    S.dma('sp', kt[:], sbqk_d[4 + hp], R=[sbqk_B[4 + hp]], W=[kB])
                    memset('pool', vp[:, :, 0, 64:128], 0.0, [vB]); memset('pool', vp[:, :, 1, 0:64], 0.0, [vB])
                    S.dma('sp', vp[:, :, 0, 0:64], sbv_d[:, hp * 128:hp * 128 + 64].rearrange("(t p) d -> p t d", p=128), R=[sbv_B], W=[vB])
                    S.dma('sp', vp[:, :, 1, 64:128], sbv_d[:, hp * 128 + 64:hp * 128 + 128].rearrange("(t p) d -> p t d", p=128), R=[sbv_B], W=[vB])
                    heads[hp] = (qz, qB, kt, kB, vp, vB)

                items = []
                for hp in range(4):
                    for z in range(2):
                        for c in range(NCH):
                            nblk = (c + 1) * QPC
                            grp_ = {}
                            for bi, sb in enumerate(range(nblk - 1, -1, -1)):
                                items.append(dict(hp=hp, z=z, c=c, sb=sb, bi=bi, nblk=nblk, g=grp_, idx=len(items)))
                first_idx = {}
                for it in items:
                    first_idx.setdefault(it['hp'], it['idx'])

                def s0(x):
                    if x['idx'] == 0:
                        load_hp(0)
                    if x['idx'] == first_idx[x['hp']] + 3 and x['hp'] + 1 < 4:
                        load_hp(x['hp'] + 1)
                    qz, qB, kt, kB, vp, vB = heads[x['hp']]
                    c, sb, z = x['c'], x['sb'], x['z']
                    x['pz'], x['pzB'] = pz_r.next()
                    e_, eB = e_r.next()
                    x['sp'], x['spB'] = sp_r.next()
                    diag = sb >= c * QPC
                    mm(x['pz'][:, 0:CH], kt[:, sb * 128:(sb + 1) * 128], qz[z][:, c * CH:(c + 1) * CH], True, not diag, [kB, qB], [x['pzB']])
                    if diag:
                        wd_ = 128 * (sb - c * QPC + 1)
                        mm(x['pz'][:, 0:wd_], identb[:], mneg[:, sb - c * QPC, 0:wd_], False, True, [identbB, mnegB], [x['pzB']])
                    act(e_[:, 0:CH], x['pz'][:, 0:CH], AF.Exp, [x['pzB']], [eB], scale=-scale)
                    act(x['sp'][:, 0:CH], e_[:, 0:CH], AF.Ln, [eB], [x['spB']], bias=1.0)

                def s1(x):
                    c, sb, bi, g = x['c'], x['sb'], x['bi'], x['g']
                    diag = sb >= c * QPC
                    r = sb - c * QPC
                    x['pb'], x['pbB'] = pb_r.next()
                    x['pc'], x['pcB'] = pc_r.next()
                    pb, pbB, pc, pcB = x['pb'], x['pbB'], x['pc'], x['pcB']
                    stt(pb[:, 0:CH], x['pz'][:, 0:CH], scale, x['sp'][:, 0:CH], ALU.mult, ALU.add, [x['pzB'], x['spB']], [pbB])
                    if diag:
                        wd_ = 128 * (r + 1)
                        tt('dve', pb[:, 0:wd_], pb[:, 0:wd_], msk_sb[:, r, 0:wd_], ALU.mult, [pbB, mskB], [pbB])
                    mm(pc[:, 0:CH], upper_b[:], pb[:, 0:CH], True, bi == 0, [upperB, pbB], [pcB])
                    if bi > 0:
                        ra, raB = g['ra']
                        mm(pc[:, 0:CH], ones_b[:], ra[:, 0:CH], False, True, [onesB, raB], [pcB])
                    if bi < x['nblk'] - 1:
                        ran, ranB = ra_r.next()
                        if bi == 0:
                            cp('pool', ran[:, 0:CH], pb[:, 0:CH], [pbB], [ranB])
                        else:
                            ra, raB = g['ra']
                            tt('pool', ran[:, 0:CH], ra[:, 0:CH], pb[:, 0:CH], ALU.add, [raB, pbB], [ranB])
                        g['ra'] = (ran, ranB)

                def s2(x):
                    c, sb = x['c'], x['sb']
                    diag = sb >= c * QPC
                    r = sb - c * QPC
                    tm, tmB = tm_r.next()
                    x['wb'], x['wbB'] = wb_r.next()
                    tt('dve', tm[:, 0:CH], x['pc'][:, 0:CH], x['sp'][:, 0:CH], ALU.add, [x['pcB'], x['spB']], [tmB])
                    act(x['wb'][:, 0:CH], tm[:, 0:CH], AF.Exp, [tmB], [x['wbB']], scale=-1.0)

                def s3(x):
                    qz, qB, kt, kB, vp, vB = heads[x['hp']]
                    c, sb, z, bi, g, hp = x['c'], x['sb'], x['z'], x['bi'], x['g'], x['hp']
                    if bi == 0:
                        g['po'] = po_r.next()
                    po, poB = g['po']
                    mm(po[:, 0:CH], vp[:, sb, z, :], x['wb'][:, 0:CH], bi == 0, bi == x['nblk'] - 1, [vB, x['wbB']], [poB])
                    if bi == x['nblk'] - 1:
                        po_ = 64 * z
                        cp('act', oT[0][po_:po_ + 64, hp, c * CH:(c + 1) * CH], po[po_:po_ + 64, 0:CH], [poB], [oTB[0][hp]])

                stages = [s0, s1, s2, s3]
                for k in range(len(items) + len(stages) - 1):
                    for j, stg_f in enumerate(stages):
                        ii = k - j
                        if 0 <= ii < len(items):
                            stg_f(items[ii])
                S.barrier()
        sb_attention()
        dump("o_sbT%d" % l, oT[0][:].rearrange("p j t -> p (j t)"), [128, 4 * SEQ], BF16)
        if stop == 'BSB':
            stB.close()
            return False

        with ExitStack() as st:
            NB = 2
            q_r = Ring([(T("mq%d" % i, [128, SEQ], BF16, st), Buf("mq%d" % i)) for i in range(NB)])
            k_r = Ring([(T("mk%d" % i, [128, SEQ], BF16, st), Buf("mk%d" % i)) for i in range(NB)])
            v_r = [Ring([(T("mv%d_%d" % (z, i), [128, NT, 128], BF16, st), Buf("mv%d_%d" % (z, i))) for i in range(1)]) for z in range(2)]
            for z in range(2):
                for it in v_r[z].items:
                    memset('pool', it[0][:].rearrange("p t e -> p (t e)"), 0.0, [it[1]])
                    if z == 0:
                        memset('pool', it[0][:, :, 64:65], 1.0, [it[1]])
                    else:
                        memset('pool', it[0][:, :, 0:1], 1.0, [it[1]])
            E_r = Ring([(T("mE%d" % i, [128, 512], BF16, st), Buf("mE%d" % i)) for i in range(4)])
            rd_r = Ring([(T("mrd%d" % i, [128, 512], F32, st), Buf("mrd%d" % i)) for i in range(2)])
            pz_r = Ring([PI[0], PI[1], PI[6]]); pa_r = Ring([PI[2], PI[3], PI[4]]); pb_r = Ring([PI[5], PI[7]])
            heads = {}

            def load_h(h):
                z = h % 2
                qT, qB = q_r.next(); kT, kB = k_r.next(); vh, vB = v_r[z].next()
                S.dma('sp', qT[0:96, :], mq_d[h], R=[mq_B], W=[qB])
                S.dma('sp', kT[0:96, :], mk_d[h], R=[mk_B], W=[kB])
                c0 = 0 if z == 0 else 64
                S.dma('sp', vh[:, :, c0:c0 + 64], mv_d[:, h * 66:h * 66 + 64].rearrange("(t p) e -> p t e", p=128), R=[mv_B], W=[vB])
                heads[h] = (qT, qB, kT, kB, vh, vB)

            items = []
            for h in range(8):
                for c in range(NCH):
                    n_sb = (c + 1) * QPC
                    g_ = {}
                    for sb in range(n_sb):
                        items.append(dict(h=h, c=c, sb=sb, last=(sb == n_sb - 1), g=g_, idx=len(items)))
            first_idx = {}
            for it in items:
                first_idx.setdefault(it['h'], it['idx'])

            def s0(x):
                h, c, sb = x['h'], x['c'], x['sb']
                if x['idx'] == 0:
                    load_h(0)
                if x['idx'] == first_idx[h] + 2 and h + 1 < 8:
                    load_h(h + 1)
                qT, qB, kT, kB, vh, vB = heads[h]
                q0 = max(0, sb - c * QPC); f0 = q0 * 128
                x['pz'], x['pzB'] = pz_r.next()
                mm(x['pz'][:, f0:CH], kT[0:96, sb * 128:(sb + 1) * 128], qT[0:96, c * CH + f0:(c + 1) * CH], True, True, [kB, qB], [x['pzB']])

            def s1(x):
                c, sb = x['c'], x['sb']
                q0 = max(0, sb - c * QPC); f0 = q0 * 128
                x['E'], x['EB'] = E_r.next()
                E, EB = x['E'], x['EB']
                act(E[:, f0:CH], x['pz'][:, f0:CH], AF.Exp, [x['pzB']], [EB])
                if sb >= c * QPC:
                    tt('pool', E[:, f0:f0 + 128], E[:, f0:f0 + 128], tri_b[:], ALU.mult, [EB, triB], [EB])

            def s2(x):
                h, c, sb, g = x['h'], x['c'], x['sb'], x['g']
                hp, z = h // 2, h % 2
                qT, qB, kT, kB, vh, vB = heads[h]
                q0 = max(0, sb - c * QPC); f0 = q0 * 128
                E, EB = x['E'], x['EB']
                if sb == 0:
                    g['acc'] = pa_r.next()
                acc, accB = g['acc']
                M_ = 65 if z == 0 else 128
                mm(acc[0:M_, f0:CH], vh[:, sb, 0:M_], E[:, f0:CH], sb == 0, x['last'], [EB, vB], [accB])
                if not x['last']:
                    return
                rd, rdB = rd_r.next(); bc, bcB = pb_r.next()
                dp = 64 if z == 0 else 0
                o0 = 0 if z == 0 else 64
                S.add('dve', (lambda o_, i_: (lambda e: e.reciprocal(out=o_, in_=i_)))(rd[dp:dp + 1, 0:CH], acc[dp:dp + 1, 0:CH]), R=[accB], W=[rdB])

                def p1():
                    mm(bc[0:128, 0:CH], ones_f[dp:dp + 1, 0:128], rd[dp:dp + 1, 0:CH], True, True, [onesfB, rdB], [bcB])

                def p2():
                    cp('act', rd[o0:o0 + 64, 0:CH], bc[o0:o0 + 64, 0:CH], [bcB], [rdB])
                    tt('dve', oT[1][o0:o0 + 64, hp, c * CH:(c + 1) * CH], acc[o0:o0 + 64, 0:CH], rd[o0:o0 + 64, 0:CH], ALU.mult, [accB, rdB], [oTB[1][hp]])
                deferred.append((cur_step[0] + 1, p1)); deferred.append((cur_step[0] + 2, p2))

            deferred = []
            cur_step = [0]
            stages = [s0, s1, s2]
            for k in range(len(items) + len(stages) - 1):
                cur_step[0] = k
                for j, stg_f in enumerate(stages):
                    ii = k - j
                    if 0 <= ii < len(items):
                        stg_f(items[ii])
                due = [d_ for d_ in deferred if d_[0] <= k]
                deferred[:] = [d_ for d_ in deferred if d_[0] > k]
                for _, fn_ in due:
                    fn_()
            for _, fn_ in deferred:
                fn_()
            S.barrier()
        dump("o_mlaT%d" % l, oT[1][:].rearrange("p j t -> p (j t)"), [128, 4 * SEQ], BF16)
        if stop == 'BMLA':
            stB.close()
            return False

        with ExitStack() as st:
            posrow = T("posrow", [128, SEQ], F32, st); prwB = Buf("posrow")
            with ExitStack() as st2:
                pri = T("posrow_i", [128, SEQ], I32, st2); priB = Buf("posrow_i")
                S.dma('sp', pri[:], pos_d.partition_broadcast(128), W=[priB])
                cp('dve', posrow[:], pri[:], [priB], [prwB])
                S.barrier()
            negpos = T("negpos", [128, NT], F32, st)
            ts('dve', negpos[:], pos_tok[:], -1.0, ALU.mult, [posB], [prwB])
            lp = T("dlp", [128, 256], F32, st); lpB = Buf("dlp")
            lam = T("dlam", [128, 4], F32, st); lamB = Buf("dlam")
            prd = T("dprd", [128, 128], F32, st); prdB = Buf("dprd")
            S.dma('sp', lp[:], Wd['diff_lambda'][l].rearrange("a d -> (a d)").partition_broadcast(128), W=[lpB])
            tt('dve', prd[:, 0:64], lp[:, 0:64], lp[:, 64:128], ALU.mult, [lpB], [prdB])
            tt('dve', prd[:, 64:128], lp[:, 128:192], lp[:, 192:256], ALU.mult, [lpB], [prdB])
            red(lam[:, 0:2], prd[:].rearrange("p (a d) -> p a d", d=64), [prdB], [lamB])
            act(lam[:, 0:2], lam[:, 0:2], AF.Exp, [lamB], [lamB])
            tt('dve', lam[:, 2:3], lam[:, 1:2], lam[:, 0:1], ALU.subtract, [lamB], [lamB])
            ts('dve', lam[:, 3:4], lam[:, 2:3], -lam_init, ALU.add, [lamB], [lamB])
            sg = T("dsg", [128, 2], F32, st); sgB = Buf("dsg")
            S.dma('sp', sg[:, 0:1], Wd['diff_subln'][l].rearrange("(p o) -> p o", o=1), W=[sgB])
            ts('dve', sg[:, 1:2], sg[:, 0:1], 1.0 - lam_init, ALU.mult, [sgB], [sgB])
            NB = 2
            qz_r = Ring([([T("dq%d_%d" % (i, z), [128, SEQ], BF16, st) for z in range(2)], Buf("dq%d" % i)) for i in range(NB)])
            k_r = Ring([(T("dk%d" % i, [128, SEQ], BF16, st), Buf("dk%d" % i)) for i in range(NB)])
            v_r = Ring([(T("dv%d" % i, [128, NT, 130], BF16, st), Buf("dv%d" % i)) for i in range(NB)])
            E_r = Ring([(T("dE%d" % i, [128, 512], BF16, st), Buf("dE%d" % i)) for i in range(4)])
            dt_r = Ring([(T("ddt%d" % i, [128, 512], F32, st), Buf("ddt%d" % i)) for i in range(3)])
            zz_r = Ring([(T("dzz%d" % i, [128, 512], F32, st), Buf("dzz%d" % i)) for i in range(3)])
            o1_r = Ring([(T("do1%d" % i, [128, QPC, 128], F32, st), Buf("do1%d" % i)) for i in range(2)])
            od_r = Ring([(T("dod%d" % i, [128, 128], F32, st), Buf("dod%d" % i)) for i in range(6)])
            on_r = Ring([(T("don%d" % i, [128, 128], F32, st), Buf("don%d" % i)) for i in range(3)])
            jk_r = Ring([(T("djk%d" % i, [128, 128], BF16, st), Buf("djk%d" % i)) for i in range(2)])
            rc_r = Ring([(T("drc%d" % i, [128, 4], F32, st), Buf("drc%d" % i)) for i in range(8)])
            pz_r = Ring([PI[0], PI[1], PI[6]]); pt_r = Ring(PI[7:8])
            heads = {}

            def load_h(h):
                qz, qB = qz_r.next(); kp, kB = k_r.next(); vh, vB = v_r.next()
                memset('pool', qz[0][64:128, :], 0.0, [qB]); memset('pool', qz[1][0:64, :], 0.0, [qB])
                S.dma('sp', qz[0][0:64, :], dq_d[h][0:64, :], R=[dq_B], W=[qB])
                S.dma('sp', qz[1][64:128, :], dq_d[h][64:128, :], R=[dq_B], W=[qB])
                S.dma('sp', kp[:], dk_d[h], R=[dk_B], W=[kB])
                S.dma('sp', vh[:], dv_d[:, h * 130:(h + 1) * 130].rearrange("(t p) e -> p t e", p=128), R=[dv_B], W=[vB])
                heads[h] = (qz, qB, kp, kB, vh, vB)

            CD = 256; QD = 2; NCD = SEQ // CD
            items = []
            for h in range(4):
                for c in range(NCD):
                    n_sb = (c + 1) * QD
                    for sb in range(n_sb):
                        items.append(dict(h=h, c=c, sb=sb, last=(sb == n_sb - 1), idx=len(items)))
            first_idx = {}
            for it in items:
                first_idx.setdefault(it['h'], it['idx'])

            def s0(x):
                h, c, sb = x['h'], x['c'], x['sb']
                if x['idx'] == 0:
                    load_h(0)
                if x['idx'] == first_idx[h] + 2 and h + 1 < 4:
                    load_h(h + 1)
                qz, qB, kp, kB, vh, vB = heads[h]
                q0 = max(0, sb - c * QD); f0 = q0 * 128
                x['pz'], x['pzB'] = pz_r.next()
                x['dt'], x['dtB'] = dt_r.next()
                for m in range(2):
                    mm(x['pz'][:, m * CD + f0:(m + 1) * CD], kp[:, sb * 128:(sb + 1) * 128], qz[m][:, c * CD + f0:(c + 1) * CD], True, True, [kB, qB], [x['pzB']])
                act(x['dt'][:, f0:CD], posrow[:, c * CD + f0:(c + 1) * CD], AF.Abs, [prwB], [x['dtB']], bias=negpos[:, sb:sb + 1])

            def s1(x):
                h, c, sb = x['h'], x['c'], x['sb']
                slope = 2.0 ** (-8.0 * (h + 1) / 4.0)
                q0 = max(0, sb - c * QD); f0 = q0 * 128
                zz, zzB = zz_r.next()
                x['E'], x['EB'] = E_r.next()
                E, EB = x['E'], x['EB']
                for m in range(2):
                    stt(zz[:, m * CD + f0:(m + 1) * CD], x['dt'][:, f0:CD], -slope, x['pz'][:, m * CD + f0:(m + 1) * CD], ALU.mult, ALU.add, [x['dtB'], x['pzB']], [zzB])
                if f0 == 0:
                    act(E[:, 0:2 * CD], zz[:, 0:2 * CD], AF.Exp, [zzB], [EB])
                else:
                    for m in range(2):
                        act(E[:, m * CD + f0:(m + 1) * CD], zz[:, m * CD + f0:(m + 1) * CD], AF.Exp, [zzB], [EB])
                if sb >= c * QD:
                    for m in range(2):
                        tt('pool', E[:, m * CD + f0:m * CD + f0 + 128], E[:, m * CD + f0:m * CD + f0 + 128], tri_b[:], ALU.mult, [EB, triB], [EB])

            def s2(x):
                h, c, sb = x['h'], x['c'], x['sb']
                qz, qB, kp, kB, vh, vB = heads[h]
                q0 = max(0, sb - c * QD)
                E, EB = x['E'], x['EB']
                for q in range(q0, QD):
                    qb = c * QD + q
                    for m in range(2):
                        bi_ = 2 + q * 2 + m
                        mm(PS[bi_][:, 0:130], E[:, m * CD + q * 128:m * CD + (q + 1) * 128], vh[:, sb, :], sb == 0, sb == qb, [EB, vB], [PB[bi_]])
                if not x['last']:
                    return
                o1, o1B = o1_r.next()
                fin = []
                for q in range(QD):
                    qb = c * QD + q
                    b0 = 2 + q * 2; b1 = b0 + 1
                    rc, rcB = rc_r.next()
                    S.add('dve', (lambda o_, i_: (lambda e: e.reciprocal(out=o_, in_=i_)))(rc[:, 0:1], PS[b0][:, 128:129]), R=[PB[b0]], W=[rcB])
                    S.add('dve', (lambda o_, i_: (lambda e: e.reciprocal(out=o_, in_=i_)))(rc[:, 1:2], PS[b1][:, 128:129]), R=[PB[b1]], W=[rcB])
                    ts('dve', o1[:, q, :], PS[b0][:, 0:128], rc[:, 0:1], ALU.mult, [PB[b0], rcB], [o1B])
                    od, odB = od_r.next(); jk, jkB = jk_r.next()
                    tt('dve', rc[:, 1:2], rc[:, 1:2], lam[:, 3:4], ALU.mult, [rcB, lamB], [rcB])
                    stt(od[:], PS[b1][:, 0:128], rc[:, 1:2], o1[:, q, :], ALU.mult, ALU.add, [PB[b1], rcB, o1B], [odB])
                    act(jk[:], od[:], AF.Square, [odB], [jkB, rcB], accum_out=rc[:, 2:3])
                    fin.append((qb, rc, rcB, od, odB))

                def p1(fin=fin):
                    for (qb, rc, rcB, od, odB) in fin:
                        ts('dve', rc[:, 3:4], rc[:, 2:3], 1.0 / 128, ALU.mult, [rcB], [rcB], s2=EPS, op1=ALU.add)
                        act(rc[:, 3:4], rc[:, 3:4], AF.Ln, [rcB], [rcB])
                        act(rc[:, 3:4], rc[:, 3:4], AF.Exp, [rcB], [rcB], scale=-0.5)

                def p2(fin=fin, h=h):
                    for (qb, rc, rcB, od, odB) in fin:
                        on, onB = on_r.next()
                        ts('dve', on[:], od[:], rc[:, 3:4], ALU.mult, [odB, rcB], [onB])
                        pt, ptB = pt_r.next()
                        tr(pt[:, 0:128], on[:], [onB], [ptB])
                        act(oT[2][:, h, qb * 128:(qb + 1) * 128], pt[:, 0:128], AF.Identity, [ptB, sgB], [oTB[2][h]], scale=sg[:, 1:2])
                deferred.append((cur_step[0] + 1, p1)); deferred.append((cur_step[0] + 2, p2))

            deferred = []
            cur_step = [0]
            stages = [s0, s1, s2]
            for k in range(len(items) + len(stages) - 1):
                cur_step[0] = k
                for j, stg_f in enumerate(stages):
                    ii = k - j
                    if 0 <= ii < len(items):
                        stg_f(items[ii])
                due = [d_ for d_ in deferred if d_[0] <= k]
                deferred[:] = [d_ for d_ in deferred if d_[0] > k]
                for _, fn_ in due:
                    fn_()
            for _, fn_ in deferred:
                fn_()
            S.barrier()
        dump("o_diffT%d" % l, oT[2][:].rearrange("p j t -> p (j t)"), [128, 4 * SEQ], BF16)
        if stop == 'BDIFF':
            stB.close()
            return False

        with ExitStack() as st:
            wbr = T("wbr", [128, 12, D], BF16, st); wbrB = Buf("wbr")
            wbsrc = Wd['w_branch'][l].rearrange("n k d -> (n k) d")
            for i in range(3):
                S.dma('pool', wbr[:, i * 4:(i + 1) * 4, :], wbsrc[i * 512:(i + 1) * 512, :].rearrange("(j p) n -> p j n", p=128), W=[wbrB])
            wo = T("wo", [128, 8, D], BF16, st); woB = Buf("wo")
            for i in range(2):
                S.dma('pool', wo[:, i * 4:(i + 1) * 4, :], Wd['w_out'][l][i * 512:(i + 1) * 512, :].rearrange("(j p) n -> p j n", p=128), W=[woB])
            for j in range(8):
                tt('pool' if j % 2 else 'dve', wo[:, j, :], wo[:, j, :], g1g2[:, 0, :], ALU.mult, [woB, g12B], [woB])
            gts_r = Ring([(T("gts%d" % i, [128, 3, CH], BF16, st), Buf("gts%d" % i)) for i in range(3)])
            M_r = Ring([(T("mM%d" % i, [128, 8, CH], BF16, st), Buf("mM%d" % i)) for i in range(2)])
            tn_r = Ring([(T("mtn%d" % i, [128, 512], F32, st), Buf("mtn%d" % i)) for i in range(6)])
            gt_v = gt_d.rearrange("(n j) p t -> j p n t", n=3)
            pr = Ring(PI)
            for c in range(NCH):
                M, MB = M_r.next()
                for j in range(8):
                    gts, gtsB = gts_r.next()
                    S.dma('sp', gts[:], gt_v[j][:, :, c * CH:(c + 1) * CH], R=[gt_B], W=[gtsB])
                    tn = []
                    for n in range(3):
                        py, pyB = pr.next()
                        for kc in range(4):
                            mm(py[:, 0:CH], wbr[:, n * 4 + kc, j * 128:(j + 1) * 128], oT[n][:, kc, c * CH:(c + 1) * CH], kc == 0, kc == 3, [wbrB, oTB[n][kc]], [pyB])
                        t_, tB_ = tn_r.next()
                        tt('dve', t_[:, 0:CH], py[:, 0:CH], gts[:, n, :], ALU.mult, [pyB, gtsB], [tB_])
                        tn.append((t_, tB_))
                    tt('dve', tn[0][0][:, 0:CH], tn[0][0][:, 0:CH], tn[1][0][:, 0:CH], ALU.add, [tn[0][1], tn[1][1]], [tn[0][1]])
                    tt('pool', M[:, j, :], tn[0][0][:, 0:CH], tn[2][0][:, 0:CH], ALU.add, [tn[0][1], tn[2][1]], [MB])
                for q in range(QPC):
                    t = c * QPC + q
                    for half in range(2):
                        px, pxB = pr.next()
                        for j in range(8):
                            mm(px[:, :], M[:, j, q * 128:(q + 1) * 128], wo[:, j, half * 512:(half + 1) * 512], j == 0, j == 7, [MB, woB], [pxB])
                        tt('dve', xs[:, t, half * 512:(half + 1) * 512], px[:, :], xs[:, t, half * 512:(half + 1) * 512], ALU.add, [pxB, xB[t]], [xB[t]])
            S.barrier()
        stB.close()
        dump("xmid%d" % l, xs[:].rearrange("p t d -> p (t d)"), [128, NT * D])
        if stop == 'C':
            return False
        stA = ExitStack()
        hT = T("hT2", [128, 8, SEQ], BF16, stA)

        moe = (l % 2 == 1)
        jl = l // 2
        with ExitStack() as st:
            router = None
            if moe:
                wr = T("wr", [128, 8, 8], F32, st); wrB = Buf("wr")
                wr2 = T("wr2", [128, 8, 8], F32, st); wr2B = Buf("wr2")
                bbc = T("bbc", [128, 8, 128], F32, st); bbcB = Buf("bbc")
                brB_t = T("brB", [128, 8], F32, st); brBB = Buf("brB")
                S.dma('sp', wr[:], Wd['w_router'][jl].rearrange("(j p) e -> p j e", p=128), W=[wrB])
                tt('dve', wr2[:], wr[:], ABT[:, 8:16].unsqueeze(2).broadcast_to([128, 8, 8]), ALU.mult, [wrB, ABTB], [wr2B])
                cp('dve', bbc[:], modT[:, 16:24].unsqueeze(2).broadcast_to([128, 8, 128]), [modTB], [bbcB])
                pl, plB = PI[4]
                for j in range(8):
                    mm(pl[:, 0:8], bbc[:, j, :], wr[:, j, :], j == 0, j == 7, [bbcB, wrB], [plB])
                cp('dve', brB_t[:], pl[:, 0:8], [plB], [brBB])
                xt_r = Ring([(T("rxt%d" % i, [128, 8, 128], F32, st), Buf("rxt%d" % i)) for i in range(2)])
                rt_r = Ring([(T("rrt%d" % i, [128, 48], F32, st), Buf("rrt%d" % i)) for i in range(2)])
                prr = Ring(PI[4:6])

                def router(t, pa, paB, pb, pbB, aoff, boff):
                    xt, xtB = xt_r.next(); rt, rtB = rt_r.next()
                    cp('dve', xt[:, 0:4, :].rearrange("p j t -> p (j t)"), pa[:, :], [paB], [xtB])
                    cp('act', xt[:, 4:8, :].rearrange("p j t -> p (j t)"), pb[:, :], [pbB], [xtB])
                    pl_, plB_ = prr.next()
                    for j in range(8):
                        mm(pl_[:, 0:8], xt[:, j, :], wr2[:, j, :], j == 0, j == 7, [xtB, wr2B], [plB_])
                    lg = rt[:, 0:8]; mx = rt[:, 8:16]; mk = rt[:, 16:24]; ex = rt[:, 24:32]
                    tt('dve', lg, pl_[:, 0:8], brB_t[:], ALU.add, [plB_, brBB], [rtB])
                    S.add('dve', (lambda o_, i_: (lambda e: e.max(out=o_, in_=i_)))(mx, lg), R=[rtB], W=[rtB])
                    ts('dve', mk, lg, rt[:, 9:10], ALU.is_ge, [rtB], [rtB])
                    ts('dve', rt[:, 32:33], rt[:, 8:9], -1.0, ALU.mult, [rtB], [rtB])
                    act(ex, lg, AF.Exp, [rtB], [rtB], bias=rt[:, 32:33])
                    tt('dve', ex, ex, mk, ALU.mult, [rtB], [rtB])
                    red(rt[:, 33:34], ex, [rtB], [rtB])
                    S.add('dve', (lambda o_, i_: (lambda e: e.reciprocal(out=o_, in_=i_)))(rt[:, 34:35], rt[:, 33:34]), R=[rtB], W=[rtB])
                    ts('dve', comb[:, t, :], ex, rt[:, 34:35], ALU.mult, [rtB], [combB])
            norm_phase(2, hT, hB, st, router)
            S.barrier()
        dump("h2T%d" % l, hT[:].rearrange("p j t -> p (j t)"), [128, 8 * SEQ], BF16)
        dump("comb%d" % l, comb[:].rearrange("p t e -> p (t e)"), [128, NT * 8])

        with ExitStack() as st:
            if moe:
                units = [(Wd['w_exp_gate'][jl][e], Wd['w_exp_up'][jl][e], Wd['w_exp_down'][jl][e], e) for e in range(8)]
            else:
                units = [(Wd['w_ffn_gate'][jl], Wd['w_ffn_up'][jl], Wd['w_ffn_down'][jl], None)]
            groups = [(0, 4), (4, 4), (8, 4), (12, 4), (16, 4), (20, 2)]
            wg_r = Ring([(T("fwg%d" % i, [128, 8, 512], BF16, st), Buf("fwg%d" % i)) for i in range(2)])
            wu_r = Ring([(T("fwu%d" % i, [128, 8, 512], BF16, st), Buf("fwu%d" % i)) for i in range(2)])
            wd_r = Ring([(T("fwd%d" % i, [128, 4, D], BF16, st), Buf("fwd%d" % i)) for i in range(2)])
            aT_r = Ring([(T("faT%d" % i, [128, 4, SEQ], BF16, st), Buf("faT%d" % i)) for i in range(2)])
            sg_r = Ring([(T("fsg%d" % i, [128, 512], F32, st), Buf("fsg%d" % i)) for i in range(3)])
            pr = Ring(PI)
            for (wg2, wu2, wd2, e) in units:
                for (f0, nf) in groups:
                    wg, wgB = wg_r.next(); wu, wuB = wu_r.next(); wd_, wdB = wd_r.next(); aT, aTB = aT_r.next()
                    load_w(wg, wgB, wg2, 0, 8, f0 * 128, nf * 128)
                    load_w(wu, wuB, wu2, 0, 8, f0 * 128, nf * 128)
                    S.dma('pool', wd_[:, 0:nf, :], wd2[f0 * 128:(f0 + nf) * 128, :].rearrange("(j p) n -> p j n", p=128), W=[wdB])
                    for fc in range(nf):
                        tt('pool', wd_[:, fc, :], wd_[:, fc, :], g1g2[:, 1, :], ALU.mult, [wdB, g12B], [wdB])
                    for fc in range(nf):
                        for c in range(NCH):
                            pg, pgB = pr.next(); pu, puB = pr.next()
                            proj_T(pg, pgB, hT, hB, wg, wgB, fc * 128, 128, c)
                            proj_T(pu, puB, hT, hB, wu, wuB, fc * 128, 128, c)
                            sg_, sgB_ = sg_r.next()
                            act(sg_[:, 0:CH], pg[:, 0:CH], AF.Silu, [pgB], [sgB_])
                            tt('dve', aT[:, fc, c * CH:(c + 1) * CH], sg_[:, 0:CH], pu[:, 0:CH], ALU.mult, [sgB_, puB], [aTB])
                    for t in range(NT):
                        for half in range(2):
                            px, pxB = pr.next()
                            for fc in range(nf):
                                mm(px[:, :], aT[:, fc, t * 128:(t + 1) * 128], wd_[:, fc, half * 512:(half + 1) * 512], fc == 0, fc == nf - 1, [aTB, wdB], [pxB])
                            xsl = xs[:, t, half * 512:(half + 1) * 512]
                            if e is None:
                                tt('dve', xsl, px[:, :], xsl, ALU.add, [pxB, xB[t]], [xB[t]])
                            else:
                                stt(xsl, px[:, :], comb[:, t, e:e + 1], xsl, ALU.mult, ALU.add, [pxB, combB, xB[t]], [xB[t]])
            S.barrier()
        stA.close()
        dump("xout%d" % l, xs[:].rearrange("p t d -> p (t d)"), [128, NT * D])

        return True

    for l in layers:
        if not layer_body(l):
            break

    g_out = S.grp("out_sp")
    S.barrier()
    for t in range(NT):
        S.dma('sp', y_d[t * 128:(t + 1) * 128, :], xs[:, t, :], R=[xB[t]], grp=g_out)
    S.barrier(['sp'])
    S.emit(nc, es)
    es.close()
    global LAST_S
    LAST_S = S
    return nc


LAST_S = None


_NC_CACHE = {}


def kernel(**inputs):
    B = 8
    SEQ = 2048
    if 'nc' not in _NC_CACHE:
        _NC_CACHE['nc'] = build(SEQ=SEQ, layers=(0, 1))
    nc = _NC_CACHE['nc']
    x = np.ascontiguousarray(inputs['x'], dtype=np.float32)
    c = np.ascontiguousarray(inputs['c'], dtype=np.float32)
    pos = np.ascontiguousarray(inputs['positions'], dtype=np.int32)
    wts = {n: np.ascontiguousarray(inputs[n], dtype=np.float32) for n in WNAMES}
    in_maps = []
    for b in range(B):
        m = {'x': x[b], 'c': c[b], 'positions': pos[b]}
        m.update(wts)
        in_maps.append(m)
    res = run_bass_kernel_spmd(nc, in_maps, core_ids=list(range(B)))
    return np.stack([np.asarray(r['y'], dtype=np.float32) for r in res.results], axis=0)
```

```python
import math
from contextlib import ExitStack
import numpy as np
import concourse.bass as bass
import concourse.mybir as mybir
from concourse.alu_op_type import AluOpType as ALU
from concourse.bass_utils import run_bass_kernel_spmd

F32 = mybir.dt.float32
BF16 = mybir.dt.bfloat16
I32 = mybir.dt.int32
AF = mybir.ActivationFunctionType
AX = mybir.AxisListType

ENGS = ['pe', 'act', 'dve', 'pool', 'sp']
SAME_ENG_SYNC = True


class Buf:
    __slots__ = ('name', 'w', 'r', 'sbuf', 'g')

    def __init__(self, name, sbuf=True):
        self.name = name
        self.w = None
        self.r = {}
        self.sbuf = sbuf
        self.g = None


class Grp:
    __slots__ = ('name', 'sem', 'count')

    def __init__(self, name):
        self.name = name
        self.sem = None
        self.count = 0


class Sched:
    def __init__(self):
        self.ops = {e: [] for e in ENGS}
        self.seen = {e: {} for e in ENGS}
        self.grps = []
        self.named = {}

    def grp(self, name):
        g = Grp(name)
        self.grps.append(g)
        return g

    def add(self, eng, fn, R=(), W=(), grp=None):
        idx = len(self.ops[eng])
        deps = []
        for b in R:
            if b.w is not None:
                deps.append(b.w)
        for b in W:
            if b.w is not None:
                deps.append(b.w)
            deps.extend(b.r.values())
        waits = {}
        seen = self.seen[eng]
        for d in deps:
            if d[0] == 'c':
                _, e2, i2 = d
                if e2 == eng and (eng == 'pe' or not SAME_ENG_SYNC):
                    continue
                key = ('c', e2)
                val = i2
            else:
                g = d[1]
                key = ('d', g)
                val = g.count
            if seen.get(key, -1) >= val:
                continue
            if waits.get(key, -1) < val:
                waits[key] = val
        for k, v in waits.items():
            seen[k] = v
            if k[0] == 'c':
                self.ops[k[1]][v]['sig'] = True
        if grp is not None:
            grp.count += 1
            tok = ('d', grp, grp.count)
            rkey = grp
        else:
            tok = ('c', eng, idx)
            rkey = eng
        self.ops[eng].append(dict(fn=fn, waits=waits, sig=False, grp=grp))
        for b in R:
            b.r[rkey] = tok
        for b in W:
            b.w = tok
            b.r = {}
        return tok

    def dma(self, q, out, in_, R=(), W=(), grp=None, **kw):
        if grp is None:
            owner = None
            for b in list(W) + list(R):
                if getattr(b, 'sbuf', False):
                    owner = b
                    break
            key = q
            if owner.g is None:
                owner.g = {}
            if key not in owner.g:
                gname = owner.name + '_' + q
                if gname not in self.named:
                    self.named[gname] = self.grp(gname)
                owner.g[key] = self.named[gname]
            grp = owner.g[key]
        return self.add(q, lambda e: e.dma_start(out=out, in_=in_, **kw), R=R, W=W, grp=grp)

    def last_compute(self, e):
        ops = self.ops[e]
        for i in range(len(ops) - 1, -1, -1):
            if ops[i]['fn'] is not None and ops[i]['grp'] is None:
                return i
        return None

    def barrier(self, engs=None):
        lasts = {e: self.last_compute(e) for e in ENGS}
        for e in (engs or ENGS):
            waits = {}
            seen = self.seen[e]
            for e2 in ENGS:
                if e2 == e or lasts[e2] is None:
                    continue
                if seen.get(('c', e2), -1) >= lasts[e2]:
                    continue
                waits[('c', e2)] = lasts[e2]
                self.ops[e2][lasts[e2]]['sig'] = True
            for g in self.grps:
                if g.count > 0 and seen.get(('d', g), -1) < g.count:
                    waits[('d', g)] = g.count
            for k, v in waits.items():
                seen[k] = v
            self.ops[e].append(dict(fn=None, waits=waits, sig=False, grp=None))

    def emit(self, nc, es):
        csem = {e: es.enter_context(nc.semaphore('c_' + e)) for e in ENGS}
        for g in self.grps:
            g.sem = es.enter_context(nc.semaphore('g_' + g.name))
        ordv = {}
        for e in ENGS:
            n = 0
            o = []
            for op in self.ops[e]:
                if op['sig'] and op['grp'] is None:
                    n += 1
                o.append(n)
            ordv[e] = o
        block = es.enter_context(nc.Block())
        ops = self.ops

        def run(e, eng):
            for op in ops[e]:
                for key, val in op['waits'].items():
                    if key[0] == 'c':
                        eng.wait_ge(csem[key[1]], ordv[key[1]][val])
                    else:
                        eng.wait_ge(key[1].sem, 16 * val)
                if op['fn'] is None:
                    continue
                inst = op['fn'](eng)
                if op['grp'] is not None:
                    inst.then_inc(op['grp'].sem, 16)
                elif op['sig']:
                    inst.then_inc(csem[e], 1)

        @block.tensor
        def _(eng):
            run('pe', eng)

        @block.scalar
        def _(eng):
            run('act', eng)

        @block.vector
        def _(eng):
            run('dve', eng)

        @block.gpsimd
        def _(eng):
            run('pool', eng)

        @block.sync
        def _(eng):
            run('sp', eng)


D = 1024
EPS = 1e-6
IN_COLS = 7200
C_SBQ, C_SBK, C_SBV = 0, 512, 1024
C_MLA = 1536
C_DQ, C_DK, C_DV = 2592, 3104, 3616
C_GATE = 4128
DFF = 2816
WNAMES = ['norm1_g', 'norm2_g', 'w_ada', 'b_ada', 'w_in', 'mla_q_norm', 'w_mla_uq', 'mla_kv_norm', 'w_mla_ukv',
          'mla_q_gain', 'mla_k_gain', 'diff_q_gain', 'diff_k_gain', 'diff_lambda', 'diff_subln', 'w_branch',
          'w_out', 'w_ffn_gate', 'w_ffn_up', 'w_ffn_down', 'w_router', 'w_exp_gate', 'w_exp_up', 'w_exp_down']
WSHAPES = {
    'norm1_g': [2, 1024], 'norm2_g': [2, 1024], 'w_ada': [2, 1024, 6144], 'b_ada': [2, 6144],
    'w_in': [2, 1024, 7200], 'mla_q_norm': [2, 768], 'w_mla_uq': [2, 768, 768], 'mla_kv_norm': [2, 256],
    'w_mla_ukv': [2, 256, 1024], 'mla_q_gain': [2, 96], 'mla_k_gain': [2, 96], 'diff_q_gain': [2, 64],
    'diff_k_gain': [2, 64], 'diff_lambda': [2, 4, 64], 'diff_subln': [2, 128], 'w_branch': [2, 3, 512, 1024],
    'w_out': [2, 1024, 1024], 'w_ffn_gate': [1, 1024, 2816], 'w_ffn_up': [1, 1024, 2816],
    'w_ffn_down': [1, 2816, 1024], 'w_router': [1, 1024, 8], 'w_exp_gate': [1, 8, 1024, 2816],
    'w_exp_up': [1, 8, 1024, 2816], 'w_exp_down': [1, 8, 2816, 1024],
}


class Ring:
    def __init__(self, items):
        self.items = items
        self.i = 0

    def next(self):
        it = self.items[self.i % len(self.items)]
        self.i += 1
        return it


def build(SEQ=2048, layers=(0, 1), stop=None, dbg=(), wshapes=None, mcut=99):
    NT = SEQ // 128
    CH = min(512, SEQ)
    NCH = SEQ // CH
    QPC = CH // 128
    nc = bass.Bass("TRN2", target_bir_lowering=False)
    S = Sched()
    es = ExitStack()
    dbg_out = {}

    x_d = nc.dram_tensor("x", [SEQ, D], F32, kind="ExternalInput").ap()
    c_d = nc.dram_tensor("c", [D], F32, kind="ExternalInput").ap()
    pos_d = nc.dram_tensor("positions", [SEQ], I32, kind="ExternalInput").ap()
    Wd = {n: nc.dram_tensor(n, (wshapes or WSHAPES)[n], F32, kind="ExternalInput").ap() for n in WNAMES}
    y_d = nc.dram_tensor("y", [SEQ, D], F32, kind="ExternalOutput").ap()

    def scratch(name, shape, dt=BF16):
        return nc.dram_tensor(name, shape, dt, kind="Internal").ap()
    sbqk_d = scratch("s_sbqk", [8, 128, SEQ]); sbqk_B = [Buf("s_sbqk%d" % i, False) for i in range(8)]
    sbv_d = scratch("s_sbv", [SEQ, 512]); sbv_B = Buf("s_sbv", False)
    mq_d = scratch("s_mq", [8, 96, SEQ]); mq_B = Buf("s_mq", False)
    mk_d = scratch("s_mk", [8, 96, SEQ]); mk_B = Buf("s_mk", False)
    mv_d = scratch("s_mv", [SEQ, 8 * 66]); mv_B = Buf("s_mv", False)
    dq_d = scratch("s_dq", [4, 128, SEQ]); dq_B = Buf("s_dq", False)
    dk_d = scratch("s_dk", [4, 128, SEQ]); dk_B = Buf("s_dk", False)
    dv_d = scratch("s_dv", [SEQ, 4 * 130]); dv_B = Buf("s_dv", False)
    gt_d = scratch("s_gt", [24, 128, SEQ]); gt_B = Buf("s_gt", False)

    uid = [0]

    def T(name, shape, dt, st=None):
        uid[0] += 1
        return (st or es).enter_context(nc.sbuf_tensor("%s_u%d" % (name, uid[0]), shape, dt))

    def act(out, in_, func, R, W, **kw):
        return S.add('act', lambda e: e.activation(out=out, in_=in_, func=func, **kw), R=R, W=W)

    def tt(eng, out, in0, in1, op, R, W):
        return S.add(eng, lambda e: e.tensor_tensor(out=out, in0=in0, in1=in1, op=op), R=R, W=W)

    def ts(eng, out, in0, s1, op0, R, W, s2=None, op1=None):
        if op1 is None:
            return S.add(eng, lambda e: e.tensor_scalar(out=out, in0=in0, scalar1=s1, scalar2=None, op0=op0), R=R, W=W)
        return S.add(eng, lambda e: e.tensor_scalar(out=out, in0=in0, scalar1=s1, scalar2=s2, op0=op0, op1=op1), R=R, W=W)

    def stt(out, in0, scalar, in1, op0, op1, R, W):
        return S.add('dve', lambda e: e.scalar_tensor_tensor(out=out, in0=in0, scalar=scalar, in1=in1, op0=op0, op1=op1), R=R, W=W)

    def mm(out, lhsT, rhs, start, stop, R, W):
        return S.add('pe', lambda e: e.matmul(out, lhsT=lhsT, rhs=rhs, start=start, stop=stop), R=R, W=W)

    def cp(eng, out, in_, R, W):
        if eng == 'act':
            return S.add(eng, lambda e: e.activation(out=out, in_=in_, func=AF.Copy), R=R, W=W)
        return S.add(eng, lambda e: e.tensor_copy(out=out, in_=in_), R=R, W=W)

    def memset(eng, ap, val, W):
        return S.add(eng, lambda e: e.memset(ap, val), W=W)

    def red(out, in_, R, W):
        return S.add('dve', lambda e: e.tensor_reduce(out=out, in_=in_, axis=AX.X, op=ALU.add), R=R, W=W)

    def dump(name, ap, shape, dt=F32):
        if name not in dbg:
            return
        S.barrier()
        d = nc.dram_tensor("dbg_" + name, shape, dt, kind="ExternalOutput").ap()
        dbg_out[name] = d
        S.add('sp', lambda e: e.dma_start(out=d, in_=ap), grp=g_dbg)
        S.barrier()

    g_dbg = S.grp("dbg_sp")
    xs = T("xs", [128, NT, D], F32); xB = [Buf("x%d" % t) for t in range(NT)]
    ident = T("ident", [128, 128], F32); identB = Buf("ident")
    tri_b = T("tri_b", [128, 128], BF16); triB = Buf("tri")
    ones_b = T("ones_b", [128, 128], BF16); onesB = Buf("ones")
    upper_b = T("upper_b", [128, 128], BF16); upperB = Buf("upper")
    pos_tok = T("pos_tok", [128, NT], F32); posB = Buf("pos")
    invf = T("invf", [128, 16], F32); invfB = Buf("invf")
    modT = T("modT", [128, 32], F32); modTB = Buf("modT")
    ABT = T("ABT", [128, 16], F32); ABTB = Buf("ABT")
    g1g2 = T("g1g2", [128, 2, D], F32); g12B = Buf("g1g2")
    comb = T("comb", [128, NT, 8], F32); combB = Buf("comb")
    identb = T("identb", [128, 128], BF16); identbB = Buf("identb")
    SC = T("SC", [128, NT, 32], F32); SCB = Buf("SC")
    PS = [es.enter_context(nc.psum_tensor("ps%d" % i, [128, 512], F32)) for i in range(8)]
    PB = [Buf("ps%d" % i) for i in range(8)]
    PI = list(zip(PS, PB))

    def tr(out, in_, R, W):
        return S.add('pe', lambda e: e.transpose(out, in_, ident[:in_.shape[0], :in_.shape[0]]), R=list(R) + [identB], W=W)

    def rsqrt(tag, out, in_, scale, R, W, st):
        n = in_.shape[1]
        v = T("rsq_" + tag, [128, n], F32, st); vB = Buf("rsq_" + tag)
        S.add('dve', lambda e: e.tensor_scalar(out=v[:], in0=in_, scalar1=scale, scalar2=EPS, op0=ALU.mult, op1=ALU.add), R=R, W=[vB])
        act(v[:], v[:], AF.Ln, [vB], [vB])
        act(out, v[:], AF.Exp, [vB], W, scale=-0.5)

    g_in = S.grp("in_sp")
    for t in range(NT):
        S.dma('sp', xs[:, t, :], x_d[t * 128:(t + 1) * 128, :], W=[xB[t]], grp=g_in)
    memset('pool', ident[:], 1.0, [identB])
    S.add('pool', lambda e: e.affine_select(out=ident[:], in_=ident[:], pattern=[[-1, 128]], compare_op=ALU.is_equal, fill=0.0, base=0, channel_multiplier=1), R=[identB], W=[identB])
    memset('pool', tri_b[:], 1.0, [triB])
    S.add('pool', lambda e: e.affine_select(out=tri_b[:], in_=tri_b[:], pattern=[[1, 128]], compare_op=ALU.is_ge, fill=0.0, base=0, channel_multiplier=-1), R=[triB], W=[triB])
    memset('pool', ones_b[:], 1.0, [onesB])
    memset('pool', upper_b[:], 1.0, [upperB])
    S.add('pool', lambda e: e.affine_select(out=upper_b[:], in_=upper_b[:], pattern=[[-1, 128]], compare_op=ALU.is_gt, fill=0.0, base=0, channel_multiplier=1), R=[upperB], W=[upperB])
    with ExitStack() as st:
        pi = T("pos_i", [128, NT], I32, st); tB = Buf("pos_i")
        S.dma('sp', pi[:], pos_d.rearrange("(t p) -> p t", p=128), W=[tB], grp=g_in, allow_slow_non_contiguous=True)
        cp('dve', pos_tok[:], pi[:], [tB], [posB])
        ii = T("iota_i", [128, 16], I32, st); iB = Buf("iota")
        S.add('pool', lambda e: e.iota(ii[:], pattern=[[1, 16]], base=0, channel_multiplier=0), W=[iB])
        cp('dve', invf[:], ii[:], [iB], [invfB])
        act(invf[:], invf[:], AF.Exp, [invfB], [invfB], scale=-math.log(10000.0) / 16.0)
        cp('dve', identb[:], ident[:], [identB], [identbB])
        TWO_PI_ = 2.0 * math.pi
        angA = T("angA", [128, NT, 16], F32, st); angB = Buf("angA")
        sct = T("sc_tmp", [128, NT * 32], F32, st); sctB = Buf("sc_tmp")
        sci = T("sc_ki", [128, NT * 32], I32, st); sciB = Buf("sc_ki")
        SC2 = SC[:].rearrange("p t s -> p (t s)")
        tt('dve', angA[:], invf[:].unsqueeze(1).broadcast_to([128, NT, 16]), pos_tok[:].unsqueeze(2).broadcast_to([128, NT, 16]), ALU.mult, [invfB, posB], [angB])
        ts('dve', SC[:, :, 0:16], angA[:], math.pi, ALU.add, [angB], [SCB])
        ts('dve', SC[:, :, 16:32], angA[:], 1.5 * math.pi, ALU.add, [angB], [SCB])
        ts('dve', sct[:], SC2, 1.0 / TWO_PI_, ALU.mult, [SCB], [sctB])
        cp('dve', sci[:], sct[:], [sctB], [sciB])
        cp('dve', sct[:], sci[:], [sciB], [sctB])
        stt(SC2, sct[:], -TWO_PI_, SC2, ALU.mult, ALU.add, [sctB, SCB], [SCB])
        ts('dve', sct[:], SC2, 0.0, ALU.is_lt, [SCB], [sctB])
        stt(SC2, sct[:], TWO_PI_, SC2, ALU.mult, ALU.add, [sctB, SCB], [SCB])
        ts('dve', SC2, SC2, -math.pi, ALU.add, [SCB], [SCB])
        ts('dve', SC2, SC2, -math.pi, ALU.max, [SCB], [SCB], s2=math.pi, op1=ALU.min)
        act(SC2, SC2, AF.Sin, [SCB], [SCB])
        S.barrier()

    def layer_body(l):
        lam_init = 0.8 - 0.6 * math.exp(-0.3 * l)
        w_in = Wd['w_in'][l]

        with ExitStack() as st:
            wa_r = Ring([(T("wa%d" % i, [128, 8, 512], F32, st), Buf("wa%d" % i)) for i in range(2)])
            bb_r = Ring([(T("bb%d" % i, [128, 512], F32, st), Buf("bb%d" % i)) for i in range(2)])
            mb_r = Ring([(T("mb%d" % i, [128, 512], F32, st), Buf("mb%d" % i)) for i in range(2)])
            gT = T("gT", [128, 16], F32, st); gTB = Buf("gT")
            condB_t = T("condB", [128, 8, 128], F32, st); condBB = Buf("condB")
            cT = T("cT", [128, 8], F32, st); cB = Buf("cT")
            S.dma('sp', cT[:], c_d.rearrange("(j p) -> p j", p=128), W=[cB], allow_slow_non_contiguous=True)
            act(cT[:], cT[:], AF.Silu, [cB], [cB])
            cp('dve', condB_t[:], cT[:].unsqueeze(2).broadcast_to([128, 8, 128]), [cB], [condBB])
            S.dma('sp', gT[:, 0:8], Wd['norm1_g'][l].rearrange("(j p) -> p j", p=128), W=[gTB], allow_slow_non_contiguous=True)
            S.dma('sp', gT[:, 8:16], Wd['norm2_g'][l].rearrange("(j p) -> p j", p=128), W=[gTB], allow_slow_non_contiguous=True)
            pr = Ring(PI[0:2]); pr2 = Ring(PI[2:4])
            for n in range(12):
                wa, waB = wa_r.next(); bb, bbB = bb_r.next(); mb, mbB = mb_r.next()
                S.dma('sp', wa[:, 0:4, :], Wd['w_ada'][l][0:512, n * 512:(n + 1) * 512].rearrange("(j p) n -> p j n", p=128), W=[waB])
                S.dma('pool', wa[:, 4:8, :], Wd['w_ada'][l][512:1024, n * 512:(n + 1) * 512].rearrange("(j p) n -> p j n", p=128), W=[waB])
                S.dma('sp', bb[:], Wd['b_ada'][l][n * 512:(n + 1) * 512].partition_broadcast(128), W=[bbB])
                ps, psB = pr.next()
                for j in range(8):
                    mm(ps[:, :], condB_t[:, j, :], wa[:, j, :], j == 0, j == 7, [condBB, waB], [psB])
                if n in (4, 5, 10, 11):
                    gi = 0 if n < 6 else 1
                    hf = n % 2
                    tt('dve', g1g2[:, gi, hf * 512:(hf + 1) * 512], ps[:, :], bb[:], ALU.add, [psB, bbB], [g12B])
                else:
                    tt('dve', mb[:], ps[:, :], bb[:], ALU.add, [psB, bbB], [mbB])
                    base = {0: 0, 1: 4, 2: 8, 3: 12, 6: 16, 7: 20, 8: 24, 9: 28}[n]
                    p2, p2B = pr2.next()
                    for q in range(4):
                        tr(p2[:, q * 128:(q + 1) * 128], mb[:, q * 128:(q + 1) * 128], [mbB], [p2B])
                    cp('act' if n % 2 else 'dve', modT[:, base:base + 4], p2[:, :].rearrange("p (q t) -> p q t", t=128)[:, :, 0], [p2B], [modTB])
            stt(ABT[:, 0:8], modT[:, 8:16], 1.0, gT[:, 0:8], ALU.add, ALU.mult, [modTB, gTB], [ABTB])
            stt(ABT[:, 8:16], modT[:, 24:32], 1.0, gT[:, 8:16], ALU.add, ALU.mult, [modTB, gTB], [ABTB])
            S.barrier()
        dump("modT%d" % l, modT[:], [128, 32])
        dump("ABT%d" % l, ABT[:], [128, 16])
        dump("g1g2_%d" % l, g1g2[:].rearrange("p a d -> p (a d)"), [128, 2 * D])
        if stop == 'A0':
            return False

        def norm_phase(which, hT, hB, st, router=None):
            aoff = 0 if which == 1 else 8
            boff = 0 if which == 1 else 16
            junk_r = Ring([(T("nj%d" % i, [128, D], BF16, st), Buf("nj%d" % i)) for i in range(2)])
            xn_r = Ring([(T("xn%d" % i, [128, D], F32, st), Buf("xn%d" % i)) for i in range(2)])
            ss_r = Ring([(T("nss%d" % i, [128, 2], F32, st), Buf("nss%d" % i)) for i in range(3)])
            pr = Ring([(PI[0], PI[1]), (PI[2], PI[3])])
            def n0(t):
                junk, jB = junk_r.next(); xn, xnB = xn_r.next(); ss, ssB = ss_r.next()
                act(junk[:], xs[:, t, :], AF.Square, [xB[t]], [jB, ssB], accum_out=ss[:, 0:1])
                ts('dve', ss[:, 1:2], ss[:, 0:1], 1.0 / D, ALU.mult, [ssB], [ssB], s2=EPS, op1=ALU.add)
                act(ss[:, 1:2], ss[:, 1:2], AF.Ln, [ssB], [ssB])
                act(ss[:, 1:2], ss[:, 1:2], AF.Exp, [ssB], [ssB], scale=-0.5)
                ts('dve', xn[:], xs[:, t, :], ss[:, 1:2], ALU.mult, [xB[t], ssB], [xnB])
                return xn, xnB

            def n1(t, xn, xnB):
                (pa, paB), (pb, pbB) = pr.next()
                for j in range(8):
                    p, pB_ = (pa, paB) if j < 4 else (pb, pbB)
                    tr(p[:, (j % 4) * 128:(j % 4 + 1) * 128], xn[:, j * 128:(j + 1) * 128], [xnB], [pB_])
                for j in range(8):
                    p, pB_ = (pa, paB) if j < 4 else (pb, pbB)
                    src = p[:, (j % 4) * 128:(j % 4 + 1) * 128]
                    dst = hT[:, j, t * 128:(t + 1) * 128]
                    if j % 2 == 0:
                        act(dst, src, AF.Identity, [pB_, ABTB, modTB], [hB[t]], scale=ABT[:, aoff + j:aoff + j + 1], bias=modT[:, boff + j:boff + j + 1])
                    else:
                        ts('dve', dst, src, ABT[:, aoff + j:aoff + j + 1], ALU.mult, [pB_, ABTB, modTB], [hB[t]], s2=modT[:, boff + j:boff + j + 1], op1=ALU.add)
                if router is not None:
                    router(t, pa, paB, pb, pbB, aoff, boff)

            pend = {}
            for k in range(NT + 1):
                if k < NT:
                    pend[k] = n0(k)
                if k >= 1:
                    n1(k - 1, *pend.pop(k - 1))

        def load_w(tile, buf, src2d, r0, nrow_chunks, c0, ncols):
            S.dma('pool', tile[:, 0:nrow_chunks, 0:ncols],
                  src2d[r0:r0 + 128 * nrow_chunks, c0:c0 + ncols].rearrange("(j p) n -> p j n", p=128), W=[buf])

        def proj_tok(ps, psB, hT, hB, W, WB, c0, ncols, t):
            for j in range(8):
                mm(ps[:, 0:ncols], hT[:, j, t * 128:(t + 1) * 128], W[:, j, c0:c0 + ncols], j == 0, j == 7, [hB[t], WB], [psB])

        def proj_T(ps, psB, hT, hB, W, WB, c0, ncols, c):
            R = [hB[t] for t in range(c * QPC, (c + 1) * QPC)] + [WB]
            for j in range(8):
                mm(ps[0:ncols, 0:CH], W[:, j, c0:c0 + ncols], hT[:, j, c * CH:(c + 1) * CH], j == 0, j == 7, R, [psB])

        stA = ExitStack()
        hT = T("hT", [128, 8, SEQ], BF16, stA); hB = [Buf("h%d" % t) for t in range(NT)]
        with ExitStack() as st:
            norm_phase(1, hT, hB, st)
            S.barrier()
        dump("hT%d" % l, hT[:].rearrange("p j t -> p (j t)"), [128, 8 * SEQ], BF16)
        if stop == 'A1':
            stA.close()
            return False

        with ExitStack() as st:
            w_r = Ring([(T("wsb%d" % i, [128, 8, 512], BF16, st), Buf("wsb%d" % i)) for i in range(2)])
            stg_r = Ring([(T("sbst%d" % i, [128, SEQ], BF16, st), Buf("sbst%d" % i)) for i in range(3)])
            stv_r = Ring([(T("sbsv%d" % i, [128, 512], BF16, st), Buf("sbsv%d" % i)) for i in range(3)])
            pr = Ring(PI)
            for part in range(3):
                W, WB = w_r.next()
                load_w(W, WB, w_in, 0, 8, part * 512, 512)
                if part < 2:
                    for m in range(4):
                        stg, stgB = stg_r.next()
                        for c in range(NCH):
                            ps, psB = pr.next()
                            proj_T(ps, psB, hT, hB, W, WB, m * 128, 128, c)
                            cp('act' if c % 2 else 'dve', stg[:, c * CH:(c + 1) * CH], ps[:, 0:CH], [psB], [stgB])
                        S.dma('sp', sbqk_d[part * 4 + m], stg[:], R=[stgB], W=[sbqk_B[part * 4 + m]])
                else:
                    for t in range(NT):
                        stv, stvB = stv_r.next()
                        ps, psB = pr.next()
                        proj_tok(ps, psB, hT, hB, W, WB, 0, 512, t)
                        cp('act' if t % 2 else 'dve', stv[:], ps[:, :], [psB], [stvB])
                        S.dma('sp', sbv_d[t * 128:(t + 1) * 128, :], stv[:], R=[stvB], W=[sbv_B])
            S.barrier()
        if stop == 'A2SB':
            stA.close()
            return False
        with ExitStack() as st:
            Wm = T("wmla", [128, 8, 1056], BF16, st); WmB = Buf("wmla")
            for i in range(3):
                c0 = i * 512
                ncol = min(512, 1056 - c0)
                S.dma('pool', Wm[:, :, c0:c0 + ncol], w_in[:, C_MLA + c0:C_MLA + c0 + ncol].rearrange("(j p) n -> p j n", p=128), W=[WmB])
            Wuq = T("wuq", [128, 6, 768], BF16, st); WuqB = Buf("wuq")
            load_w(Wuq, WuqB, Wd['w_mla_uq'][l], 0, 6, 0, 768)
            Wukv = T("wukv", [128, 2, 1024], BF16, st); WukvB = Buf("wukv")
            load_w(Wukv, WukvB, Wd['w_mla_ukv'][l], 0, 2, 0, 1024)
            nrm = T("mnrm", [128, 8], F32, st); nrmB = Buf("mnrm")
            S.dma('sp', nrm[:, 0:6], Wd['mla_q_norm'][l].rearrange("(j p) -> p j", p=128), W=[nrmB], allow_slow_non_contiguous=True)
            S.dma('sp', nrm[:, 6:8], Wd['mla_kv_norm'][l].rearrange("(j p) -> p j", p=128), W=[nrmB], allow_slow_non_contiguous=True)
            for j in range(6):
                ts('dve', Wuq[:, j, :], Wuq[:, j, :], nrm[:, j:j + 1], ALU.mult, [WuqB, nrmB], [WuqB])
            for j in range(2):
                ts('dve', Wukv[:, j, :], Wukv[:, j, :], nrm[:, 6 + j:7 + j], ALU.mult, [WukvB, nrmB], [WukvB])
            gq = T("mgq", [128, 3], F32, st); gqB = Buf("mgq")
            S.dma('sp', gq[0:96, 0:1], Wd['mla_q_gain'][l].rearrange("(p o) -> p o", o=1), W=[gqB])
            S.dma('sp', gq[0:96, 1:2], Wd['mla_k_gain'][l].rearrange("(p o) -> p o", o=1), W=[gqB])
            stt(gq[0:96, 2:3], gq[0:96, 0:1], 1.0 / math.sqrt(96.0), gq[0:96, 1:2], ALU.mult, ALU.mult, [gqB], [gqB])
            pr = Ring(PI)
            NR = 2
            junk_r = Ring([(T("mj%d" % i, [128, 512], BF16, st), Buf("mj%d" % i)) for i in range(NR)])
            ss_r = Ring([(T("mss%d" % i, [128, 8], F32, st), Buf("mss%d" % i)) for i in range(NR)])
            cf_r = Ring([(T("mcf%d" % i, [128, 1024], F32, st), Buf("mcf%d" % i)) for i in range(NR)])
            cT_r = Ring([(T("mcT%d" % i, [128, 8, 128], BF16, st), Buf("mcT%d" % i)) for i in range(NR)])
            qf_r = Ring([(T("mqf%d" % i, [128, 768], F32, st), Buf("mqf%d" % i)) for i in range(NR)])
            kf_r = Ring([(T("mkf%d" % i, [128, 768], F32, st), Buf("mkf%d" % i)) for i in range(NR)])
            vb_r = Ring([(T("mvb%d" % i, [128, 8, 66], BF16, st), Buf("mvb%d" % i)) for i in range(NR)])
            rp_r = Ring([(T("mrp%d" % i, [128, 8, 64], F32, st), Buf("mrp%d" % i)) for i in range(NR)])
            sm_r = Ring([(T("msm%d" % i, [128, 192], F32, st), Buf("msm%d" % i)) for i in range(NR)])
            sq_r = Ring([(T("msq%d" % i, [128, 768], F32, st), Buf("msq%d" % i)) for i in range(NR)])
            rq_r = Ring([(T("mrq%d" % i, [128, 32], F32, st), Buf("mrq%d" % i)) for i in range(NR)])
            stq_r = Ring([(T("mstq%d" % i, [128, 8, 128], BF16, st), Buf("mstq%d" % i)) for i in range(NR)])
            stk_r = Ring([(T("mstk%d" % i, [128, 8, 128], BF16, st), Buf("mstk%d" % i)) for i in range(NR)])
            for it in vb_r.items:
                memset('pool', it[0][:, :, 64:66], 1.0, [it[1]])
            prA = Ring(PI[0:3]); prB = Ring(PI[3:8])
            ST = {}

            def sA_pe(t):
                d = ST.setdefault(t, {})
                pa, paB = prA.next(); pb, pbB = prA.next(); pc, pcB = prA.next()
                d['pa'] = (pa, paB); d['pb'] = (pb, pbB); d['pc'] = (pc, pcB)
                proj_tok(pa, paB, hT, hB, Wm, WmB, 0, 512, t)
                proj_tok(pb, pbB, hT, hB, Wm, WmB, 512, 512, t)
                proj_tok(pc, pcB, hT, hB, Wm, WmB, 1024, 32, t)
                junk, jB = junk_r.next(); ss, ssB = ss_r.next()
                d['ss'] = (ss, ssB)
                act(junk[:, 0:512], pa[:, :], AF.Square, [paB], [jB, ssB], accum_out=ss[:, 0:1])
                act(junk[:, 0:256], pb[:, 0:256], AF.Square, [pbB], [jB, ssB], accum_out=ss[:, 1:2])
                act(junk[:, 0:256], pb[:, 256:512], AF.Square, [pbB], [jB, ssB], accum_out=ss[:, 2:3])

            def sA_dve(t):
                d = ST[t]
                pa, paB = d['pa']; pb, pbB = d['pb']; pc, pcB = d['pc']; ss, ssB = d['ss']
                cf, cfB = cf_r.next(); sm, smB = sm_r.next()
                d['cf'] = (cf, cfB); d['sm'] = (sm, smB)
                cp('dve', cf[:, 0:512], pa[:, :], [paB], [cfB])
                cp('dve', cf[:, 512:1024], pb[:, :], [pbB], [cfB])
                tt('dve', ss[:, 0:1], ss[:, 0:1], ss[:, 1:2], ALU.add, [ssB], [ssB])
                ts('dve', ss[:, 4:5], ss[:, 0:1], 1.0 / 768, ALU.mult, [ssB], [ssB], s2=EPS, op1=ALU.add)
                ts('dve', ss[:, 5:6], ss[:, 2:3], 1.0 / 256, ALU.mult, [ssB], [ssB], s2=EPS, op1=ALU.add)
                act(ss[:, 4:6], ss[:, 4:6], AF.Ln, [ssB], [ssB])
                act(ss[:, 4:6], ss[:, 4:6], AF.Exp, [ssB], [ssB], scale=-0.5)
                sinv = SC[:, t, 0:16]; cosv = SC[:, t, 16:32]
                k1 = pc[:, 0:16]; k2 = pc[:, 16:32]
                tt('dve', sm[:, 48:64], k1, cosv, ALU.mult, [pcB, SCB], [smB])
                tt('dve', sm[:, 64:80], k2, sinv, ALU.mult, [pcB, SCB], [smB])
                tt('dve', sm[:, 80:96], k1, sinv, ALU.mult, [pcB, SCB], [smB])
                tt('dve', sm[:, 96:112], k2, cosv, ALU.mult, [pcB, SCB], [smB])
                tt('dve', sm[:, 112:128], sm[:, 48:64], sm[:, 64:80], ALU.subtract, [smB], [smB])
                tt('dve', sm[:, 128:144], sm[:, 80:96], sm[:, 96:112], ALU.add, [smB], [smB])

            def sB(t):
                d = ST[t]
                cf, cfB = d['cf']; ss, ssB = d['ss']
                cT, cTB = cT_r.next()
                pt1, pt1B = prB.next(); pt2, pt2B = prB.next()
                for j in range(8):
                    p, pB_ = (pt1, pt1B) if j < 4 else (pt2, pt2B)
                    tr(p[:, (j % 4) * 128:(j % 4 + 1) * 128], cf[:, j * 128:(j + 1) * 128], [cfB], [pB_])
                cp('act', cT[:, 0:4, :].rearrange("p j t -> p (j t)"), pt1[:, :], [pt1B], [cTB])
                cp('dve', cT[:, 4:8, :].rearrange("p j t -> p (j t)"), pt2[:, :], [pt2B], [cTB])
                pq = [prB.next(), prB.next()]; pkv = [prB.next(), prB.next()]
                for half in range(2):
                    for j in range(6):
                        mm(pq[half][0][:, 0:384], cT[:, j, :], Wuq[:, j, half * 384:(half + 1) * 384], j == 0, j == 5, [cTB, WuqB], [pq[half][1]])
                for half in range(2):
                    for j in range(2):
                        mm(pkv[half][0][:, 0:512], cT[:, 6 + j, :], Wukv[:, j, half * 512:(half + 1) * 512], j == 0, j == 1, [cTB, WukvB], [pkv[half][1]])
                qf, qfB = qf_r.next(); kf, kfB = kf_r.next(); vb, vbB = vb_r.next()
                d['qf'] = (qf, qfB); d['kf'] = (kf, kfB); d['vb'] = (vb, vbB)
                k3 = kf[:].rearrange("p (h d) -> p h d", d=96)
                for half in range(2):
                    ts('dve', qf[:, half * 384:(half + 1) * 384], pq[half][0][:, 0:384], ss[:, 4:5], ALU.mult, [pq[half][1], ssB], [qfB])
                    src3 = pkv[half][0][:, :].rearrange("p (h e) -> p h e", e=128)
                    ts('dve', k3[:, half * 4:(half + 1) * 4, 0:64], src3[:, :, 0:64], ss[:, 5:6], ALU.mult, [pkv[half][1], ssB], [kfB])
                    ts('dve', vb[:, half * 4:(half + 1) * 4, 0:64], src3[:, :, 64:128], ss[:, 5:6], ALU.mult, [pkv[half][1], ssB], [vbB])

            def sC_dve(t):
                d = ST[t]
                qf, qfB = d['qf']; kf, kfB = d['kf']; sm, smB = d['sm']
                q3 = qf[:].rearrange("p (h d) -> p h d", d=96); k3 = kf[:].rearrange("p (h d) -> p h d", d=96)
                rp, rpB = rp_r.next()
                sinv = SC[:, t, 0:16]; cosv = SC[:, t, 16:32]
                sinB3 = sinv.unsqueeze(1).broadcast_to([128, 8, 16]); cosB3 = cosv.unsqueeze(1).broadcast_to([128, 8, 16])
                q1 = q3[:, :, 64:80]; q2 = q3[:, :, 80:96]
                tt('dve', rp[:, :, 0:16], q1, cosB3, ALU.mult, [qfB, SCB], [rpB])
                tt('dve', rp[:, :, 16:32], q2, sinB3, ALU.mult, [qfB, SCB], [rpB])
                tt('dve', rp[:, :, 32:48], q1, sinB3, ALU.mult, [qfB, SCB], [rpB])
                tt('dve', rp[:, :, 48:64], q2, cosB3, ALU.mult, [qfB, SCB], [rpB])
                tt('dve', q1, rp[:, :, 0:16], rp[:, :, 16:32], ALU.subtract, [rpB], [qfB])
                tt('dve', q2, rp[:, :, 32:48], rp[:, :, 48:64], ALU.add, [rpB], [qfB])
                cp('dve', k3[:, :, 64:96], sm[:, 112:144].unsqueeze(1).broadcast_to([128, 8, 32]), [smB], [kfB])
                sq, sqB = sq_r.next(); rq, rqB = rq_r.next()
                tt('dve', sq[:], qf[:], qf[:], ALU.mult, [qfB], [sqB])
                red(rq[:, 0:8], sq[:].rearrange("p (h d) -> p h d", d=96), [sqB], [rqB])
                tt('pool', sq[:], kf[:], kf[:], ALU.mult, [kfB, rqB], [sqB])
                red(rq[:, 8:16], sq[:].rearrange("p (h d) -> p h d", d=96), [sqB], [rqB])
                ts('dve', rq[:, 16:32], rq[:, 0:16], 1.0 / 96, ALU.mult, [rqB], [rqB], s2=EPS, op1=ALU.add)
                act(rq[:, 16:32], rq[:, 16:32], AF.Ln, [rqB], [rqB])
                act(rq[:, 16:32], rq[:, 16:32], AF.Exp, [rqB], [rqB], scale=-0.5)
                tt('dve', q3, q3, rq[:, 16:24].unsqueeze(2).broadcast_to([128, 8, 96]), ALU.mult, [qfB, rqB], [qfB])
                tt('dve', k3, k3, rq[:, 24:32].unsqueeze(2).broadcast_to([128, 8, 96]), ALU.mult, [kfB, rqB], [kfB])

            def sC_rest(t):
                d = ST.pop(t)
                qf, qfB = d['qf']; kf, kfB = d['kf']; vb, vbB = d['vb']
                q3 = qf[:].rearrange("p (h d) -> p h d", d=96); k3 = kf[:].rearrange("p (h d) -> p h d", d=96)
                stq, stqB = stq_r.next(); stk, stkB = stk_r.next()
                for (src3_, srcB, stg, stgB, isq) in ((q3, qfB, stq, stqB, True), (k3, kfB, stk, stkB, False)):
                    for half in range(2):
                        p, pB_ = prB.next()
                        for hh in range(4):
                            tr(p[0:96, hh * 128:(hh + 1) * 128], src3_[:, half * 4 + hh, :], [srcB], [pB_])
                        src = p[0:96, :]
                        dst = stg[0:96, half * 4:(half + 1) * 4, :].rearrange("p h t -> p (h t)")
                        if isq:
                            act(dst, src, AF.Identity, [pB_, gqB], [stgB], scale=gq[0:96, 2:3])
                        else:
                            cp('dve', dst, src, [pB_], [stgB])
                S.dma('sp', mq_d[:, :, t * 128:(t + 1) * 128].rearrange("h p t -> p h t"), stq[0:96, :, :], R=[stqB], W=[mq_B])
                S.dma('sp', mk_d[:, :, t * 128:(t + 1) * 128].rearrange("h p t -> p h t"), stk[0:96, :, :], R=[stkB], W=[mk_B])
                S.dma('sp', mv_d[t * 128:(t + 1) * 128, :], vb[:].rearrange("p h e -> p (h e)"), R=[vbB], W=[mv_B])

            sA_pe(0); sA_dve(0)
            for k in range(NT):
                sB(k)
                if k + 1 < NT:
                    sA_pe(k + 1)
                sC_dve(k)
                if k + 1 < NT:
                    sA_dve(k + 1)
                sC_rest(k)
            S.barrier()

        if stop == 'A2MLA':
            stA.close()
            return False
        with ExitStack() as st:
            W3 = [(T("wdf%d" % i, [128, 8, 512], BF16, st), Buf("wdf%d" % i)) for i in range(3)]
            for i, c0 in enumerate((C_DQ, C_DK, C_DV)):
                load_w(W3[i][0], W3[i][1], w_in, 0, 8, c0, 512)
            gd = T("dgd", [128, 3], F32, st); gdB = Buf("dgd")
            for half in range(2):
                S.dma('sp', gd[half * 64:(half + 1) * 64, 0:1], Wd['diff_q_gain'][l].rearrange("(p o) -> p o", o=1), W=[gdB])
                S.dma('sp', gd[half * 64:(half + 1) * 64, 1:2], Wd['diff_k_gain'][l].rearrange("(p o) -> p o", o=1), W=[gdB])
            stt(gd[:, 2:3], gd[:, 0:1], 1.0 / 8.0, gd[:, 1:2], ALU.mult, ALU.mult, [gdB], [gdB])
            pr = Ring(PI)
            NR = 2
            sq_r = Ring([(T("dsq%d" % i, [128, 512], F32, st), Buf("dsq%d" % i)) for i in range(NR)])
            rr_r = Ring([(T("drr%d" % i, [128, 32], F32, st), Buf("drr%d" % i)) for i in range(NR)])
            qn_r = Ring([(T("dqn%d" % i, [128, 512], F32, st), Buf("dqn%d" % i)) for i in range(NR)])
            kn_r = Ring([(T("dkn%d" % i, [128, 512], F32, st), Buf("dkn%d" % i)) for i in range(NR)])
            stq_r = Ring([(T("dstq%d" % i, [128, 4, 128], BF16, st), Buf("dstq%d" % i)) for i in range(NR)])
            stk_r = Ring([(T("dstk%d" % i, [128, 4, 128], BF16, st), Buf("dstk%d" % i)) for i in range(NR)])
            vb_r = Ring([(T("dvb%d" % i, [128, 4, 130], BF16, st), Buf("dvb%d" % i)) for i in range(NR)])
            for it in vb_r.items:
                memset('pool', it[0][:, :, 128:130], 1.0, [it[1]])
            prA = Ring(PI[0:3]); prB = Ring(PI[3:8])
            DS = {}

            def d0(t):
                pq, pqB = prA.next(); pk, pkB = prA.next(); pv, pvB = prA.next()
                proj_tok(pq, pqB, hT, hB, W3[0][0], W3[0][1], 0, 512, t)
                proj_tok(pk, pkB, hT, hB, W3[1][0], W3[1][1], 0, 512, t)
                proj_tok(pv, pvB, hT, hB, W3[2][0], W3[2][1], 0, 512, t)
                rr, rrB = rr_r.next()
                for (p, pB_, off) in ((pq, pqB, 0), (pk, pkB, 8)):
                    sq, sqB = sq_r.next()
                    act(sq[:], p[:, :], AF.Square, [pB_], [sqB])
                    red(rr[:, off:off + 8], sq[:].rearrange("p (g d) -> p g d", d=64), [sqB], [rrB])
                ts('dve', rr[:, 16:32], rr[:, 0:16], 1.0 / 64, ALU.mult, [rrB], [rrB], s2=EPS, op1=ALU.add)
                act(rr[:, 16:32], rr[:, 16:32], AF.Ln, [rrB], [rrB])
                act(rr[:, 16:32], rr[:, 16:32], AF.Exp, [rrB], [rrB], scale=-0.5)
                qn, qnB = qn_r.next(); kn, knB = kn_r.next(); vb, vbB = vb_r.next()
                tt('dve', qn[:].rearrange("p (g d) -> p g d", d=64), pq[:, :].rearrange("p (g d) -> p g d", d=64),
                   rr[:, 16:24].unsqueeze(2).broadcast_to([128, 8, 64]), ALU.mult, [pqB, rrB], [qnB])
                tt('dve', kn[:].rearrange("p (g d) -> p g d", d=64), pk[:, :].rearrange("p (g d) -> p g d", d=64),
                   rr[:, 24:32].unsqueeze(2).broadcast_to([128, 8, 64]), ALU.mult, [pkB, rrB], [knB])
                cp('dve', vb[:, :, 0:128], pv[:, :].rearrange("p (h e) -> p h e", e=128), [pvB], [vbB])
                DS[t] = (qn, qnB, kn, knB, vb, vbB)

            def d1(t):
                qn, qnB, kn, knB, vb, vbB = DS.pop(t)
                stq, stqB = stq_r.next(); stk, stkB = stk_r.next()
                p, pB_ = prB.next()
                for g in range(4):
                    tr(p[:, g * 128:(g + 1) * 128], qn[:, g * 128:(g + 1) * 128], [qnB], [pB_])
                act(stq[:].rearrange("p g t -> p (g t)"), p[:, :], AF.Identity, [pB_, gdB], [stqB], scale=gd[:, 2:3])
                p, pB_ = prB.next()
                for g in range(4):
                    tr(p[:, g * 128:(g + 1) * 128], kn[:, g * 128:(g + 1) * 128], [knB], [pB_])
                cp('dve', stk[:].rearrange("p g t -> p (g t)"), p[:, :], [pB_], [stkB])
                S.dma('sp', dq_d[:, :, t * 128:(t + 1) * 128].rearrange("g p t -> p g t"), stq[:], R=[stqB], W=[dq_B])
                S.dma('sp', dk_d[:, :, t * 128:(t + 1) * 128].rearrange("g p t -> p g t"), stk[:], R=[stkB], W=[dk_B])
                S.dma('sp', dv_d[t * 128:(t + 1) * 128, :], vb[:].rearrange("p h e -> p (h e)"), R=[vbB], W=[dv_B])

            d0(0)
            for k in range(NT):
                if k + 1 < NT:
                    d0(k + 1)
                d1(k)
            S.barrier()

        if stop == 'A2DIFF':
            stA.close()
            return False
        with ExitStack() as st:
            w_r = Ring([(T("wgt%d" % i, [128, 8, 512], BF16, st), Buf("wgt%d" % i)) for i in range(2)])
            stg_r = Ring([(T("gst%d" % i, [128, SEQ], BF16, st), Buf("gst%d" % i)) for i in range(3)])
            pr = Ring(PI)
            for part in range(6):
                W, WB = w_r.next()
                load_w(W, WB, w_in, 0, 8, C_GATE + part * 512, 512)
                for m in range(4):
                    stg, stgB = stg_r.next()
                    for c in range(NCH):
                        ps, psB = pr.next()
                        proj_T(ps, psB, hT, hB, W, WB, m * 128, 128, c)
                        act(stg[:, c * CH:(c + 1) * CH], ps[:, 0:CH], AF.Sigmoid, [psB], [stgB])
                    S.dma('sp', gt_d[part * 4 + m], stg[:], R=[stgB], W=[gt_B])
            S.barrier()

        if stop == 'A2G':
            stA.close()
            return False
        stA.close()
        stB = ExitStack()
        oT = [T("oT%d" % n, [128, 4, SEQ], BF16, stB) for n in range(3)]
        oTB = [[Buf("oT%d_%d" % (n, k)) for k in range(4)] for n in range(3)]

        def sb_attention():
            with ExitStack() as st:
                NB = 2
                msk_sb = T("msk_sb", [128, 4, 512], BF16, st); mskB = Buf("msk")
                memset('pool', msk_sb[:], 1.0, [mskB])
                for r in range(4):
                    def f(e, r=r):
                        return e.affine_select(out=msk_sb[:, r, :], in_=msk_sb[:, r, :], pattern=[[1, 512]], compare_op=ALU.is_gt, fill=0.0, base=-128 * r, channel_multiplier=-1)
                    S.add('pool', f, R=[mskB], W=[mskB])
                mneg = T("mneg_sb", [128, 4, 512], BF16, st); mnegB = Buf("mneg")
                ts('dve', mneg[:].rearrange("p r t -> p (r t)"), msk_sb[:].rearrange("p r t -> p (r t)"), 240.0, ALU.mult, [mskB], [mnegB], s2=-240.0, op1=ALU.add)
                qz_r = Ring([([T("sbq%d_%d" % (i, z), [128, SEQ], BF16, st) for z in range(2)], Buf("sbq%d" % i)) for i in range(NB)])
                k_r = Ring([(T("sbk%d" % i, [128, SEQ], BF16, st), Buf("sbk%d" % i)) for i in range(NB)])
                v_r = Ring([(T("sbvp%d" % i, [128, NT, 2, 128], BF16, st), Buf("sbvp%d" % i)) for i in range(NB)])
                e_r = Ring([(T("sbe%d" % i, [128, 512], F32, st), Buf("sbe%d" % i)) for i in range(2)])
                sp_r = Ring([(T("sbsp%d" % i, [128, 512], F32, st), Buf("sbsp%d" % i)) for i in range(4)])
                tm_r = Ring([(T("sbtm%d" % i, [128, 512], F32, st), Buf("sbtm%d" % i)) for i in range(2)])
                pb_r = Ring([(T("sbpb%d" % i, [128, 512], BF16, st), Buf("sbpb%d" % i)) for i in range(3)])
                wb_r = Ring([(T("sbwb%d" % i, [128, 512], BF16, st), Buf("sbwb%d" % i)) for i in range(3)])
                ra_r = Ring([(T("sbra%d" % i, [128, 512], BF16, st), Buf("sbra%d" % i)) for i in range(3)])
                pz_r = Ring(PI[0:3]); pc_r = Ring(PI[3:6]); po_r = Ring(PI[6:8])
                scale = 1.0 / 8.0
                heads = {}

                def load_hp(hp):
                    qz, qB = qz_r.next(); kt, kB = k_r.next(); vp, vB = v_r.next()
                    memset('pool', qz[0][64:128, :], 0.0, [qB]); memset('pool', qz[1][0:64, :], 0.0, [qB])
                    S.dma('sp', qz[0][0:64, :], sbqk_d[hp][0:64, :], R=[sbqk_B[hp]], W=[qB])
                    S.dma('sp', qz[1][64:128, :], sbqk_d[hp][64:128, :], R=[sbqk_B[hp]], W=[qB])
                    S.dma('sp', kt[:], sbqk_d[4 + hp], R=[sbqk_B[4 + hp]], W=[kB])
                    memset('pool', vp[:, :, 0, 64:128], 0.0, [vB]); memset('pool', vp[:, :, 1, 0:64], 0.0, [vB])
                    S.dma('sp', vp[:, :, 0, 0:64], sbv_d[:, hp * 128:hp * 128 + 64].rearrange("(t p) d -> p t d", p=128), R=[sbv_B], W=[vB])
                    S.dma('sp', vp[:, :, 1, 64:128], sbv_d[:, hp * 128 + 64:hp * 128 + 128].rearrange("(t p) d -> p t d", p=128), R=[sbv_B], W=[vB])
                    heads[hp] = (qz, qB, kt, kB, vp, vB)

                items = []
                for hp in range(4):
                    for z in range(2):
                        for c in range(NCH):
                            nblk = (c + 1) * QPC
                            grp_ = {}
                            for bi, sb in enumerate(range(nblk - 1, -1, -1)):
                                items.append(dict(hp=hp, z=z, c=c, sb=sb, bi=bi, nblk=nblk, g=grp_, idx=len(items)))
                first_idx = {}
                for it in items:
                    first_idx.setdefault(it['hp'], it['idx'])

                def s0(x):
                    if x['idx'] == 0:
                        load_hp(0)
                    if x['idx'] == first_idx[x['hp']] + 3 and x['hp'] + 1 < 4:
                        load_hp(x['hp'] + 1)
                    qz, qB, kt, kB, vp, vB = heads[x['hp']]
                    c, sb, z = x['c'], x['sb'], x['z']
                    x['pz'], x['pzB'] = pz_r.next()
                    e_, eB = e_r.next()
                    x['sp'], x['spB'] = sp_r.next()
                    diag = sb >= c * QPC
                    mm(x['pz'][:, 0:CH], kt[:, sb * 128:(sb + 1) * 128], qz[z][:, c * CH:(c + 1) * CH], True, not diag, [kB, qB], [x['pzB']])
                    if diag:
                        wd_ = 128 * (sb - c * QPC + 1)
                        mm(x['pz'][:, 0:wd_], identb[:], mneg[:, sb - c * QPC, 0:wd_], False, True, [identbB, mnegB], [x['pzB']])
                    act(e_[:, 0:CH], x['pz'][:, 0:CH], AF.Exp, [x['pzB']], [eB], scale=-scale)
                    act(x['sp'][:, 0:CH], e_[:, 0:CH], AF.Ln, [eB], [x['spB']], bias=1.0)

                def s1(x):
                    c, sb, bi, g = x['c'], x['sb'], x['bi'], x['g']
                    diag = sb >= c * QPC
                    r = sb - c * QPC
                    x['pb'], x['pbB'] = pb_r.next()
                    x['pc'], x['pcB'] = pc_r.next()
                    pb, pbB, pc, pcB = x['pb'], x['pbB'], x['pc'], x['pcB']
                    stt(pb[:, 0:CH], x['pz'][:, 0:CH], scale, x['sp'][:, 0:CH], ALU.mult, ALU.add, [x['pzB'], x['spB']], [pbB])
                    if diag:
                        wd_ = 128 * (r + 1)
                        tt('dve', pb[:, 0:wd_], pb[:, 0:wd_], msk_sb[:, r, 0:wd_], ALU.mult, [pbB, mskB], [pbB])
                    mm(pc[:, 0:CH], upper_b[:], pb[:, 0:CH], True, bi == 0, [upperB, pbB], [pcB])
                    if bi > 0:
                        ra, raB = g['ra']
                        mm(pc[:, 0:CH], ones_b[:], ra[:, 0:CH], False, True, [onesB, raB], [pcB])
                    if bi < x['nblk'] - 1:
                        ran, ranB = ra_r.next()
                        if bi == 0:
                            cp('pool', ran[:, 0:CH], pb[:, 0:CH], [pbB], [ranB])
                        else:
                            ra, raB = g['ra']
                            tt('pool', ran[:, 0:CH], ra[:, 0:CH], pb[:, 0:CH], ALU.add, [raB, pbB], [ranB])
                        g['ra'] = (ran, ranB)

                def s2(x):
                    c, sb = x['c'], x['sb']
                    diag = sb >= c * QPC
                    r = sb - c * QPC
                    tm, tmB = tm_r.next()
                    x['wb'], x['wbB'] = wb_r.next()
                    tt('dve', tm[:, 0:CH], x['pc'][:, 0:CH], x['sp'][:, 0:CH], ALU.add, [x['pcB'], x['spB']], [tmB])
                    act(x['wb'][:, 0:CH], tm[:, 0:CH], AF.Exp, [tmB], [x['wbB']], scale=-1.0)

                def s3(x):
                    qz, qB, kt, kB, vp, vB = heads[x['hp']]
                    c, sb, z, bi, g, hp = x['c'], x['sb'], x['z'], x['bi'], x['g'], x['hp']
                    if bi == 0:
                        g['po'] = po_r.next()
                    po, poB = g['po']
                    mm(po[:, 0:CH], vp[:, sb, z, :], x['wb'][:, 0:CH], bi == 0, bi == x['nblk'] - 1, [vB, x['wbB']], [poB])
                    if bi == x['nblk'] - 1:
                        po_ = 64 * z
                        cp('act', oT[0][po_:po_ + 64, hp, c * CH:(c + 1) * CH], po[po_:po_ + 64, 0:CH], [poB], [oTB[0][hp]])

                stages = [s0, s1, s2, s3]
                for k in range(len(items) + len(stages) - 1):
                    for j, stg_f in enumerate(stages):
                        ii = k - j
                        if 0 <= ii < len(items):
                            stg_f(items[ii])
                S.barrier()
        sb_attention()
        dump("o_sbT%d" % l, oT[0][:].rearrange("p j t -> p (j t)"), [128, 4 * SEQ], BF16)
        if stop == 'BSB':
            stB.close()
            return False

        def softmax_attn(tag, nheads_pair_iter):
            pass

        with ExitStack() as st:
            NB = 2
            q_r = Ring([(T("mq%d" % i, [128, SEQ], BF16, st), Buf("mq%d" % i)) for i in range(NB)])
            k_r = Ring([(T("mk%d" % i, [128, SEQ], BF16, st), Buf("mk%d" % i)) for i in range(NB)])
            v_r = Ring([(T("mv%d" % i, [128, NT, 66], BF16, st), Buf("mv%d" % i)) for i in range(NB)])
            E_r = Ring([(T("mE%d" % i, [128, 512], BF16, st), Buf("mE%d" % i)) for i in range(4)])
            op_r = Ring([(T("mop%d" % i, [128, NT, 128], F32, st), Buf("mop%d" % i)) for i in range(2)])
            rc_r = Ring([(T("mrc%d" % i, [128, 1], F32, st), Buf("mrc%d" % i)) for i in range(4)])
            pz_r = Ring([PI[0], PI[1], PI[6]]); pt_r = Ring(PI[7:8])
            heads = {}
            ops_ = {}

            def load_h(h):
                qT, qB = q_r.next(); kT, kB = k_r.next(); vh, vB = v_r.next()
                S.dma('sp', qT[0:96, :], mq_d[h], R=[mq_B], W=[qB])
                S.dma('sp', kT[0:96, :], mk_d[h], R=[mk_B], W=[kB])
                S.dma('sp', vh[:], mv_d[:, h * 66:(h + 1) * 66].rearrange("(t p) e -> p t e", p=128), R=[mv_B], W=[vB])
                heads[h] = (qT, qB, kT, kB, vh, vB)

            items = []
            for h in range(8):
                for c in range(NCH):
                    n_sb = (c + 1) * QPC
                    for sb in range(n_sb):
                        items.append(dict(h=h, c=c, sb=sb, last=(sb == n_sb - 1), idx=len(items)))
            first_idx = {}
            for it in items:
                first_idx.setdefault(it['h'], it['idx'])

            def s0(x):
                h, c, sb = x['h'], x['c'], x['sb']
                if x['idx'] == 0:
                    load_h(0)
                if x['idx'] == first_idx[h] + 2 and h + 1 < 8:
                    load_h(h + 1)
                qT, qB, kT, kB, vh, vB = heads[h]
                q0 = max(0, sb - c * QPC); f0 = q0 * 128
                x['pz'], x['pzB'] = pz_r.next()
                mm(x['pz'][:, f0:CH], kT[0:96, sb * 128:(sb + 1) * 128], qT[0:96, c * CH + f0:(c + 1) * CH], True, True, [kB, qB], [x['pzB']])

            def s1(x):
                c, sb = x['c'], x['sb']
                q0 = max(0, sb - c * QPC); f0 = q0 * 128
                x['E'], x['EB'] = E_r.next()
                E, EB = x['E'], x['EB']
                act(E[:, f0:CH], x['pz'][:, f0:CH], AF.Exp, [x['pzB']], [EB])
                if sb >= c * QPC:
                    tt('pool', E[:, f0:f0 + 128], E[:, f0:f0 + 128], tri_b[:], ALU.mult, [EB, triB], [EB])

            def s2(x):
                h, c, sb = x['h'], x['c'], x['sb']
                hp, z = h // 2, h % 2
                qT, qB, kT, kB, vh, vB = heads[h]
                q0 = max(0, sb - c * QPC)
                E, EB = x['E'], x['EB']
                for q in range(q0, QPC):
                    qb = c * QPC + q
                    mm(PS[2 + q][:, 0:66], E[:, q * 128:(q + 1) * 128], vh[:, sb, :], sb == 0, sb == qb, [EB, vB], [PB[2 + q]])
                if x['last']:
                    if z == 0 and c == 0:
                        ops_[hp] = op_r.next()
                    op, opB = ops_[hp]
                    for q in range(QPC):
                        qb = c * QPC + q
                        rc, rcB = rc_r.next()
                        S.add('dve', (lambda o_, i_: (lambda e: e.reciprocal(out=o_, in_=i_)))(rc[:], PS[2 + q][:, 64:65]), R=[PB[2 + q]], W=[rcB])
                        ts('dve', op[:, qb, z * 64:(z + 1) * 64], PS[2 + q][:, 0:64], rc[:, 0:1], ALU.mult, [PB[2 + q], rcB], [opB])
                    if z == 1 and c == NCH - 1:
                        for g4 in range(NT // 4):
                            pt, ptB = pt_r.next()
                            for i4 in range(4):
                                tr(pt[:, i4 * 128:(i4 + 1) * 128], op[:, g4 * 4 + i4, :], [opB], [ptB])
                            cp('act', oT[1][:, hp, g4 * 512:(g4 + 1) * 512], pt[:, 0:512], [ptB], [oTB[1][hp]])

            stages = [s0, s1, s2]
            for k in range(len(items) + len(stages) - 1):
                for j, stg_f in enumerate(stages):
                    ii = k - j
                    if 0 <= ii < len(items):
                        stg_f(items[ii])
            S.barrier()
        dump("o_mlaT%d" % l, oT[1][:].rearrange("p j t -> p (j t)"), [128, 4 * SEQ], BF16)
        if stop == 'BMLA':
            stB.close()
            return False

        with ExitStack() as st:
            posrow = T("posrow", [128, SEQ], F32, st); prwB = Buf("posrow")
            with ExitStack() as st2:
                pri = T("posrow_i", [128, SEQ], I32, st2); priB = Buf("posrow_i")
                S.dma('sp', pri[:], pos_d.partition_broadcast(128), W=[priB])
                cp('dve', posrow[:], pri[:], [priB], [prwB])
                S.barrier()
            negpos = T("negpos", [128, NT], F32, st)
            ts('dve', negpos[:], pos_tok[:], -1.0, ALU.mult, [posB], [prwB])
            lp = T("dlp", [128, 256], F32, st); lpB = Buf("dlp")
            lam = T("dlam", [128, 4], F32, st); lamB = Buf("dlam")
            prd = T("dprd", [128, 128], F32, st); prdB = Buf("dprd")
            S.dma('sp', lp[:], Wd['diff_lambda'][l].rearrange("a d -> (a d)").partition_broadcast(128), W=[lpB])
            tt('dve', prd[:, 0:64], lp[:, 0:64], lp[:, 64:128], ALU.mult, [lpB], [prdB])
            tt('dve', prd[:, 64:128], lp[:, 128:192], lp[:, 192:256], ALU.mult, [lpB], [prdB])
            red(lam[:, 0:2], prd[:].rearrange("p (a d) -> p a d", d=64), [prdB], [lamB])
            act(lam[:, 0:2], lam[:, 0:2], AF.Exp, [lamB], [lamB])
            tt('dve', lam[:, 2:3], lam[:, 1:2], lam[:, 0:1], ALU.subtract, [lamB], [lamB])
            ts('dve', lam[:, 3:4], lam[:, 2:3], -lam_init, ALU.add, [lamB], [lamB])
            sg = T("dsg", [128, 2], F32, st); sgB = Buf("dsg")
            S.dma('sp', sg[:, 0:1], Wd['diff_subln'][l].rearrange("(p o) -> p o", o=1), W=[sgB])
            ts('dve', sg[:, 1:2], sg[:, 0:1], 1.0 - lam_init, ALU.mult, [sgB], [sgB])
            NB = 2
            qz_r = Ring([([T("dq%d_%d" % (i, z), [128, SEQ], BF16, st) for z in range(2)], Buf("dq%d" % i)) for i in range(NB)])
            k_r = Ring([(T("dk%d" % i, [128, SEQ], BF16, st), Buf("dk%d" % i)) for i in range(NB)])
            v_r = Ring([(T("dv%d" % i, [128, NT, 130], BF16, st), Buf("dv%d" % i)) for i in range(NB)])
            E_r = Ring([(T("dE%d" % i, [128, 512], BF16, st), Buf("dE%d" % i)) for i in range(4)])
            dt_r = Ring([(T("ddt%d" % i, [128, 512], F32, st), Buf("ddt%d" % i)) for i in range(3)])
            zz_r = Ring([(T("dzz%d" % i, [128, 512], F32, st), Buf("dzz%d" % i)) for i in range(3)])
            o1_r = Ring([(T("do1%d" % i, [128, QPC, 128], F32, st), Buf("do1%d" % i)) for i in range(2)])
            od_r = Ring([(T("dod%d" % i, [128, 128], F32, st), Buf("dod%d" % i)) for i in range(6)])
            on_r = Ring([(T("don%d" % i, [128, 128], F32, st), Buf("don%d" % i)) for i in range(3)])
            jk_r = Ring([(T("djk%d" % i, [128, 128], BF16, st), Buf("djk%d" % i)) for i in range(2)])
            rc_r = Ring([(T("drc%d" % i, [128, 4], F32, st), Buf("drc%d" % i)) for i in range(8)])
            pz_r = Ring([PI[0], PI[1], PI[6]]); pt_r = Ring(PI[7:8])
            heads = {}

            def load_h(h):
                qz, qB = qz_r.next(); kp, kB = k_r.next(); vh, vB = v_r.next()
                memset('pool', qz[0][64:128, :], 0.0, [qB]); memset('pool', qz[1][0:64, :], 0.0, [qB])
                S.dma('sp', qz[0][0:64, :], dq_d[h][0:64, :], R=[dq_B], W=[qB])
                S.dma('sp', qz[1][64:128, :], dq_d[h][64:128, :], R=[dq_B], W=[qB])
                S.dma('sp', kp[:], dk_d[h], R=[dk_B], W=[kB])
                S.dma('sp', vh[:], dv_d[:, h * 130:(h + 1) * 130].rearrange("(t p) e -> p t e", p=128), R=[dv_B], W=[vB])
                heads[h] = (qz, qB, kp, kB, vh, vB)

            CD = 256; QD = 2; NCD = SEQ // CD
            items = []
            for h in range(4):
                for c in range(NCD):
                    n_sb = (c + 1) * QD
                    for sb in range(n_sb):
                        items.append(dict(h=h, c=c, sb=sb, last=(sb == n_sb - 1), idx=len(items)))
            first_idx = {}
            for it in items:
                first_idx.setdefault(it['h'], it['idx'])

            def s0(x):
                h, c, sb = x['h'], x['c'], x['sb']
                if x['idx'] == 0:
                    load_h(0)
                if x['idx'] == first_idx[h] + 2 and h + 1 < 4:
                    load_h(h + 1)
                qz, qB, kp, kB, vh, vB = heads[h]
                q0 = max(0, sb - c * QD); f0 = q0 * 128
                x['pz'], x['pzB'] = pz_r.next()
                x['dt'], x['dtB'] = dt_r.next()
                for m in range(2):
                    mm(x['pz'][:, m * CD + f0:(m + 1) * CD], kp[:, sb * 128:(sb + 1) * 128], qz[m][:, c * CD + f0:(c + 1) * CD], True, True, [kB, qB], [x['pzB']])
                act(x['dt'][:, f0:CD], posrow[:, c * CD + f0:(c + 1) * CD], AF.Abs, [prwB], [x['dtB']], bias=negpos[:, sb:sb + 1])

            def s1(x):
                h, c, sb = x['h'], x['c'], x['sb']
                slope = 2.0 ** (-8.0 * (h + 1) / 4.0)
                q0 = max(0, sb - c * QD); f0 = q0 * 128
                zz, zzB = zz_r.next()
                x['E'], x['EB'] = E_r.next()
                E, EB = x['E'], x['EB']
                for m in range(2):
                    stt(zz[:, m * CD + f0:(m + 1) * CD], x['dt'][:, f0:CD], -slope, x['pz'][:, m * CD + f0:(m + 1) * CD], ALU.mult, ALU.add, [x['dtB'], x['pzB']], [zzB])
                if f0 == 0:
                    act(E[:, 0:2 * CD], zz[:, 0:2 * CD], AF.Exp, [zzB], [EB])
                else:
                    for m in range(2):
                        act(E[:, m * CD + f0:(m + 1) * CD], zz[:, m * CD + f0:(m + 1) * CD], AF.Exp, [zzB], [EB])
                if sb >= c * QD:
                    for m in range(2):
                        tt('pool', E[:, m * CD + f0:m * CD + f0 + 128], E[:, m * CD + f0:m * CD + f0 + 128], tri_b[:], ALU.mult, [EB, triB], [EB])

            def s2(x):
                h, c, sb = x['h'], x['c'], x['sb']
                qz, qB, kp, kB, vh, vB = heads[h]
                q0 = max(0, sb - c * QD)
                E, EB = x['E'], x['EB']
                for q in range(q0, QD):
                    qb = c * QD + q
                    for m in range(2):
                        bi_ = 2 + q * 2 + m
                        mm(PS[bi_][:, 0:130], E[:, m * CD + q * 128:m * CD + (q + 1) * 128], vh[:, sb, :], sb == 0, sb == qb, [EB, vB], [PB[bi_]])
                if not x['last']:
                    return
                o1, o1B = o1_r.next()
                fin = []
                for q in range(QD):
                    qb = c * QD + q
                    b0 = 2 + q * 2; b1 = b0 + 1
                    rc, rcB = rc_r.next()
                    S.add('dve', (lambda o_, i_: (lambda e: e.reciprocal(out=o_, in_=i_)))(rc[:, 0:1], PS[b0][:, 128:129]), R=[PB[b0]], W=[rcB])
                    S.add('dve', (lambda o_, i_: (lambda e: e.reciprocal(out=o_, in_=i_)))(rc[:, 1:2], PS[b1][:, 128:129]), R=[PB[b1]], W=[rcB])
                    ts('dve', o1[:, q, :], PS[b0][:, 0:128], rc[:, 0:1], ALU.mult, [PB[b0], rcB], [o1B])
                    od, odB = od_r.next(); jk, jkB = jk_r.next()
                    tt('dve', rc[:, 1:2], rc[:, 1:2], lam[:, 3:4], ALU.mult, [rcB, lamB], [rcB])
                    stt(od[:], PS[b1][:, 0:128], rc[:, 1:2], o1[:, q, :], ALU.mult, ALU.add, [PB[b1], rcB, o1B], [odB])
                    act(jk[:], od[:], AF.Square, [odB], [jkB, rcB], accum_out=rc[:, 2:3])
                    fin.append((qb, rc, rcB, od, odB))

                def p1(fin=fin):
                    for (qb, rc, rcB, od, odB) in fin:
                        ts('dve', rc[:, 3:4], rc[:, 2:3], 1.0 / 128, ALU.mult, [rcB], [rcB], s2=EPS, op1=ALU.add)
                        act(rc[:, 3:4], rc[:, 3:4], AF.Ln, [rcB], [rcB])
                        act(rc[:, 3:4], rc[:, 3:4], AF.Exp, [rcB], [rcB], scale=-0.5)

                def p2(fin=fin, h=h, c=c):
                    pt, ptB = pt_r.next()
                    for i2, (qb, rc, rcB, od, odB) in enumerate(fin):
                        on, onB = on_r.next()
                        ts('dve', on[:], od[:], rc[:, 3:4], ALU.mult, [odB, rcB], [onB])
                        tr(pt[:, i2 * 128:(i2 + 1) * 128], on[:], [onB], [ptB])
                    act(oT[2][:, h, c * CD:(c + 1) * CD], pt[:, 0:CD], AF.Identity, [ptB, sgB], [oTB[2][h]], scale=sg[:, 1:2])
                deferred.append((cur_step[0] + 1, p1)); deferred.append((cur_step[0] + 2, p2))

            deferred = []
            cur_step = [0]
            stages = [s0, s1, s2]
            for k in range(len(items) + len(stages) - 1):
                cur_step[0] = k
                for j, stg_f in enumerate(stages):
                    ii = k - j
                    if 0 <= ii < len(items):
                        stg_f(items[ii])
                due = [d_ for d_ in deferred if d_[0] <= k]
                deferred[:] = [d_ for d_ in deferred if d_[0] > k]
                for _, fn_ in due:
                    fn_()
            for _, fn_ in deferred:
                fn_()
            S.barrier()
        dump("o_diffT%d" % l, oT[2][:].rearrange("p j t -> p (j t)"), [128, 4 * SEQ], BF16)
        if stop == 'BDIFF':
            stB.close()
            return False

        with ExitStack() as st:
            wbr = T("wbr", [128, 12, D], BF16, st); wbrB = Buf("wbr")
            wbsrc = Wd['w_branch'][l].rearrange("n k d -> (n k) d")
            for i in range(3):
                S.dma('pool', wbr[:, i * 4:(i + 1) * 4, :], wbsrc[i * 512:(i + 1) * 512, :].rearrange("(j p) n -> p j n", p=128), W=[wbrB])
            wo = T("wo", [128, 8, D], BF16, st); woB = Buf("wo")
            for i in range(2):
                S.dma('pool', wo[:, i * 4:(i + 1) * 4, :], Wd['w_out'][l][i * 512:(i + 1) * 512, :].rearrange("(j p) n -> p j n", p=128), W=[woB])
            for j in range(8):
                tt('pool' if j % 2 else 'dve', wo[:, j, :], wo[:, j, :], g1g2[:, 0, :], ALU.mult, [woB, g12B], [woB])
            gts_r = Ring([(T("gts%d" % i, [128, 3, CH], BF16, st), Buf("gts%d" % i)) for i in range(3)])
            M_r = Ring([(T("mM%d" % i, [128, 8, CH], BF16, st), Buf("mM%d" % i)) for i in range(2)])
            tn_r = Ring([(T("mtn%d" % i, [128, 512], F32, st), Buf("mtn%d" % i)) for i in range(6)])
            gt_v = gt_d.rearrange("(n j) p t -> j p n t", n=3)
            pr = Ring(PI)
            for c in range(NCH):
                M, MB = M_r.next()
                for j in range(8):
                    gts, gtsB = gts_r.next()
                    S.dma('sp', gts[:], gt_v[j][:, :, c * CH:(c + 1) * CH], R=[gt_B], W=[gtsB])
                    tn = []
                    for n in range(3):
                        py, pyB = pr.next()
                        for kc in range(4):
                            mm(py[:, 0:CH], wbr[:, n * 4 + kc, j * 128:(j + 1) * 128], oT[n][:, kc, c * CH:(c + 1) * CH], kc == 0, kc == 3, [wbrB, oTB[n][kc]], [pyB])
                        t_, tB_ = tn_r.next()
                        tt('dve', t_[:, 0:CH], py[:, 0:CH], gts[:, n, :], ALU.mult, [pyB, gtsB], [tB_])
                        tn.append((t_, tB_))
                    tt('dve', tn[0][0][:, 0:CH], tn[0][0][:, 0:CH], tn[1][0][:, 0:CH], ALU.add, [tn[0][1], tn[1][1]], [tn[0][1]])
                    tt('pool', M[:, j, :], tn[0][0][:, 0:CH], tn[2][0][:, 0:CH], ALU.add, [tn[0][1], tn[2][1]], [MB])
                for q in range(QPC):
                    t = c * QPC + q
                    for half in range(2):
                        px, pxB = pr.next()
                        for j in range(8):
                            mm(px[:, :], M[:, j, q * 128:(q + 1) * 128], wo[:, j, half * 512:(half + 1) * 512], j == 0, j == 7, [MB, woB], [pxB])
                        tt('dve', xs[:, t, half * 512:(half + 1) * 512], px[:, :], xs[:, t, half * 512:(half + 1) * 512], ALU.add, [pxB, xB[t]], [xB[t]])
            S.barrier()
        stB.close()
        dump("xmid%d" % l, xs[:].rearrange("p t d -> p (t d)"), [128, NT * D])
        if stop == 'C':
            return False
        stA = ExitStack()
        hT = T("hT2", [128, 8, SEQ], BF16, stA)

        moe = (l % 2 == 1)
        jl = l // 2
        with ExitStack() as st:
            router = None
            if moe:
                wr = T("wr", [128, 8, 8], F32, st); wrB = Buf("wr")
                wr2 = T("wr2", [128, 8, 8], F32, st); wr2B = Buf("wr2")
                bbc = T("bbc", [128, 8, 128], F32, st); bbcB = Buf("bbc")
                brB_t = T("brB", [128, 8], F32, st); brBB = Buf("brB")
                S.dma('sp', wr[:], Wd['w_router'][jl].rearrange("(j p) e -> p j e", p=128), W=[wrB])
                tt('dve', wr2[:], wr[:], ABT[:, 8:16].unsqueeze(2).broadcast_to([128, 8, 8]), ALU.mult, [wrB, ABTB], [wr2B])
                cp('dve', bbc[:], modT[:, 16:24].unsqueeze(2).broadcast_to([128, 8, 128]), [modTB], [bbcB])
                pl, plB = PI[4]
                for j in range(8):
                    mm(pl[:, 0:8], bbc[:, j, :], wr[:, j, :], j == 0, j == 7, [bbcB, wrB], [plB])
                cp('dve', brB_t[:], pl[:, 0:8], [plB], [brBB])
                xt_r = Ring([(T("rxt%d" % i, [128, 8, 128], F32, st), Buf("rxt%d" % i)) for i in range(2)])
                rt_r = Ring([(T("rrt%d" % i, [128, 48], F32, st), Buf("rrt%d" % i)) for i in range(2)])
                prr = Ring(PI[4:6])

                def router(t, pa, paB, pb, pbB, aoff, boff):
                    xt, xtB = xt_r.next(); rt, rtB = rt_r.next()
                    cp('dve', xt[:, 0:4, :].rearrange("p j t -> p (j t)"), pa[:, :], [paB], [xtB])
                    cp('act', xt[:, 4:8, :].rearrange("p j t -> p (j t)"), pb[:, :], [pbB], [xtB])
                    pl_, plB_ = prr.next()
                    for j in range(8):
                        mm(pl_[:, 0:8], xt[:, j, :], wr2[:, j, :], j == 0, j == 7, [xtB, wr2B], [plB_])
                    lg = rt[:, 0:8]; mx = rt[:, 8:16]; mk = rt[:, 16:24]; ex = rt[:, 24:32]
                    tt('dve', lg, pl_[:, 0:8], brB_t[:], ALU.add, [plB_, brBB], [rtB])
                    S.add('dve', (lambda o_, i_: (lambda e: e.max(out=o_, in_=i_)))(mx, lg), R=[rtB], W=[rtB])
                    ts('dve', mk, lg, rt[:, 9:10], ALU.is_ge, [rtB], [rtB])
                    ts('dve', rt[:, 32:33], rt[:, 8:9], -1.0, ALU.mult, [rtB], [rtB])
                    act(ex, lg, AF.Exp, [rtB], [rtB], bias=rt[:, 32:33])
                    tt('dve', ex, ex, mk, ALU.mult, [rtB], [rtB])
                    red(rt[:, 33:34], ex, [rtB], [rtB])
                    S.add('dve', (lambda o_, i_: (lambda e: e.reciprocal(out=o_, in_=i_)))(rt[:, 34:35], rt[:, 33:34]), R=[rtB], W=[rtB])
                    ts('dve', comb[:, t, :], ex, rt[:, 34:35], ALU.mult, [rtB], [combB])
            norm_phase(2, hT, hB, st, router)
            S.barrier()
        dump("h2T%d" % l, hT[:].rearrange("p j t -> p (j t)"), [128, 8 * SEQ], BF16)
        dump("comb%d" % l, comb[:].rearrange("p t e -> p (t e)"), [128, NT * 8])

        with ExitStack() as st:
            if moe:
                units = [(Wd['w_exp_gate'][jl][e], Wd['w_exp_up'][jl][e], Wd['w_exp_down'][jl][e], e) for e in range(8)]
            else:
                units = [(Wd['w_ffn_gate'][jl], Wd['w_ffn_up'][jl], Wd['w_ffn_down'][jl], None)]
            groups = [(0, 4), (4, 4), (8, 4), (12, 4), (16, 4), (20, 2)]
            wg_r = Ring([(T("fwg%d" % i, [128, 8, 512], BF16, st), Buf("fwg%d" % i)) for i in range(2)])
            wu_r = Ring([(T("fwu%d" % i, [128, 8, 512], BF16, st), Buf("fwu%d" % i)) for i in range(2)])
            wd_r = Ring([(T("fwd%d" % i, [128, 4, D], BF16, st), Buf("fwd%d" % i)) for i in range(2)])
            aT_r = Ring([(T("faT%d" % i, [128, 4, SEQ], BF16, st), Buf("faT%d" % i)) for i in range(2)])
            sg_r = Ring([(T("fsg%d" % i, [128, 512], F32, st), Buf("fsg%d" % i)) for i in range(3)])
            pr = Ring(PI)
            for (wg2, wu2, wd2, e) in units:
                for (f0, nf) in groups:
                    wg, wgB = wg_r.next(); wu, wuB = wu_r.next(); wd_, wdB = wd_r.next(); aT, aTB = aT_r.next()
                    load_w(wg, wgB, wg2, 0, 8, f0 * 128, nf * 128)
                    load_w(wu, wuB, wu2, 0, 8, f0 * 128, nf * 128)
                    S.dma('pool', wd_[:, 0:nf, :], wd2[f0 * 128:(f0 + nf) * 128, :].rearrange("(j p) n -> p j n", p=128), W=[wdB])
                    for fc in range(nf):
                        tt('pool', wd_[:, fc, :], wd_[:, fc, :], g1g2[:, 1, :], ALU.mult, [wdB, g12B], [wdB])
                    for fc in range(nf):
                        for c in range(NCH):
                            pg, pgB = pr.next(); pu, puB = pr.next()
                            proj_T(pg, pgB, hT, hB, wg, wgB, fc * 128, 128, c)
                            proj_T(pu, puB, hT, hB, wu, wuB, fc * 128, 128, c)
                            sg_, sgB_ = sg_r.next()
                            act(sg_[:, 0:CH], pg[:, 0:CH], AF.Silu, [pgB], [sgB_])
                            tt('dve', aT[:, fc, c * CH:(c + 1) * CH], sg_[:, 0:CH], pu[:, 0:CH], ALU.mult, [sgB_, puB], [aTB])
                    for t in range(NT):
                        for half in range(2):
                            px, pxB = pr.next()
                            for fc in range(nf):
                                mm(px[:, :], aT[:, fc, t * 128:(t + 1) * 128], wd_[:, fc, half * 512:(half + 1) * 512], fc == 0, fc == nf - 1, [aTB, wdB], [pxB])
                            xsl = xs[:, t, half * 512:(half + 1) * 512]
                            if e is None:
                                tt('dve', xsl, px[:, :], xsl, ALU.add, [pxB, xB[t]], [xB[t]])
                            else:
                                stt(xsl, px[:, :], comb[:, t, e:e + 1], xsl, ALU.mult, ALU.add, [pxB, combB, xB[t]], [xB[t]])
            S.barrier()
        stA.close()
        dump("xout%d" % l, xs[:].rearrange("p t d -> p (t d)"), [128, NT * D])

        return True

    for l in layers:
        if not layer_body(l):
            break

    g_out = S.grp("out_sp")
    S.barrier()
    for t in range(NT):
        S.dma('sp', y_d[t * 128:(t + 1) * 128, :], xs[:, t, :], R=[xB[t]], grp=g_out)
    S.barrier(['sp'])
    S.emit(nc, es)
    es.close()
    global LAST_S
    LAST_S = S
    return nc


LAST_S = None


_NC_CACHE = {}


def kernel(**inputs):
    B = 8
    SEQ = 2048
    if 'nc' not in _NC_CACHE:
        _NC_CACHE['nc'] = build(SEQ=SEQ, layers=(0, 1))
    nc = _NC_CACHE['nc']
    x = np.ascontiguousarray(inputs['x'], dtype=np.float32)
    c = np.ascontiguousarray(inputs['c'], dtype=np.float32)
    pos = np.ascontiguousarray(inputs['positions'], dtype=np.int32)
    wts = {n: np.ascontiguousarray(inputs[n], dtype=np.float32) for n in WNAMES}
    in_maps = []
    for b in range(B):
        m = {'x': x[b], 'c': c[b], 'positions': pos[b]}
        m.update(wts)
        in_maps.append(m)
    res = run_bass_kernel_spmd(nc, in_maps, core_ids=list(range(B)))
    return np.stack([np.asarray(r['y'], dtype=np.float32) for r in res.results], axis=0)
```

```python
import math
from contextlib import ExitStack
import numpy as np
import concourse.bass as bass
import concourse.mybir as mybir
from concourse.alu_op_type import AluOpType as ALU
from concourse.bass_utils import run_bass_kernel_spmd

F32 = mybir.dt.float32
BF16 = mybir.dt.bfloat16
I32 = mybir.dt.int32
AF = mybir.ActivationFunctionType
AX = mybir.AxisListType

ENGS = ['pe', 'act', 'dve', 'pool', 'sp']
SAME_ENG_SYNC = True


class Buf:
    __slots__ = ('name', 'w', 'r', 'sbuf', 'g')

    def __init__(self, name, sbuf=True):
        self.name = name
        self.w = None
        self.r = {}
        self.sbuf = sbuf
        self.g = None


class Grp:
    __slots__ = ('name', 'sem', 'count')

    def __init__(self, name):
        self.name = name
        self.sem = None
        self.count = 0


class Sched:
    def __init__(self):
        self.ops = {e: [] for e in ENGS}
        self.seen = {e: {} for e in ENGS}
        self.grps = []
        self.named = {}

    def grp(self, name):
        g = Grp(name)
        self.grps.append(g)
        return g

    def add(self, eng, fn, R=(), W=(), grp=None):
        idx = len(self.ops[eng])
        deps = []
        for b in R:
            if b.w is not None:
                deps.append(b.w)
        for b in W:
            if b.w is not None:
                deps.append(b.w)
            deps.extend(b.r.values())
        waits = {}
        seen = self.seen[eng]
        for d in deps:
            if d[0] == 'c':
                _, e2, i2 = d
                if e2 == eng and (eng == 'pe' or not SAME_ENG_SYNC):
                    continue
                key = ('c', e2)
                val = i2
            else:
                g = d[1]
                key = ('d', g)
                val = g.count
            if seen.get(key, -1) >= val:
                continue
            if waits.get(key, -1) < val:
                waits[key] = val
        for k, v in waits.items():
            seen[k] = v
            if k[0] == 'c':
                self.ops[k[1]][v]['sig'] = True
        if grp is not None:
            grp.count += 1
            tok = ('d', grp, grp.count)
            rkey = grp
        else:
            tok = ('c', eng, idx)
            rkey = eng
        self.ops[eng].append(dict(fn=fn, waits=waits, sig=False, grp=grp))
        for b in R:
            b.r[rkey] = tok
        for b in W:
            b.w = tok
            b.r = {}
        return tok

    def dma(self, q, out, in_, R=(), W=(), grp=None, **kw):
        if grp is None:
            owner = None
            for b in list(W) + list(R):
                if getattr(b, 'sbuf', False):
                    owner = b
                    break
            key = q
            if owner.g is None:
                owner.g = {}
            if key not in owner.g:
                gname = owner.name + '_' + q
                if gname not in self.named:
                    self.named[gname] = self.grp(gname)
                owner.g[key] = self.named[gname]
            grp = owner.g[key]
        return self.add(q, lambda e: e.dma_start(out=out, in_=in_, **kw), R=R, W=W, grp=grp)

    def last_compute(self, e):
        ops = self.ops[e]
        for i in range(len(ops) - 1, -1, -1):
            if ops[i]['fn'] is not None and ops[i]['grp'] is None:
                return i
        return None

    def barrier(self, engs=None):
        lasts = {e: self.last_compute(e) for e in ENGS}
        for e in (engs or ENGS):
            waits = {}
            seen = self.seen[e]
            for e2 in ENGS:
                if e2 == e or lasts[e2] is None:
                    continue
                if seen.get(('c', e2), -1) >= lasts[e2]:
                    continue
                waits[('c', e2)] = lasts[e2]
                self.ops[e2][lasts[e2]]['sig'] = True
            for g in self.grps:
                if g.count > 0 and seen.get(('d', g), -1) < g.count:
                    waits[('d', g)] = g.count
            for k, v in waits.items():
                seen[k] = v
            self.ops[e].append(dict(fn=None, waits=waits, sig=False, grp=None))

    def emit(self, nc, es):
        csem = {e: es.enter_context(nc.semaphore('c_' + e)) for e in ENGS}
        for g in self.grps:
            g.sem = es.enter_context(nc.semaphore('g_' + g.name))
        ordv = {}
        for e in ENGS:
            n = 0
            o = []
            for op in self.ops[e]:
                if op['sig'] and op['grp'] is None:
                    n += 1
                o.append(n)
            ordv[e] = o
        block = es.enter_context(nc.Block())
        ops = self.ops

        def run(e, eng):
            for op in ops[e]:
                for key, val in op['waits'].items():
                    if key[0] == 'c':
                        eng.wait_ge(csem[key[1]], ordv[key[1]][val])
                    else:
                        eng.wait_ge(key[1].sem, 16 * val)
                if op['fn'] is None:
                    continue
                inst = op['fn'](eng)
                if op['grp'] is not None:
                    inst.then_inc(op['grp'].sem, 16)
                elif op['sig']:
                    inst.then_inc(csem[e], 1)

        @block.tensor
        def _(eng):
            run('pe', eng)

        @block.scalar
        def _(eng):
            run('act', eng)

        @block.vector
        def _(eng):
            run('dve', eng)

        @block.gpsimd
        def _(eng):
            run('pool', eng)

        @block.sync
        def _(eng):
            run('sp', eng)


D = 1024
EPS = 1e-6
IN_COLS = 7200
C_SBQ, C_SBK, C_SBV = 0, 512, 1024
C_MLA = 1536
C_DQ, C_DK, C_DV = 2592, 3104, 3616
C_GATE = 4128
DFF = 2816
WNAMES = ['norm1_g', 'norm2_g', 'w_ada', 'b_ada', 'w_in', 'mla_q_norm', 'w_mla_uq', 'mla_kv_norm', 'w_mla_ukv',
          'mla_q_gain', 'mla_k_gain', 'diff_q_gain', 'diff_k_gain', 'diff_lambda', 'diff_subln', 'w_branch',
          'w_out', 'w_ffn_gate', 'w_ffn_up', 'w_ffn_down', 'w_router', 'w_exp_gate', 'w_exp_up', 'w_exp_down']
WSHAPES = {
    'norm1_g': [2, 1024], 'norm2_g': [2, 1024], 'w_ada': [2, 1024, 6144], 'b_ada': [2, 6144],
    'w_in': [2, 1024, 7200], 'mla_q_norm': [2, 768], 'w_mla_uq': [2, 768, 768], 'mla_kv_norm': [2, 256],
    'w_mla_ukv': [2, 256, 1024], 'mla_q_gain': [2, 96], 'mla_k_gain': [2, 96], 'diff_q_gain': [2, 64],
    'diff_k_gain': [2, 64], 'diff_lambda': [2, 4, 64], 'diff_subln': [2, 128], 'w_branch': [2, 3, 512, 1024],
    'w_out': [2, 1024, 1024], 'w_ffn_gate': [1, 1024, 2816], 'w_ffn_up': [1, 1024, 2816],
    'w_ffn_down': [1, 2816, 1024], 'w_router': [1, 1024, 8], 'w_exp_gate': [1, 8, 1024, 2816],
    'w_exp_up': [1, 8, 1024, 2816], 'w_exp_down': [1, 8, 2816, 1024],
}


class Ring:
    def __init__(self, items):
        self.items = items
        self.i = 0

    def next(self):
        it = self.items[self.i % len(self.items)]
        self.i += 1
        return it


def build(SEQ=2048, layers=(0, 1), stop=None, dbg=(), wshapes=None, mcut=99):
    NT = SEQ // 128
    CH = min(512, SEQ)
    NCH = SEQ // CH
    QPC = CH // 128
    nc = bass.Bass("TRN2", target_bir_lowering=False)
    S = Sched()
    es = ExitStack()
    dbg_out = {}

    x_d = nc.dram_tensor("x", [SEQ, D], F32, kind="ExternalInput").ap()
    c_d = nc.dram_tensor("c", [D], F32, kind="ExternalInput").ap()
    pos_d = nc.dram_tensor("positions", [SEQ], I32, kind="ExternalInput").ap()
    Wd = {n: nc.dram_tensor(n, (wshapes or WSHAPES)[n], F32, kind="ExternalInput").ap() for n in WNAMES}
    y_d = nc.dram_tensor("y", [SEQ, D], F32, kind="ExternalOutput").ap()

    def scratch(name, shape, dt=BF16):
        return nc.dram_tensor(name, shape, dt, kind="Internal").ap()
    sbqk_d = scratch("s_sbqk", [8, 128, SEQ]); sbqk_B = [Buf("s_sbqk%d" % i, False) for i in range(8)]
    sbv_d = scratch("s_sbv", [SEQ, 512]); sbv_B = Buf("s_sbv", False)
    mq_d = scratch("s_mq", [8, 96, SEQ]); mq_B = Buf("s_mq", False)
    mk_d = scratch("s_mk", [8, 96, SEQ]); mk_B = Buf("s_mk", False)
    mv_d = scratch("s_mv", [SEQ, 8 * 66]); mv_B = Buf("s_mv", False)
    dq_d = scratch("s_dq", [4, 128, SEQ]); dq_B = Buf("s_dq", False)
    dk_d = scratch("s_dk", [4, 128, SEQ]); dk_B = Buf("s_dk", False)
    dv_d = scratch("s_dv", [SEQ, 4 * 130]); dv_B = Buf("s_dv", False)
    gt_d = scratch("s_gt", [24, 128, SEQ]); gt_B = Buf("s_gt", False)

    uid = [0]

    def T(name, shape, dt, st=None):
        uid[0] += 1
        return (st or es).enter_context(nc.sbuf_tensor("%s_u%d" % (name, uid[0]), shape, dt))

    def act(out, in_, func, R, W, **kw):
        return S.add('act', lambda e: e.activation(out=out, in_=in_, func=func, **kw), R=R, W=W)

    def tt(eng, out, in0, in1, op, R, W):
        return S.add(eng, lambda e: e.tensor_tensor(out=out, in0=in0, in1=in1, op=op), R=R, W=W)

    def ts(eng, out, in0, s1, op0, R, W, s2=None, op1=None):
        if op1 is None:
            return S.add(eng, lambda e: e.tensor_scalar(out=out, in0=in0, scalar1=s1, scalar2=None, op0=op0), R=R, W=W)
        return S.add(eng, lambda e: e.tensor_scalar(out=out, in0=in0, scalar1=s1, scalar2=s2, op0=op0, op1=op1), R=R, W=W)

    def stt(out, in0, scalar, in1, op0, op1, R, W):
        return S.add('dve', lambda e: e.scalar_tensor_tensor(out=out, in0=in0, scalar=scalar, in1=in1, op0=op0, op1=op1), R=R, W=W)

    def mm(out, lhsT, rhs, start, stop, R, W):
        return S.add('pe', lambda e: e.matmul(out, lhsT=lhsT, rhs=rhs, start=start, stop=stop), R=R, W=W)

    def cp(eng, out, in_, R, W):
        if eng == 'act':
            return S.add(eng, lambda e: e.activation(out=out, in_=in_, func=AF.Copy), R=R, W=W)
        return S.add(eng, lambda e: e.tensor_copy(out=out, in_=in_), R=R, W=W)

    def memset(eng, ap, val, W):
        return S.add(eng, lambda e: e.memset(ap, val), W=W)

    def red(out, in_, R, W):
        return S.add('dve', lambda e: e.tensor_reduce(out=out, in_=in_, axis=AX.X, op=ALU.add), R=R, W=W)

    def dump(name, ap, shape, dt=F32):
        if name not in dbg:
            return
        S.barrier()
        d = nc.dram_tensor("dbg_" + name, shape, dt, kind="ExternalOutput").ap()
        dbg_out[name] = d
        S.add('sp', lambda e: e.dma_start(out=d, in_=ap), grp=g_dbg)
        S.barrier()

    g_dbg = S.grp("dbg_sp")
    xs = T("xs", [128, NT, D], F32); xB = [Buf("x%d" % t) for t in range(NT)]
    ident = T("ident", [128, 128], F32); identB = Buf("ident")
    tri_b = T("tri_b", [128, 128], BF16); triB = Buf("tri")
    ones_b = T("ones_b", [128, 128], BF16); onesB = Buf("ones")
    upper_b = T("upper_b", [128, 128], BF16); upperB = Buf("upper")
    pos_tok = T("pos_tok", [128, NT], F32); posB = Buf("pos")
    invf = T("invf", [128, 16], F32); invfB = Buf("invf")
    modT = T("modT", [128, 32], F32); modTB = Buf("modT")
    ABT = T("ABT", [128, 16], F32); ABTB = Buf("ABT")
    g1g2 = T("g1g2", [128, 2, D], F32); g12B = Buf("g1g2")
    comb = T("comb", [128, NT, 8], F32); combB = Buf("comb")
    identb = T("identb", [128, 128], BF16); identbB = Buf("identb")
    SC = T("SC", [128, NT, 32], F32); SCB = Buf("SC")
    PS = [es.enter_context(nc.psum_tensor("ps%d" % i, [128, 512], F32)) for i in range(8)]
    PB = [Buf("ps%d" % i) for i in range(8)]
    PI = list(zip(PS, PB))

    def tr(out, in_, R, W):
        return S.add('pe', lambda e: e.transpose(out, in_, ident[:in_.shape[0], :in_.shape[0]]), R=list(R) + [identB], W=W)

    def rsqrt(tag, out, in_, scale, R, W, st):
        n = in_.shape[1]
        v = T("rsq_" + tag, [128, n], F32, st); vB = Buf("rsq_" + tag)
        S.add('dve', lambda e: e.tensor_scalar(out=v[:], in0=in_, scalar1=scale, scalar2=EPS, op0=ALU.mult, op1=ALU.add), R=R, W=[vB])
        act(v[:], v[:], AF.Ln, [vB], [vB])
        act(out, v[:], AF.Exp, [vB], W, scale=-0.5)

    g_in = S.grp("in_sp")
    for t in range(NT):
        S.dma('sp', xs[:, t, :], x_d[t * 128:(t + 1) * 128, :], W=[xB[t]], grp=g_in)
    memset('pool', ident[:], 1.0, [identB])
    S.add('pool', lambda e: e.affine_select(out=ident[:], in_=ident[:], pattern=[[-1, 128]], compare_op=ALU.is_equal, fill=0.0, base=0, channel_multiplier=1), R=[identB], W=[identB])
    memset('pool', tri_b[:], 1.0, [triB])
    S.add('pool', lambda e: e.affine_select(out=tri_b[:], in_=tri_b[:], pattern=[[1, 128]], compare_op=ALU.is_ge, fill=0.0, base=0, channel_multiplier=-1), R=[triB], W=[triB])
    memset('pool', ones_b[:], 1.0, [onesB])
    memset('pool', upper_b[:], 1.0, [upperB])
    S.add('pool', lambda e: e.affine_select(out=upper_b[:], in_=upper_b[:], pattern=[[-1, 128]], compare_op=ALU.is_gt, fill=0.0, base=0, channel_multiplier=1), R=[upperB], W=[upperB])
    with ExitStack() as st:
        pi = T("pos_i", [128, NT], I32, st); tB = Buf("pos_i")
        S.dma('sp', pi[:], pos_d.rearrange("(t p) -> p t", p=128), W=[tB], grp=g_in, allow_slow_non_contiguous=True)
        cp('dve', pos_tok[:], pi[:], [tB], [posB])
        ii = T("iota_i", [128, 16], I32, st); iB = Buf("iota")
        S.add('pool', lambda e: e.iota(ii[:], pattern=[[1, 16]], base=0, channel_multiplier=0), W=[iB])
        cp('dve', invf[:], ii[:], [iB], [invfB])
        act(invf[:], invf[:], AF.Exp, [invfB], [invfB], scale=-math.log(10000.0) / 16.0)
        cp('dve', identb[:], ident[:], [identB], [identbB])
        TWO_PI_ = 2.0 * math.pi
        angA = T("angA", [128, NT, 16], F32, st); angB = Buf("angA")
        sct = T("sc_tmp", [128, NT * 32], F32, st); sctB = Buf("sc_tmp")
        sci = T("sc_ki", [128, NT * 32], I32, st); sciB = Buf("sc_ki")
        SC2 = SC[:].rearrange("p t s -> p (t s)")
        tt('dve', angA[:], invf[:].unsqueeze(1).broadcast_to([128, NT, 16]), pos_tok[:].unsqueeze(2).broadcast_to([128, NT, 16]), ALU.mult, [invfB, posB], [angB])
        ts('dve', SC[:, :, 0:16], angA[:], math.pi, ALU.add, [angB], [SCB])
        ts('dve', SC[:, :, 16:32], angA[:], 1.5 * math.pi, ALU.add, [angB], [SCB])
        ts('dve', sct[:], SC2, 1.0 / TWO_PI_, ALU.mult, [SCB], [sctB])
        cp('dve', sci[:], sct[:], [sctB], [sciB])
        cp('dve', sct[:], sci[:], [sciB], [sctB])
        stt(SC2, sct[:], -TWO_PI_, SC2, ALU.mult, ALU.add, [sctB, SCB], [SCB])
        ts('dve', sct[:], SC2, 0.0, ALU.is_lt, [SCB], [sctB])
        stt(SC2, sct[:], TWO_PI_, SC2, ALU.mult, ALU.add, [sctB, SCB], [SCB])
        ts('dve', SC2, SC2, -math.pi, ALU.add, [SCB], [SCB])
        ts('dve', SC2, SC2, -math.pi, ALU.max, [SCB], [SCB], s2=math.pi, op1=ALU.min)
        act(SC2, SC2, AF.Sin, [SCB], [SCB])
        S.barrier()

    def layer_body(l):
        lam_init = 0.8 - 0.6 * math.exp(-0.3 * l)
        w_in = Wd['w_in'][l]

        with ExitStack() as st:
            wa_r = Ring([(T("wa%d" % i, [128, 8, 512], F32, st), Buf("wa%d" % i)) for i in range(2)])
            bb_r = Ring([(T("bb%d" % i, [128, 512], F32, st), Buf("bb%d" % i)) for i in range(2)])
            mb_r = Ring([(T("mb%d" % i, [128, 512], F32, st), Buf("mb%d" % i)) for i in range(2)])
            gT = T("gT", [128, 16], F32, st); gTB = Buf("gT")
            condB_t = T("condB", [128, 8, 128], F32, st); condBB = Buf("condB")
            cT = T("cT", [128, 8], F32, st); cB = Buf("cT")
            S.dma('sp', cT[:], c_d.rearrange("(j p) -> p j", p=128), W=[cB], allow_slow_non_contiguous=True)
            act(cT[:], cT[:], AF.Silu, [cB], [cB])
            cp('dve', condB_t[:], cT[:].unsqueeze(2).broadcast_to([128, 8, 128]), [cB], [condBB])
            S.dma('sp', gT[:, 0:8], Wd['norm1_g'][l].rearrange("(j p) -> p j", p=128), W=[gTB], allow_slow_non_contiguous=True)
            S.dma('sp', gT[:, 8:16], Wd['norm2_g'][l].rearrange("(j p) -> p j", p=128), W=[gTB], allow_slow_non_contiguous=True)
            pr = Ring(PI[0:2]); pr2 = Ring(PI[2:4])
            for n in range(12):
                wa, waB = wa_r.next(); bb, bbB = bb_r.next(); mb, mbB = mb_r.next()
                S.dma('sp', wa[:, 0:4, :], Wd['w_ada'][l][0:512, n * 512:(n + 1) * 512].rearrange("(j p) n -> p j n", p=128), W=[waB])
                S.dma('pool', wa[:, 4:8, :], Wd['w_ada'][l][512:1024, n * 512:(n + 1) * 512].rearrange("(j p) n -> p j n", p=128), W=[waB])
                S.dma('sp', bb[:], Wd['b_ada'][l][n * 512:(n + 1) * 512].partition_broadcast(128), W=[bbB])
                ps, psB = pr.next()
                for j in range(8):
                    mm(ps[:, :], condB_t[:, j, :], wa[:, j, :], j == 0, j == 7, [condBB, waB], [psB])
                if n in (4, 5, 10, 11):
                    gi = 0 if n < 6 else 1
                    hf = n % 2
                    tt('dve', g1g2[:, gi, hf * 512:(hf + 1) * 512], ps[:, :], bb[:], ALU.add, [psB, bbB], [g12B])
                else:
                    tt('dve', mb[:], ps[:, :], bb[:], ALU.add, [psB, bbB], [mbB])
                    base = {0: 0, 1: 4, 2: 8, 3: 12, 6: 16, 7: 20, 8: 24, 9: 28}[n]
                    p2, p2B = pr2.next()
                    for q in range(4):
                        tr(p2[:, q * 128:(q + 1) * 128], mb[:, q * 128:(q + 1) * 128], [mbB], [p2B])
                    cp('act' if n % 2 else 'dve', modT[:, base:base + 4], p2[:, :].rearrange("p (q t) -> p q t", t=128)[:, :, 0], [p2B], [modTB])
            stt(ABT[:, 0:8], modT[:, 8:16], 1.0, gT[:, 0:8], ALU.add, ALU.mult, [modTB, gTB], [ABTB])
            stt(ABT[:, 8:16], modT[:, 24:32], 1.0, gT[:, 8:16], ALU.add, ALU.mult, [modTB, gTB], [ABTB])
            S.barrier()
        dump("modT%d" % l, modT[:], [128, 32])
        dump("ABT%d" % l, ABT[:], [128, 16])
        dump("g1g2_%d" % l, g1g2[:].rearrange("p a d -> p (a d)"), [128, 2 * D])
        if stop == 'A0':
            return False

        def norm_phase(which, hT, hB, st, router=None):
            aoff = 0 if which == 1 else 8
            boff = 0 if which == 1 else 16
            junk_r = Ring([(T("nj%d" % i, [128, D], BF16, st), Buf("nj%d" % i)) for i in range(2)])
            xn_r = Ring([(T("xn%d" % i, [128, D], F32, st), Buf("xn%d" % i)) for i in range(2)])
            ss_r = Ring([(T("nss%d" % i, [128, 2], F32, st), Buf("nss%d" % i)) for i in range(3)])
            pr = Ring([(PI[0], PI[1]), (PI[2], PI[3])])
            def n0(t):
                junk, jB = junk_r.next(); xn, xnB = xn_r.next(); ss, ssB = ss_r.next()
                act(junk[:], xs[:, t, :], AF.Square, [xB[t]], [jB, ssB], accum_out=ss[:, 0:1])
                ts('dve', ss[:, 1:2], ss[:, 0:1], 1.0 / D, ALU.mult, [ssB], [ssB], s2=EPS, op1=ALU.add)
                act(ss[:, 1:2], ss[:, 1:2], AF.Ln, [ssB], [ssB])
                act(ss[:, 1:2], ss[:, 1:2], AF.Exp, [ssB], [ssB], scale=-0.5)
                ts('dve', xn[:], xs[:, t, :], ss[:, 1:2], ALU.mult, [xB[t], ssB], [xnB])
                return xn, xnB

            def n1(t, xn, xnB):
                (pa, paB), (pb, pbB) = pr.next()
                for j in range(8):
                    p, pB_ = (pa, paB) if j < 4 else (pb, pbB)
                    tr(p[:, (j % 4) * 128:(j % 4 + 1) * 128], xn[:, j * 128:(j + 1) * 128], [xnB], [pB_])
                for j in range(8):
                    p, pB_ = (pa, paB) if j < 4 else (pb, pbB)
                    src = p[:, (j % 4) * 128:(j % 4 + 1) * 128]
                    dst = hT[:, j, t * 128:(t + 1) * 128]
                    if j % 2 == 0:
                        act(dst, src, AF.Identity, [pB_, ABTB, modTB], [hB[t]], scale=ABT[:, aoff + j:aoff + j + 1], bias=modT[:, boff + j:boff + j + 1])
                    else:
                        ts('dve', dst, src, ABT[:, aoff + j:aoff + j + 1], ALU.mult, [pB_, ABTB, modTB], [hB[t]], s2=modT[:, boff + j:boff + j + 1], op1=ALU.add)
                if router is not None:
                    router(t, pa, paB, pb, pbB, aoff, boff)

            pend = {}
            for k in range(NT + 1):
                if k < NT:
                    pend[k] = n0(k)
                if k >= 1:
                    n1(k - 1, *pend.pop(k - 1))

        def load_w(tile, buf, src2d, r0, nrow_chunks, c0, ncols):
            S.dma('pool', tile[:, 0:nrow_chunks, 0:ncols],
                  src2d[r0:r0 + 128 * nrow_chunks, c0:c0 + ncols].rearrange("(j p) n -> p j n", p=128), W=[buf])

        def proj_tok(ps, psB, hT, hB, W, WB, c0, ncols, t):
            for j in range(8):
                mm(ps[:, 0:ncols], hT[:, j, t * 128:(t + 1) * 128], W[:, j, c0:c0 + ncols], j == 0, j == 7, [hB[t], WB], [psB])

        def proj_T(ps, psB, hT, hB, W, WB, c0, ncols, c):
            R = [hB[t] for t in range(c * QPC, (c + 1) * QPC)] + [WB]
            for j in range(8):
                mm(ps[0:ncols, 0:CH], W[:, j, c0:c0 + ncols], hT[:, j, c * CH:(c + 1) * CH], j == 0, j == 7, R, [psB])

        stA = ExitStack()
        hT = T("hT", [128, 8, SEQ], BF16, stA); hB = [Buf("h%d" % t) for t in range(NT)]
        with ExitStack() as st:
            norm_phase(1, hT, hB, st)
            S.barrier()
        dump("hT%d" % l, hT[:].rearrange("p j t -> p (j t)"), [128, 8 * SEQ], BF16)
        if stop == 'A1':
            stA.close()
            return False

        with ExitStack() as st:
            w_r = Ring([(T("wsb%d" % i, [128, 8, 512], BF16, st), Buf("wsb%d" % i)) for i in range(2)])
            stg_r = Ring([(T("sbst%d" % i, [128, SEQ], BF16, st), Buf("sbst%d" % i)) for i in range(3)])
            stv_r = Ring([(T("sbsv%d" % i, [128, 512], BF16, st), Buf("sbsv%d" % i)) for i in range(3)])
            pr = Ring(PI)
            for part in range(3):
                W, WB = w_r.next()
                load_w(W, WB, w_in, 0, 8, part * 512, 512)
                if part < 2:
                    for m in range(4):
                        stg, stgB = stg_r.next()
                        for c in range(NCH):
                            ps, psB = pr.next()
                            proj_T(ps, psB, hT, hB, W, WB, m * 128, 128, c)
                            cp('act' if c % 2 else 'dve', stg[:, c * CH:(c + 1) * CH], ps[:, 0:CH], [psB], [stgB])
                        S.dma('sp', sbqk_d[part * 4 + m], stg[:], R=[stgB], W=[sbqk_B[part * 4 + m]])
                else:
                    for t in range(NT):
                        stv, stvB = stv_r.next()
                        ps, psB = pr.next()
                        proj_tok(ps, psB, hT, hB, W, WB, 0, 512, t)
                        cp('act' if t % 2 else 'dve', stv[:], ps[:, :], [psB], [stvB])
                        S.dma('sp', sbv_d[t * 128:(t + 1) * 128, :], stv[:], R=[stvB], W=[sbv_B])
            S.barrier()
        if stop == 'A2SB':
            stA.close()
            return False
        with ExitStack() as st:
            Wm = T("wmla", [128, 8, 1056], BF16, st); WmB = Buf("wmla")
            for i in range(3):
                c0 = i * 512
                ncol = min(512, 1056 - c0)
                S.dma('pool', Wm[:, :, c0:c0 + ncol], w_in[:, C_MLA + c0:C_MLA + c0 + ncol].rearrange("(j p) n -> p j n", p=128), W=[WmB])
            Wuq = T("wuq", [128, 6, 768], BF16, st); WuqB = Buf("wuq")
            load_w(Wuq, WuqB, Wd['w_mla_uq'][l], 0, 6, 0, 768)
            Wukv = T("wukv", [128, 2, 1024], BF16, st); WukvB = Buf("wukv")
            load_w(Wukv, WukvB, Wd['w_mla_ukv'][l], 0, 2, 0, 1024)
            nrm = T("mnrm", [128, 8], F32, st); nrmB = Buf("mnrm")
            S.dma('sp', nrm[:, 0:6], Wd['mla_q_norm'][l].rearrange("(j p) -> p j", p=128), W=[nrmB], allow_slow_non_contiguous=True)
            S.dma('sp', nrm[:, 6:8], Wd['mla_kv_norm'][l].rearrange("(j p) -> p j", p=128), W=[nrmB], allow_slow_non_contiguous=True)
            for j in range(6):
                ts('dve', Wuq[:, j, :], Wuq[:, j, :], nrm[:, j:j + 1], ALU.mult, [WuqB, nrmB], [WuqB])
            for j in range(2):
                ts('dve', Wukv[:, j, :], Wukv[:, j, :], nrm[:, 6 + j:7 + j], ALU.mult, [WukvB, nrmB], [WukvB])
            gq = T("mgq", [128, 3], F32, st); gqB = Buf("mgq")
            S.dma('sp', gq[0:96, 0:1], Wd['mla_q_gain'][l].rearrange("(p o) -> p o", o=1), W=[gqB])
            S.dma('sp', gq[0:96, 1:2], Wd['mla_k_gain'][l].rearrange("(p o) -> p o", o=1), W=[gqB])
            stt(gq[0:96, 2:3], gq[0:96, 0:1], 1.0 / math.sqrt(96.0), gq[0:96, 1:2], ALU.mult, ALU.mult, [gqB], [gqB])
            pr = Ring(PI)
            NR = 2
            junk_r = Ring([(T("mj%d" % i, [128, 512], BF16, st), Buf("mj%d" % i)) for i in range(NR)])
            ss_r = Ring([(T("mss%d" % i, [128, 8], F32, st), Buf("mss%d" % i)) for i in range(NR)])
            cf_r = Ring([(T("mcf%d" % i, [128, 1024], F32, st), Buf("mcf%d" % i)) for i in range(NR)])
            cT_r = Ring([(T("mcT%d" % i, [128, 8, 128], BF16, st), Buf("mcT%d" % i)) for i in range(NR)])
            qf_r = Ring([(T("mqf%d" % i, [128, 768], F32, st), Buf("mqf%d" % i)) for i in range(NR)])
            kf_r = Ring([(T("mkf%d" % i, [128, 768], F32, st), Buf("mkf%d" % i)) for i in range(NR)])
            vb_r = Ring([(T("mvb%d" % i, [128, 8, 66], BF16, st), Buf("mvb%d" % i)) for i in range(NR)])
            rp_r = Ring([(T("mrp%d" % i, [128, 8, 64], F32, st), Buf("mrp%d" % i)) for i in range(NR)])
            sm_r = Ring([(T("msm%d" % i, [128, 192], F32, st), Buf("msm%d" % i)) for i in range(NR)])
            sq_r = Ring([(T("msq%d" % i, [128, 768], F32, st), Buf("msq%d" % i)) for i in range(NR)])
            rq_r = Ring([(T("mrq%d" % i, [128, 32], F32, st), Buf("mrq%d" % i)) for i in range(NR)])
            stq_r = Ring([(T("mstq%d" % i, [128, 8, 128], BF16, st), Buf("mstq%d" % i)) for i in range(NR)])
            stk_r = Ring([(T("mstk%d" % i, [128, 8, 128], BF16, st), Buf("mstk%d" % i)) for i in range(NR)])
            for it in vb_r.items:
                memset('pool', it[0][:, :, 64:66], 1.0, [it[1]])
            prA = Ring(PI[0:3]); prB = Ring(PI[3:8])
            ST = {}

            def sA_pe(t):
                d = ST.setdefault(t, {})
                pa, paB = prA.next(); pb, pbB = prA.next(); pc, pcB = prA.next()
                d['pa'] = (pa, paB); d['pb'] = (pb, pbB); d['pc'] = (pc, pcB)
                proj_tok(pa, paB, hT, hB, Wm, WmB, 0, 512, t)
                proj_tok(pb, pbB, hT, hB, Wm, WmB, 512, 512, t)
                proj_tok(pc, pcB, hT, hB, Wm, WmB, 1024, 32, t)
                junk, jB = junk_r.next(); ss, ssB = ss_r.next()
                d['ss'] = (ss, ssB)
                act(junk[:, 0:512], pa[:, :], AF.Square, [paB], [jB, ssB], accum_out=ss[:, 0:1])
                act(junk[:, 0:256], pb[:, 0:256], AF.Square, [pbB], [jB, ssB], accum_out=ss[:, 1:2])
                act(junk[:, 0:256], pb[:, 256:512], AF.Square, [pbB], [jB, ssB], accum_out=ss[:, 2:3])

            def sA_dve(t):
                d = ST[t]
                pa, paB = d['pa']; pb, pbB = d['pb']; pc, pcB = d['pc']; ss, ssB = d['ss']
                cf, cfB = cf_r.next(); sm, smB = sm_r.next()
                d['cf'] = (cf, cfB); d['sm'] = (sm, smB)
                cp('dve', cf[:, 0:512], pa[:, :], [paB], [cfB])
                cp('dve', cf[:, 512:1024], pb[:, :], [pbB], [cfB])
                tt('dve', ss[:, 0:1], ss[:, 0:1], ss[:, 1:2], ALU.add, [ssB], [ssB])
                ts('dve', ss[:, 4:5], ss[:, 0:1], 1.0 / 768, ALU.mult, [ssB], [ssB], s2=EPS, op1=ALU.add)
                ts('dve', ss[:, 5:6], ss[:, 2:3], 1.0 / 256, ALU.mult, [ssB], [ssB], s2=EPS, op1=ALU.add)
                act(ss[:, 4:6], ss[:, 4:6], AF.Ln, [ssB], [ssB])
                act(ss[:, 4:6], ss[:, 4:6], AF.Exp, [ssB], [ssB], scale=-0.5)
                sinv = SC[:, t, 0:16]; cosv = SC[:, t, 16:32]
                k1 = pc[:, 0:16]; k2 = pc[:, 16:32]
                tt('dve', sm[:, 48:64], k1, cosv, ALU.mult, [pcB, SCB], [smB])
                tt('dve', sm[:, 64:80], k2, sinv, ALU.mult, [pcB, SCB], [smB])
                tt('dve', sm[:, 80:96], k1, sinv, ALU.mult, [pcB, SCB], [smB])
                tt('dve', sm[:, 96:112], k2, cosv, ALU.mult, [pcB, SCB], [smB])
                tt('dve', sm[:, 112:128], sm[:, 48:64], sm[:, 64:80], ALU.subtract, [smB], [smB])
                tt('dve', sm[:, 128:144], sm[:, 80:96], sm[:, 96:112], ALU.add, [smB], [smB])

            def sB(t):
                d = ST[t]
                cf, cfB = d['cf']; ss, ssB = d['ss']
                cT, cTB = cT_r.next()
                pt1, pt1B = prB.next(); pt2, pt2B = prB.next()
                for j in range(8):
                    p, pB_ = (pt1, pt1B) if j < 4 else (pt2, pt2B)
                    tr(p[:, (j % 4) * 128:(j % 4 + 1) * 128], cf[:, j * 128:(j + 1) * 128], [cfB], [pB_])
                cp('act', cT[:, 0:4, :].rearrange("p j t -> p (j t)"), pt1[:, :], [pt1B], [cTB])
                cp('dve', cT[:, 4:8, :].rearrange("p j t -> p (j t)"), pt2[:, :], [pt2B], [cTB])
                pq = [prB.next(), prB.next()]; pkv = [prB.next(), prB.next()]
                for half in range(2):
                    for j in range(6):
                        mm(pq[half][0][:, 0:384], cT[:, j, :], Wuq[:, j, half * 384:(half + 1) * 384], j == 0, j == 5, [cTB, WuqB], [pq[half][1]])
                for half in range(2):
                    for j in range(2):
                        mm(pkv[half][0][:, 0:512], cT[:, 6 + j, :], Wukv[:, j, half * 512:(half + 1) * 512], j == 0, j == 1, [cTB, WukvB], [pkv[half][1]])
                qf, qfB = qf_r.next(); kf, kfB = kf_r.next(); vb, vbB = vb_r.next()
                d['qf'] = (qf, qfB); d['kf'] = (kf, kfB); d['vb'] = (vb, vbB)
                k3 = kf[:].rearrange("p (h d) -> p h d", d=96)
                for half in range(2):
                    ts('dve', qf[:, half * 384:(half + 1) * 384], pq[half][0][:, 0:384], ss[:, 4:5], ALU.mult, [pq[half][1], ssB], [qfB])
                    src3 = pkv[half][0][:, :].rearrange("p (h e) -> p h e", e=128)
                    ts('dve', k3[:, half * 4:(half + 1) * 4, 0:64], src3[:, :, 0:64], ss[:, 5:6], ALU.mult, [pkv[half][1], ssB], [kfB])
                    ts('dve', vb[:, half * 4:(half + 1) * 4, 0:64], src3[:, :, 64:128], ss[:, 5:6], ALU.mult, [pkv[half][1], ssB], [vbB])

            def sC_dve(t):
                d = ST[t]
                qf, qfB = d['qf']; kf, kfB = d['kf']; sm, smB = d['sm']
                q3 = qf[:].rearrange("p (h d) -> p h d", d=96); k3 = kf[:].rearrange("p (h d) -> p h d", d=96)
                rp, rpB = rp_r.next()
                sinv = SC[:, t, 0:16]; cosv = SC[:, t, 16:32]
                sinB3 = sinv.unsqueeze(1).broadcast_to([128, 8, 16]); cosB3 = cosv.unsqueeze(1).broadcast_to([128, 8, 16])
                q1 = q3[:, :, 64:80]; q2 = q3[:, :, 80:96]
                tt('dve', rp[:, :, 0:16], q1, cosB3, ALU.mult, [qfB, SCB], [rpB])
                tt('dve', rp[:, :, 16:32], q2, sinB3, ALU.mult, [qfB, SCB], [rpB])
                tt('dve', rp[:, :, 32:48], q1, sinB3, ALU.mult, [qfB, SCB], [rpB])
                tt('dve', rp[:, :, 48:64], q2, cosB3, ALU.mult, [qfB, SCB], [rpB])
                tt('dve', q1, rp[:, :, 0:16], rp[:, :, 16:32], ALU.subtract, [rpB], [qfB])
                tt('dve', q2, rp[:, :, 32:48], rp[:, :, 48:64], ALU.add, [rpB], [qfB])
                cp('dve', k3[:, :, 64:96], sm[:, 112:144].unsqueeze(1).broadcast_to([128, 8, 32]), [smB], [kfB])
                sq, sqB = sq_r.next(); rq, rqB = rq_r.next()
                tt('dve', sq[:], qf[:], qf[:], ALU.mult, [qfB], [sqB])
                red(rq[:, 0:8], sq[:].rearrange("p (h d) -> p h d", d=96), [sqB], [rqB])
                tt('pool', sq[:], kf[:], kf[:], ALU.mult, [kfB, rqB], [sqB])
                red(rq[:, 8:16], sq[:].rearrange("p (h d) -> p h d", d=96), [sqB], [rqB])
                ts('dve', rq[:, 16:32], rq[:, 0:16], 1.0 / 96, ALU.mult, [rqB], [rqB], s2=EPS, op1=ALU.add)
                act(rq[:, 16:32], rq[:, 16:32], AF.Ln, [rqB], [rqB])
                act(rq[:, 16:32], rq[:, 16:32], AF.Exp, [rqB], [rqB], scale=-0.5)
                tt('dve', q3, q3, rq[:, 16:24].unsqueeze(2).broadcast_to([128, 8, 96]), ALU.mult, [qfB, rqB], [qfB])
                tt('dve', k3, k3, rq[:, 24:32].unsqueeze(2).broadcast_to([128, 8, 96]), ALU.mult, [kfB, rqB], [kfB])

            def sC_rest(t):
                d = ST.pop(t)
                qf, qfB = d['qf']; kf, kfB = d['kf']; vb, vbB = d['vb']
                q3 = qf[:].rearrange("p (h d) -> p h d", d=96); k3 = kf[:].rearrange("p (h d) -> p h d", d=96)
                stq, stqB = stq_r.next(); stk, stkB = stk_r.next()
                for (src3_, srcB, stg, stgB, isq) in ((q3, qfB, stq, stqB, True), (k3, kfB, stk, stkB, False)):
                    for half in range(2):
                        p, pB_ = prB.next()
                        for hh in range(4):
                            tr(p[0:96, hh * 128:(hh + 1) * 128], src3_[:, half * 4 + hh, :], [srcB], [pB_])
                        src = p[0:96, :]
                        dst = stg[0:96, half * 4:(half + 1) * 4, :].rearrange("p h t -> p (h t)")
                        if isq:
                            act(dst, src, AF.Identity, [pB_, gqB], [stgB], scale=gq[0:96, 2:3])
                        else:
                            cp('dve', dst, src, [pB_], [stgB])
                S.dma('sp', mq_d[:, :, t * 128:(t + 1) * 128].rearrange("h p t -> p h t"), stq[0:96, :, :], R=[stqB], W=[mq_B])
                S.dma('sp', mk_d[:, :, t * 128:(t + 1) * 128].rearrange("h p t -> p h t"), stk[0:96, :, :], R=[stkB], W=[mk_B])
                S.dma('sp', mv_d[t * 128:(t + 1) * 128, :], vb[:].rearrange("p h e -> p (h e)"), R=[vbB], W=[mv_B])

            sA_pe(0); sA_dve(0)
            for k in range(NT):
                sB(k)
                if k + 1 < NT:
                    sA_pe(k + 1)
                sC_dve(k)
                if k + 1 < NT:
                    sA_dve(k + 1)
                sC_rest(k)
            S.barrier()

        if stop == 'A2MLA':
            stA.close()
            return False
        with ExitStack() as st:
            W3 = [(T("wdf%d" % i, [128, 8, 512], BF16, st), Buf("wdf%d" % i)) for i in range(3)]
            for i, c0 in enumerate((C_DQ, C_DK, C_DV)):
                load_w(W3[i][0], W3[i][1], w_in, 0, 8, c0, 512)
            gd = T("dgd", [128, 3], F32, st); gdB = Buf("dgd")
            for half in range(2):
                S.dma('sp', gd[half * 64:(half + 1) * 64, 0:1], Wd['diff_q_gain'][l].rearrange("(p o) -> p o", o=1), W=[gdB])
                S.dma('sp', gd[half * 64:(half + 1) * 64, 1:2], Wd['diff_k_gain'][l].rearrange("(p o) -> p o", o=1), W=[gdB])
            stt(gd[:, 2:3], gd[:, 0:1], 1.0 / 8.0, gd[:, 1:2], ALU.mult, ALU.mult, [gdB], [gdB])
            pr = Ring(PI)
            NR = 2
            sq_r = Ring([(T("dsq%d" % i, [128, 512], F32, st), Buf("dsq%d" % i)) for i in range(NR)])
            rr_r = Ring([(T("drr%d" % i, [128, 32], F32, st), Buf("drr%d" % i)) for i in range(NR)])
            qn_r = Ring([(T("dqn%d" % i, [128, 512], F32, st), Buf("dqn%d" % i)) for i in range(NR)])
            kn_r = Ring([(T("dkn%d" % i, [128, 512], F32, st), Buf("dkn%d" % i)) for i in range(NR)])
            stq_r = Ring([(T("dstq%d" % i, [128, 4, 128], BF16, st), Buf("dstq%d" % i)) for i in range(NR)])
            stk_r = Ring([(T("dstk%d" % i, [128, 4, 128], BF16, st), Buf("dstk%d" % i)) for i in range(NR)])
            vb_r = Ring([(T("dvb%d" % i, [128, 4, 130], BF16, st), Buf("dvb%d" % i)) for i in range(NR)])
            for it in vb_r.items:
                memset('pool', it[0][:, :, 128:130], 1.0, [it[1]])
            prA = Ring(PI[0:3]); prB = Ring(PI[3:8])
            DS = {}

            def d0(t):
                pq, pqB = prA.next(); pk, pkB = prA.next(); pv, pvB = prA.next()
                proj_tok(pq, pqB, hT, hB, W3[0][0], W3[0][1], 0, 512, t)
                proj_tok(pk, pkB, hT, hB, W3[1][0], W3[1][1], 0, 512, t)
                proj_tok(pv, pvB, hT, hB, W3[2][0], W3[2][1], 0, 512, t)
                rr, rrB = rr_r.next()
                for (p, pB_, off) in ((pq, pqB, 0), (pk, pkB, 8)):
                    sq, sqB = sq_r.next()
                    act(sq[:], p[:, :], AF.Square, [pB_], [sqB])
                    red(rr[:, off:off + 8], sq[:].rearrange("p (g d) -> p g d", d=64), [sqB], [rrB])
                ts('dve', rr[:, 16:32], rr[:, 0:16], 1.0 / 64, ALU.mult, [rrB], [rrB], s2=EPS, op1=ALU.add)
                act(rr[:, 16:32], rr[:, 16:32], AF.Ln, [rrB], [rrB])
                act(rr[:, 16:32], rr[:, 16:32], AF.Exp, [rrB], [rrB], scale=-0.5)
                qn, qnB = qn_r.next(); kn, knB = kn_r.next(); vb, vbB = vb_r.next()
                tt('dve', qn[:].rearrange("p (g d) -> p g d", d=64), pq[:, :].rearrange("p (g d) -> p g d", d=64),
                   rr[:, 16:24].unsqueeze(2).broadcast_to([128, 8, 64]), ALU.mult, [pqB, rrB], [qnB])
                tt('dve', kn[:].rearrange("p (g d) -> p g d", d=64), pk[:, :].rearrange("p (g d) -> p g d", d=64),
                   rr[:, 24:32].unsqueeze(2).broadcast_to([128, 8, 64]), ALU.mult, [pkB, rrB], [knB])
                cp('dve', vb[:, :, 0:128], pv[:, :].rearrange("p (h e) -> p h e", e=128), [pvB], [vbB])
                DS[t] = (qn, qnB, kn, knB, vb, vbB)

            def d1(t):
                qn, qnB, kn, knB, vb, vbB = DS.pop(t)
                stq, stqB = stq_r.next(); stk, stkB = stk_r.next()
                p, pB_ = prB.next()
                for g in range(4):
                    tr(p[:, g * 128:(g + 1) * 128], qn[:, g * 128:(g + 1) * 128], [qnB], [pB_])
                act(stq[:].rearrange("p g t -> p (g t)"), p[:, :], AF.Identity, [pB_, gdB], [stqB], scale=gd[:, 2:3])
                p, pB_ = prB.next()
                for g in range(4):
                    tr(p[:, g * 128:(g + 1) * 128], kn[:, g * 128:(g + 1) * 128], [knB], [pB_])
                cp('dve', stk[:].rearrange("p g t -> p (g t)"), p[:, :], [pB_], [stkB])
                S.dma('sp', dq_d[:, :, t * 128:(t + 1) * 128].rearrange("g p t -> p g t"), stq[:], R=[stqB], W=[dq_B])
                S.dma('sp', dk_d[:, :, t * 128:(t + 1) * 128].rearrange("g p t -> p g t"), stk[:], R=[stkB], W=[dk_B])
                S.dma('sp', dv_d[t * 128:(t + 1) * 128, :], vb[:].rearrange("p h e -> p (h e)"), R=[vbB], W=[dv_B])

            d0(0)
            for k in range(NT):
                if k + 1 < NT:
                    d0(k + 1)
                d1(k)
            S.barrier()

        if stop == 'A2DIFF':
            stA.close()
            return False
        with ExitStack() as st:
            w_r = Ring([(T("wgt%d" % i, [128, 8, 512], BF16, st), Buf("wgt%d" % i)) for i in range(2)])
            stg_r = Ring([(T("gst%d" % i, [128, SEQ], BF16, st), Buf("gst%d" % i)) for i in range(3)])
            pr = Ring(PI)
            for part in range(6):
                W, WB = w_r.next()
                load_w(W, WB, w_in, 0, 8, C_GATE + part * 512, 512)
                for m in range(4):
                    stg, stgB = stg_r.next()
                    for c in range(NCH):
                        ps, psB = pr.next()
                        proj_T(ps, psB, hT, hB, W, WB, m * 128, 128, c)
                        act(stg[:, c * CH:(c + 1) * CH], ps[:, 0:CH], AF.Sigmoid, [psB], [stgB])
                    S.dma('sp', gt_d[part * 4 + m], stg[:], R=[stgB], W=[gt_B])
            S.barrier()

        if stop == 'A2G':
            stA.close()
            return False
        stA.close()
        stB = ExitStack()
        oT = [T("oT%d" % n, [128, 4, SEQ], BF16, stB) for n in range(3)]
        oTB = [[Buf("oT%d_%d" % (n, k)) for k in range(4)] for n in range(3)]

        def sb_attention():
            with ExitStack() as st:
                NB = 2
                msk_sb = T("msk_sb", [128, 4, 512], BF16, st); mskB = Buf("msk")
                memset('pool', msk_sb[:], 1.0, [mskB])
                for r in range(4):
                    def f(e, r=r):
                        return e.affine_select(out=msk_sb[:, r, :], in_=msk_sb[:, r, :], pattern=[[1, 512]], compare_op=ALU.is_gt, fill=0.0, base=-128 * r, channel_multiplier=-1)
                    S.add('pool', f, R=[mskB], W=[mskB])
                mneg = T("mneg_sb", [128, 4, 512], BF16, st); mnegB = Buf("mneg")
                ts('dve', mneg[:].rearrange("p r t -> p (r t)"), msk_sb[:].rearrange("p r t -> p (r t)"), 240.0, ALU.mult, [mskB], [mnegB], s2=-240.0, op1=ALU.add)
                qz_r = Ring([([T("sbq%d_%d" % (i, z), [128, SEQ], BF16, st) for z in range(2)], Buf("sbq%d" % i)) for i in range(NB)])
                k_r = Ring([(T("sbk%d" % i, [128, SEQ], BF16, st), Buf("sbk%d" % i)) for i in range(NB)])
                v_r = Ring([(T("sbvp%d" % i, [128, NT, 2, 128], BF16, st), Buf("sbvp%d" % i)) for i in range(NB)])
                e_r = Ring([(T("sbe%d" % i, [128, 512], F32, st), Buf("sbe%d" % i)) for i in range(2)])
                sp_r = Ring([(T("sbsp%d" % i, [128, 512], F32, st), Buf("sbsp%d" % i)) for i in range(4)])
                tm_r = Ring([(T("sbtm%d" % i, [128, 512], F32, st), Buf("sbtm%d" % i)) for i in range(2)])
                pb_r = Ring([(T("sbpb%d" % i, [128, 512], BF16, st), Buf("sbpb%d" % i)) for i in range(3)])
                wb_r = Ring([(T("sbwb%d" % i, [128, 512], BF16, st), Buf("sbwb%d" % i)) for i in range(3)])
                ra_r = Ring([(T("sbra%d" % i, [128, 512], BF16, st), Buf("sbra%d" % i)) for i in range(3)])
                pz_r = Ring(PI[0:3]); pc_r = Ring(PI[3:6]); po_r = Ring(PI[6:8])
                scale = 1.0 / 8.0
                heads = {}

                def load_hp(hp):
                    qz, qB = qz_r.next(); kt, kB = k_r.next(); vp, vB = v_r.next()
                    memset('pool', qz[0][64:128, :], 0.0, [qB]); memset('pool', qz[1][0:64, :], 0.0, [qB])
                    S.dma('sp', qz[0][0:64, :], sbqk_d[hp][0:64, :], R=[sbqk_B[hp]], W=[qB])
                    S.dma('sp', qz[1][64:128, :], sbqk_d[hp][64:128, :], R=[sbqk_B[hp]], W=[qB])
                    S.dma('sp', kt[:], sbqk_d[4 + hp], R=[sbqk_B[4 + hp]], W=[kB])
                    memset('pool', vp[:, :, 0, 64:128], 0.0, [vB]); memset('pool', vp[:, :, 1, 0:64], 0.0, [vB])
                    S.dma('sp', vp[:, :, 0, 0:64], sbv_d[:, hp * 128:hp * 128 + 64].rearrange("(t p) d -> p t d", p=128), R=[sbv_B], W=[vB])
                    S.dma('sp', vp[:, :, 1, 64:128], sbv_d[:, hp * 128 + 64:hp * 128 + 128].rearrange("(t p) d -> p t d", p=128), R=[sbv_B], W=[vB])
                    heads[hp] = (qz, qB, kt, kB, vp, vB)

                items = []
                for hp in range(4):
                    for z in range(2):
                        for c in range(NCH):
                            nblk = (c + 1) * QPC
                            grp_ = {}
                            for bi, sb in enumerate(range(nblk - 1, -1, -1)):
                                items.append(dict(hp=hp, z=z, c=c, sb=sb, bi=bi, nblk=nblk, g=grp_, idx=len(items)))
                first_idx = {}
                for it in items:
                    first_idx.setdefault(it['hp'], it['idx'])

                def s0(x):
                    if x['idx'] == 0:
                        load_hp(0)
                    if x['idx'] == first_idx[x['hp']] + 3 and x['hp'] + 1 < 4:
                        load_hp(x['hp'] + 1)
                    qz, qB, kt, kB, vp, vB = heads[x['hp']]
                    c, sb, z = x['c'], x['sb'], x['z']
                    x['pz'], x['pzB'] = pz_r.next()
                    e_, eB = e_r.next()
                    x['sp'], x['spB'] = sp_r.next()
                    diag = sb >= c * QPC
                    mm(x['pz'][:, 0:CH], kt[:, sb * 128:(sb + 1) * 128], qz[z][:, c * CH:(c + 1) * CH], True, not diag, [kB, qB], [x['pzB']])
                    if diag:
                        wd_ = 128 * (sb - c * QPC + 1)
                        mm(x['pz'][:, 0:wd_], identb[:], mneg[:, sb - c * QPC, 0:wd_], False, True, [identbB, mnegB], [x['pzB']])
                    act(e_[:, 0:CH], x['pz'][:, 0:CH], AF.Exp, [x['pzB']], [eB], scale=-scale)
                    act(x['sp'][:, 0:CH], e_[:, 0:CH], AF.Ln, [eB], [x['spB']], bias=1.0)

                def s1(x):
                    c, sb, bi, g = x['c'], x['sb'], x['bi'], x['g']
                    diag = sb >= c * QPC
                    r = sb - c * QPC
                    x['pb'], x['pbB'] = pb_r.next()
                    x['pc'], x['pcB'] = pc_r.next()
                    pb, pbB, pc, pcB = x['pb'], x['pbB'], x['pc'], x['pcB']
                    stt(pb[:, 0:CH], x['pz'][:, 0:CH], scale, x['sp'][:, 0:CH], ALU.mult, ALU.add, [x['pzB'], x['spB']], [pbB])
                    if diag:
                        wd_ = 128 * (r + 1)
                        tt('dve', pb[:, 0:wd_], pb[:, 0:wd_], msk_sb[:, r, 0:wd_], ALU.mult, [pbB, mskB], [pbB])
                    mm(pc[:, 0:CH], upper_b[:], pb[:, 0:CH], True, bi == 0, [upperB, pbB], [pcB])
                    if bi > 0:
                        ra, raB = g['ra']
                        mm(pc[:, 0:CH], ones_b[:], ra[:, 0:CH], False, True, [onesB, raB], [pcB])
                    if bi < x['nblk'] - 1:
                        ran, ranB = ra_r.next()
                        if bi == 0:
                            cp('pool', ran[:, 0:CH], pb[:, 0:CH], [pbB], [ranB])
                        else:
                            ra, raB = g['ra']
                            tt('pool', ran[:, 0:CH], ra[:, 0:CH], pb[:, 0:CH], ALU.add, [raB, pbB], [ranB])
                        g['ra'] = (ran, ranB)

                def s2(x):
                    c, sb = x['c'], x['sb']
                    diag = sb >= c * QPC
                    r = sb - c * QPC
                    tm, tmB = tm_r.next()
                    x['wb'], x['wbB'] = wb_r.next()
                    tt('dve', tm[:, 0:CH], x['pc'][:, 0:CH], x['sp'][:, 0:CH], ALU.add, [x['pcB'], x['spB']], [tmB])
                    act(x['wb'][:, 0:CH], tm[:, 0:CH], AF.Exp, [tmB], [x['wbB']], scale=-1.0)

                def s3(x):
                    qz, qB, kt, kB, vp, vB = heads[x['hp']]
                    c, sb, z, bi, g, hp = x['c'], x['sb'], x['z'], x['bi'], x['g'], x['hp']
                    if bi == 0:
                        g['po'] = po_r.next()
                    po, poB = g['po']
                    mm(po[:, 0:CH], vp[:, sb, z, :], x['wb'][:, 0:CH], bi == 0, bi == x['nblk'] - 1, [vB, x['wbB']], [poB])
                    if bi == x['nblk'] - 1:
                        po_ = 64 * z
                        cp('act', oT[0][po_:po_ + 64, hp, c * CH:(c + 1) * CH], po[po_:po_ + 64, 0:CH], [poB], [oTB[0][hp]])

                stages = [s0, s1, s2, s3]
                for k in range(len(items) + len(stages) - 1):
                    for j, stg_f in enumerate(stages):
                        ii = k - j
                        if 0 <= ii < len(items):
                            stg_f(items[ii])
                S.barrier()
        sb_attention()
        dump("o_sbT%d" % l, oT[0][:].rearrange("p j t -> p (j t)"), [128, 4 * SEQ], BF16)
        if stop == 'BSB':
            stB.close()
            return False

        def softmax_attn(tag, nheads_pair_iter):
            pass

        with ExitStack() as st:
            NB = 2
            q_r = Ring([(T("mq%d" % i, [128, SEQ], BF16, st), Buf("mq%d" % i)) for i in range(NB)])
            k_r = Ring([(T("mk%d" % i, [128, SEQ], BF16, st), Buf("mk%d" % i)) for i in range(NB)])
            v_r = Ring([(T("mv%d" % i, [128, NT, 66], BF16, st), Buf("mv%d" % i)) for i in range(NB)])
            E_r = Ring([(T("mE%d" % i, [128, 512], BF16, st), Buf("mE%d" % i)) for i in range(4)])
            op_r = Ring([(T("mop%d" % i, [128, NT, 128], F32, st), Buf("mop%d" % i)) for i in range(2)])
            rc_r = Ring([(T("mrc%d" % i, [128, 1], F32, st), Buf("mrc%d" % i)) for i in range(4)])
            pz_r = Ring([PI[0], PI[1], PI[6]]); pt_r = Ring(PI[7:8])
            heads = {}
            ops_ = {}

            def load_h(h):
                qT, qB = q_r.next(); kT, kB = k_r.next(); vh, vB = v_r.next()
                S.dma('sp', qT[0:96, :], mq_d[h], R=[mq_B], W=[qB])
                S.dma('sp', kT[0:96, :], mk_d[h], R=[mk_B], W=[kB])
                S.dma('sp', vh[:], mv_d[:, h * 66:(h + 1) * 66].rearrange("(t p) e -> p t e", p=128), R=[mv_B], W=[vB])
                heads[h] = (qT, qB, kT, kB, vh, vB)

            items = []
            for h in range(8):
                for c in range(NCH):
                    n_sb = (c + 1) * QPC
                    for sb in range(n_sb):
                        items.append(dict(h=h, c=c, sb=sb, last=(sb == n_sb - 1), idx=len(items)))
            first_idx = {}
            for it in items:
                first_idx.setdefault(it['h'], it['idx'])

            def s0(x):
                h, c, sb = x['h'], x['c'], x['sb']
                if x['idx'] == 0:
                    load_h(0)
                if x['idx'] == first_idx[h] + 2 and h + 1 < 8:
                    load_h(h + 1)
                qT, qB, kT, kB, vh, vB = heads[h]
                q0 = max(0, sb - c * QPC); f0 = q0 * 128
                x['pz'], x['pzB'] = pz_r.next()
                mm(x['pz'][:, f0:CH], kT[0:96, sb * 128:(sb + 1) * 128], qT[0:96, c * CH + f0:(c + 1) * CH], True, True, [kB, qB], [x['pzB']])

            def s1(x):
                c, sb = x['c'], x['sb']
                q0 = max(0, sb - c * QPC); f0 = q0 * 128
                x['E'], x['EB'] = E_r.next()
                E, EB = x['E'], x['EB']
                act(E[:, f0:CH], x['pz'][:, f0:CH], AF.Exp, [x['pzB']], [EB])
                if sb >= c * QPC:
                    tt('pool', E[:, f0:f0 + 128], E[:, f0:f0 + 128], tri_b[:], ALU.mult, [EB, triB], [EB])

            def s2(x):
                h, c, sb = x['h'], x['c'], x['sb']
                hp, z = h // 2, h % 2
                qT, qB, kT, kB, vh, vB = heads[h]
                q0 = max(0, sb - c * QPC)
                E, EB = x['E'], x['EB']
                for q in range(q0, QPC):
                    qb = c * QPC + q
                    mm(PS[2 + q][:, 0:66], E[:, q * 128:(q + 1) * 128], vh[:, sb, :], sb == 0, sb == qb, [EB, vB], [PB[2 + q]])
                if x['last']:
                    if z == 0 and c == 0:
                        ops_[hp] = op_r.next()
                    op, opB = ops_[hp]
                    for q in range(QPC):
                        qb = c * QPC + q
                        rc, rcB = rc_r.next()
                        S.add('dve', (lambda o_, i_: (lambda e: e.reciprocal(out=o_, in_=i_)))(rc[:], PS[2 + q][:, 64:65]), R=[PB[2 + q]], W=[rcB])
                        ts('dve', op[:, qb, z * 64:(z + 1) * 64], PS[2 + q][:, 0:64], rc[:, 0:1], ALU.mult, [PB[2 + q], rcB], [opB])
                    if z == 1 and c == NCH - 1:
                        for g4 in range(NT // 4):
                            pt, ptB = pt_r.next()
                            for i4 in range(4):
                                tr(pt[:, i4 * 128:(i4 + 1) * 128], op[:, g4 * 4 + i4, :], [opB], [ptB])
                            cp('act', oT[1][:, hp, g4 * 512:(g4 + 1) * 512], pt[:, 0:512], [ptB], [oTB[1][hp]])

            stages = [s0, s1, s2]
            for k in range(len(items) + len(stages) - 1):
                for j, stg_f in enumerate(stages):
                    ii = k - j
                    if 0 <= ii < len(items):
                        stg_f(items[ii])
            S.barrier()
        dump("o_mlaT%d" % l, oT[1][:].rearrange("p j t -> p (j t)"), [128, 4 * SEQ], BF16)
        if stop == 'BMLA':
            stB.close()
            return False

        with ExitStack() as st:
            posrow = T("posrow", [128, SEQ], F32, st); prwB = Buf("posrow")
            with ExitStack() as st2:
                pri = T("posrow_i", [128, SEQ], I32, st2); priB = Buf("posrow_i")
                S.dma('sp', pri[:], pos_d.partition_broadcast(128), W=[priB])
                cp('dve', posrow[:], pri[:], [priB], [prwB])
                S.barrier()
            negpos = T("negpos", [128, NT], F32, st)
            ts('dve', negpos[:], pos_tok[:], -1.0, ALU.mult, [posB], [prwB])
            lp = T("dlp", [128, 256], F32, st); lpB = Buf("dlp")
            lam = T("dlam", [128, 4], F32, st); lamB = Buf("dlam")
            prd = T("dprd", [128, 128], F32, st); prdB = Buf("dprd")
            S.dma('sp', lp[:], Wd['diff_lambda'][l].rearrange("a d -> (a d)").partition_broadcast(128), W=[lpB])
            tt('dve', prd[:, 0:64], lp[:, 0:64], lp[:, 64:128], ALU.mult, [lpB], [prdB])
            tt('dve', prd[:, 64:128], lp[:, 128:192], lp[:, 192:256], ALU.mult, [lpB], [prdB])
            red(lam[:, 0:2], prd[:].rearrange("p (a d) -> p a d", d=64), [prdB], [lamB])
            act(lam[:, 0:2], lam[:, 0:2], AF.Exp, [lamB], [lamB])
            tt('dve', lam[:, 2:3], lam[:, 1:2], lam[:, 0:1], ALU.subtract, [lamB], [lamB])
            ts('dve', lam[:, 3:4], lam[:, 2:3], -lam_init, ALU.add, [lamB], [lamB])
            sg = T("dsg", [128, 2], F32, st); sgB = Buf("dsg")
            S.dma('sp', sg[:, 0:1], Wd['diff_subln'][l].rearrange("(p o) -> p o", o=1), W=[sgB])
            ts('dve', sg[:, 1:2], sg[:, 0:1], 1.0 - lam_init, ALU.mult, [sgB], [sgB])
            NB = 2
            qz_r = Ring([([T("dq%d_%d" % (i, z), [128, SEQ], BF16, st) for z in range(2)], Buf("dq%d" % i)) for i in range(NB)])
            k_r = Ring([(T("dk%d" % i, [128, SEQ], BF16, st), Buf("dk%d" % i)) for i in range(NB)])
            v_r = Ring([(T("dv%d" % i, [128, NT, 130], BF16, st), Buf("dv%d" % i)) for i in range(NB)])
            E_r = Ring([(T("dE%d" % i, [128, 512], BF16, st), Buf("dE%d" % i)) for i in range(4)])
            dt_r = Ring([(T("ddt%d" % i, [128, 512], F32, st), Buf("ddt%d" % i)) for i in range(3)])
            zz_r = Ring([(T("dzz%d" % i, [128, 512], F32, st), Buf("dzz%d" % i)) for i in range(3)])
            o1_r = Ring([(T("do1%d" % i, [128, QPC, 128], F32, st), Buf("do1%d" % i)) for i in range(2)])
            od_r = Ring([(T("dod%d" % i, [128, 128], F32, st), Buf("dod%d" % i)) for i in range(6)])
            on_r = Ring([(T("don%d" % i, [128, 128], F32, st), Buf("don%d" % i)) for i in range(3)])
            jk_r = Ring([(T("djk%d" % i, [128, 128], BF16, st), Buf("djk%d" % i)) for i in range(2)])
            rc_r = Ring([(T("drc%d" % i, [128, 4], F32, st), Buf("drc%d" % i)) for i in range(8)])
            pz_r = Ring([PI[0], PI[1], PI[6]]); pt_r = Ring(PI[7:8])
            heads = {}

            def load_h(h):
                qz, qB = qz_r.next(); kp, kB = k_r.next(); vh, vB = v_r.next()
                memset('pool', qz[0][64:128, :], 0.0, [qB]); memset('pool', qz[1][0:64, :], 0.0, [qB])
                S.dma('sp', qz[0][0:64, :], dq_d[h][0:64, :], R=[dq_B], W=[qB])
                S.dma('sp', qz[1][64:128, :], dq_d[h][64:128, :], R=[dq_B], W=[qB])
                S.dma('sp', kp[:], dk_d[h], R=[dk_B], W=[kB])
                S.dma('sp', vh[:], dv_d[:, h * 130:(h + 1) * 130].rearrange("(t p) e -> p t e", p=128), R=[dv_B], W=[vB])
                heads[h] = (qz, qB, kp, kB, vh, vB)

            CD = 256; QD = 2; NCD = SEQ // CD
            items = []
            for h in range(4):
                for c in range(NCD):
                    n_sb = (c + 1) * QD
                    for sb in range(n_sb):
                        items.append(dict(h=h, c=c, sb=sb, last=(sb == n_sb - 1), idx=len(items)))
            first_idx = {}
            for it in items:
                first_idx.setdefault(it['h'], it['idx'])

            def s0(x):
                h, c, sb = x['h'], x['c'], x['sb']
                if x['idx'] == 0:
                    load_h(0)
                if x['idx'] == first_idx[h] + 2 and h + 1 < 4:
                    load_h(h + 1)
                qz, qB, kp, kB, vh, vB = heads[h]
                q0 = max(0, sb - c * QD); f0 = q0 * 128
                x['pz'], x['pzB'] = pz_r.next()
                x['dt'], x['dtB'] = dt_r.next()
                for m in range(2):
                    mm(x['pz'][:, m * CD + f0:(m + 1) * CD], kp[:, sb * 128:(sb + 1) * 128], qz[m][:, c * CD + f0:(c + 1) * CD], True, True, [kB, qB], [x['pzB']])
                act(x['dt'][:, f0:CD], posrow[:, c * CD + f0:(c + 1) * CD], AF.Abs, [prwB], [x['dtB']], bias=negpos[:, sb:sb + 1])

            def s1(x):
                h, c, sb = x['h'], x['c'], x['sb']
                slope = 2.0 ** (-8.0 * (h + 1) / 4.0)
                q0 = max(0, sb - c * QD); f0 = q0 * 128
                zz, zzB = zz_r.next()
                x['E'], x['EB'] = E_r.next()
                E, EB = x['E'], x['EB']
                for m in range(2):
                    stt(zz[:, m * CD + f0:(m + 1) * CD], x['dt'][:, f0:CD], -slope, x['pz'][:, m * CD + f0:(m + 1) * CD], ALU.mult, ALU.add, [x['dtB'], x['pzB']], [zzB])
                if f0 == 0:
                    act(E[:, 0:2 * CD], zz[:, 0:2 * CD], AF.Exp, [zzB], [EB])
                else:
                    for m in range(2):
                        act(E[:, m * CD + f0:(m + 1) * CD], zz[:, m * CD + f0:(m + 1) * CD], AF.Exp, [zzB], [EB])
                if sb >= c * QD:
                    for m in range(2):
                        tt('pool', E[:, m * CD + f0:m * CD + f0 + 128], E[:, m * CD + f0:m * CD + f0 + 128], tri_b[:], ALU.mult, [EB, triB], [EB])

            def s2(x):
                h, c, sb = x['h'], x['c'], x['sb']
                qz, qB, kp, kB, vh, vB = heads[h]
                q0 = max(0, sb - c * QD)
                E, EB = x['E'], x['EB']
                for q in range(q0, QD):
                    qb = c * QD + q
                    for m in range(2):
                        bi_ = 2 + q * 2 + m
                        mm(PS[bi_][:, 0:130], E[:, m * CD + q * 128:m * CD + (q + 1) * 128], vh[:, sb, :], sb == 0, sb == qb, [EB, vB], [PB[bi_]])
                if not x['last']:
                    return
                o1, o1B = o1_r.next()
                fin = []
                for q in range(QD):
                    qb = c * QD + q
                    b0 = 2 + q * 2; b1 = b0 + 1
                    rc, rcB = rc_r.next()
                    S.add('dve', (lambda o_, i_: (lambda e: e.reciprocal(out=o_, in_=i_)))(rc[:, 0:1], PS[b0][:, 128:129]), R=[PB[b0]], W=[rcB])
                    S.add('dve', (lambda o_, i_: (lambda e: e.reciprocal(out=o_, in_=i_)))(rc[:, 1:2], PS[b1][:, 128:129]), R=[PB[b1]], W=[rcB])
                    ts('dve', o1[:, q, :], PS[b0][:, 0:128], rc[:, 0:1], ALU.mult, [PB[b0], rcB], [o1B])
                    od, odB = od_r.next(); jk, jkB = jk_r.next()
                    tt('dve', rc[:, 1:2], rc[:, 1:2], lam[:, 3:4], ALU.mult, [rcB, lamB], [rcB])
                    stt(od[:], PS[b1][:, 0:128], rc[:, 1:2], o1[:, q, :], ALU.mult, ALU.add, [PB[b1], rcB, o1B], [odB])
                    act(jk[:], od[:], AF.Square, [odB], [jkB, rcB], accum_out=rc[:, 2:3])
                    fin.append((qb, rc, rcB, od, odB))

                def p1(fin=fin):
                    for (qb, rc, rcB, od, odB) in fin:
                        ts('dve', rc[:, 3:4], rc[:, 2:3], 1.0 / 128, ALU.mult, [rcB], [rcB], s2=EPS, op1=ALU.add)
                        act(rc[:, 3:4], rc[:, 3:4], AF.Ln, [rcB], [rcB])
                        act(rc[:, 3:4], rc[:, 3:4], AF.Exp, [rcB], [rcB], scale=-0.5)

                def p2(fin=fin, h=h, c=c):
                    pt, ptB = pt_r.next()
                    for i2, (qb, rc, rcB, od, odB) in enumerate(fin):
                        on, onB = on_r.next()
                        ts('dve', on[:], od[:], rc[:, 3:4], ALU.mult, [odB, rcB], [onB])
                        tr(pt[:, i2 * 128:(i2 + 1) * 128], on[:], [onB], [ptB])
                    act(oT[2][:, h, c * CD:(c + 1) * CD], pt[:, 0:CD], AF.Identity, [ptB, sgB], [oTB[2][h]], scale=sg[:, 1:2])
                deferred.append((cur_step[0] + 1, p1)); deferred.append((cur_step[0] + 2, p2))

            deferred = []
            cur_step = [0]
            stages = [s0, s1, s2]
            for k in range(len(items) + len(stages) - 1):
                cur_step[0] = k
                for j, stg_f in enumerate(stages):
                    ii = k - j
                    if 0 <= ii < len(items):
                        stg_f(items[ii])
                due = [d_ for d_ in deferred if d_[0] <= k]
                deferred[:] = [d_ for d_ in deferred if d_[0] > k]
                for _, fn_ in due:
                    fn_()
            for _, fn_ in deferred:
                fn_()
            S.barrier()
        dump("o_diffT%d" % l, oT[2][:].rearrange("p j t -> p (j t)"), [128, 4 * SEQ], BF16)
        if stop == 'BDIFF':
            stB.close()
            return False

        with ExitStack() as st:
            wbr = T("wbr", [128, 12, D], BF16, st); wbrB = Buf("wbr")
            wbsrc = Wd['w_branch'][l].rearrange("n k d -> (n k) d")
            for i in range(3):
                S.dma('pool', wbr[:, i * 4:(i + 1) * 4, :], wbsrc[i * 512:(i + 1) * 512, :].rearrange("(j p) n -> p j n", p=128), W=[wbrB])
            wo = T("wo", [128, 8, D], BF16, st); woB = Buf("wo")
            for i in range(2):
                S.dma('pool', wo[:, i * 4:(i + 1) * 4, :], Wd['w_out'][l][i * 512:(i + 1) * 512, :].rearrange("(j p) n -> p j n", p=128), W=[woB])
            for j in range(8):
                tt('pool' if j % 2 else 'dve', wo[:, j, :], wo[:, j, :], g1g2[:, 0, :], ALU.mult, [woB, g12B], [woB])
            gts_r = Ring([(T("gts%d" % i, [128, 3, CH], BF16, st), Buf("gts%d" % i)) for i in range(3)])
            M_r = Ring([(T("mM%d" % i, [128, 8, CH], BF16, st), Buf("mM%d" % i)) for i in range(2)])
            tn_r = Ring([(T("mtn%d" % i, [128, 512], F32, st), Buf("mtn%d" % i)) for i in range(6)])
            gt_v = gt_d.rearrange("(n j) p t -> j p n t", n=3)
            pr = Ring(PI)
            for c in range(NCH):
                M, MB = M_r.next()
                for j in range(8):
                    gts, gtsB = gts_r.next()
                    S.dma('sp', gts[:], gt_v[j][:, :, c * CH:(c + 1) * CH], R=[gt_B], W=[gtsB])
                    tn = []
                    for n in range(3):
                        py, pyB = pr.next()
                        for kc in range(4):
                            mm(py[:, 0:CH], wbr[:, n * 4 + kc, j * 128:(j + 1) * 128], oT[n][:, kc, c * CH:(c + 1) * CH], kc == 0, kc == 3, [wbrB, oTB[n][kc]], [pyB])
                        t_, tB_ = tn_r.next()
                        tt('dve', t_[:, 0:CH], py[:, 0:CH], gts[:, n, :], ALU.mult, [pyB, gtsB], [tB_])
                        tn.append((t_, tB_))
                    tt('dve', tn[0][0][:, 0:CH], tn[0][0][:, 0:CH], tn[1][0][:, 0:CH], ALU.add, [tn[0][1], tn[1][1]], [tn[0][1]])
                    tt('pool', M[:, j, :], tn[0][0][:, 0:CH], tn[2][0][:, 0:CH], ALU.add, [tn[0][1], tn[2][1]], [MB])
                for q in range(QPC):
                    t = c * QPC + q
                    for half in range(2):
                        px, pxB = pr.next()
                        for j in range(8):
                            mm(px[:, :], M[:, j, q * 128:(q + 1) * 128], wo[:, j, half * 512:(half + 1) * 512], j == 0, j == 7, [MB, woB], [pxB])
                        tt('dve', xs[:, t, half * 512:(half + 1) * 512], px[:, :], xs[:, t, half * 512:(half + 1) * 512], ALU.add, [pxB, xB[t]], [xB[t]])
            S.barrier()
        stB.close()
        dump("xmid%d" % l, xs[:].rearrange("p t d -> p (t d)"), [128, NT * D])
        if stop == 'C':
            return False
        stA = ExitStack()
        hT = T("hT2", [128, 8, SEQ], BF16, stA)

        moe = (l % 2 == 1)
        jl = l // 2
        if moe:
            units = [(Wd['w_exp_gate'][jl][e], Wd['w_exp_up'][jl][e], Wd['w_exp_down'][jl][e], e) for e in range(8)]
        else:
            units = [(Wd['w_ffn_gate'][jl], Wd['w_ffn_up'][jl], Wd['w_ffn_down'][jl], None)]
        groups = [(0, 4), (4, 4), (8, 4), (12, 4), (16, 4), (20, 2)]
        wg_r = Ring([(T("fwg%d" % i, [128, 8, 512], BF16, stA), Buf("fwg%d" % i)) for i in range(2)])
        wu_r = Ring([(T("fwu%d" % i, [128, 8, 512], BF16, stA), Buf("fwu%d" % i)) for i in range(2)])
        wd_r = Ring([(T("fwd%d" % i, [128, 4, D], BF16, stA), Buf("fwd%d" % i)) for i in range(2)])

        def ffn_load(wg2, wu2, wd2, f0, nf):
            wg, wgB = wg_r.next(); wu, wuB = wu_r.next(); wd_, wdB = wd_r.next()
            load_w(wg, wgB, wg2, 0, 8, f0 * 128, nf * 128)
            load_w(wu, wuB, wu2, 0, 8, f0 * 128, nf * 128)
            S.dma('pool', wd_[:, 0:nf, :], wd2[f0 * 128:(f0 + nf) * 128, :].rearrange("(j p) n -> p j n", p=128), W=[wdB])
            for fc in range(nf):
                tt('pool', wd_[:, fc, :], wd_[:, fc, :], g1g2[:, 1, :], ALU.mult, [wdB, g12B], [wdB])
            return wg, wgB, wu, wuB, wd_, wdB
        ffn_pre = [ffn_load(units[0][0], units[0][1], units[0][2], groups[0][0], groups[0][1])]
        with ExitStack() as st:
            router = None
            if moe:
                wr = T("wr", [128, 8, 8], F32, st); wrB = Buf("wr")
                wr2 = T("wr2", [128, 8, 8], F32, st); wr2B = Buf("wr2")
                bbc = T("bbc", [128, 8, 128], F32, st); bbcB = Buf("bbc")
                brB_t = T("brB", [128, 8], F32, st); brBB = Buf("brB")
                S.dma('sp', wr[:], Wd['w_router'][jl].rearrange("(j p) e -> p j e", p=128), W=[wrB])
                tt('dve', wr2[:], wr[:], ABT[:, 8:16].unsqueeze(2).broadcast_to([128, 8, 8]), ALU.mult, [wrB, ABTB], [wr2B])
                cp('dve', bbc[:], modT[:, 16:24].unsqueeze(2).broadcast_to([128, 8, 128]), [modTB], [bbcB])
                pl, plB = PI[4]
                for j in range(8):
                    mm(pl[:, 0:8], bbc[:, j, :], wr[:, j, :], j == 0, j == 7, [bbcB, wrB], [plB])
                cp('dve', brB_t[:], pl[:, 0:8], [plB], [brBB])
                xt_r = Ring([(T("rxt%d" % i, [128, 8, 128], F32, st), Buf("rxt%d" % i)) for i in range(2)])
                rt_r = Ring([(T("rrt%d" % i, [128, 48], F32, st), Buf("rrt%d" % i)) for i in range(2)])
                prr = Ring(PI[4:6])

                def router(t, pa, paB, pb, pbB, aoff, boff):
                    xt, xtB = xt_r.next(); rt, rtB = rt_r.next()
                    cp('dve', xt[:, 0:4, :].rearrange("p j t -> p (j t)"), pa[:, :], [paB], [xtB])
                    cp('act', xt[:, 4:8, :].rearrange("p j t -> p (j t)"), pb[:, :], [pbB], [xtB])
                    pl_, plB_ = prr.next()
                    for j in range(8):
                        mm(pl_[:, 0:8], xt[:, j, :], wr2[:, j, :], j == 0, j == 7, [xtB, wr2B], [plB_])
                    lg = rt[:, 0:8]; mx = rt[:, 8:16]; mk = rt[:, 16:24]; ex = rt[:, 24:32]
                    tt('dve', lg, pl_[:, 0:8], brB_t[:], ALU.add, [plB_, brBB], [rtB])
                    S.add('dve', (lambda o_, i_: (lambda e: e.max(out=o_, in_=i_)))(mx, lg), R=[rtB], W=[rtB])
                    ts('dve', mk, lg, rt[:, 9:10], ALU.is_ge, [rtB], [rtB])
                    ts('dve', rt[:, 32:33], rt[:, 8:9], -1.0, ALU.mult, [rtB], [rtB])
                    act(ex, lg, AF.Exp, [rtB], [rtB], bias=rt[:, 32:33])
                    tt('dve', ex, ex, mk, ALU.mult, [rtB], [rtB])
                    red(rt[:, 33:34], ex, [rtB], [rtB])
                    S.add('dve', (lambda o_, i_: (lambda e: e.reciprocal(out=o_, in_=i_)))(rt[:, 34:35], rt[:, 33:34]), R=[rtB], W=[rtB])
                    ts('dve', comb[:, t, :], ex, rt[:, 34:35], ALU.mult, [rtB], [combB])
            norm_phase(2, hT, hB, st, router)
            S.barrier()
        dump("h2T%d" % l, hT[:].rearrange("p j t -> p (j t)"), [128, 8 * SEQ], BF16)
        dump("comb%d" % l, comb[:].rearrange("p t e -> p (t e)"), [128, NT * 8])

        with ExitStack() as st:
            aT_r = Ring([(T("faT%d" % i, [128, 4, SEQ], BF16, st), Buf("faT%d" % i)) for i in range(2)])
            sg_r = Ring([(T("fsg%d" % i, [128, 512], F32, st), Buf("fsg%d" % i)) for i in range(3)])
            pr = Ring(PI)
            for (wg2, wu2, wd2, e) in units:
                for (f0, nf) in groups:
                    aT, aTB = aT_r.next()
                    if ffn_pre:
                        wg, wgB, wu, wuB, wd_, wdB = ffn_pre.pop()
                    else:
                        wg, wgB, wu, wuB, wd_, wdB = ffn_load(wg2, wu2, wd2, f0, nf)
                    for fc in range(nf):
                        for c in range(NCH):
                            pg, pgB = pr.next(); pu, puB = pr.next()
                            proj_T(pg, pgB, hT, hB, wg, wgB, fc * 128, 128, c)
                            proj_T(pu, puB, hT, hB, wu, wuB, fc * 128, 128, c)
                            sg_, sgB_ = sg_r.next()
                            act(sg_[:, 0:CH], pg[:, 0:CH], AF.Silu, [pgB], [sgB_])
                            tt('dve', aT[:, fc, c * CH:(c + 1) * CH], sg_[:, 0:CH], pu[:, 0:CH], ALU.mult, [sgB_, puB], [aTB])
                    for t in range(NT):
                        for half in range(2):
                            px, pxB = pr.next()
                            for fc in range(nf):
                                mm(px[:, :], aT[:, fc, t * 128:(t + 1) * 128], wd_[:, fc, half * 512:(half + 1) * 512], fc == 0, fc == nf - 1, [aTB, wdB], [pxB])
                            xsl = xs[:, t, half * 512:(half + 1) * 512]
                            if e is None:
                                tt('dve', xsl, px[:, :], xsl, ALU.add, [pxB, xB[t]], [xB[t]])
                            else:
                                stt(xsl, px[:, :], comb[:, t, e:e + 1], xsl, ALU.mult, ALU.add, [pxB, combB, xB[t]], [xB[t]])
            S.barrier()
        stA.close()
        dump("xout%d" % l, xs[:].rearrange("p t d -> p (t d)"), [128, NT * D])

        return True

    for l in layers:
        if not layer_body(l):
            break

    g_out = S.grp("out_sp")
    S.barrier()
    for t in range(NT):
        S.dma('sp', y_d[t * 128:(t + 1) * 128, :], xs[:, t, :], R=[xB[t]], grp=g_out)
    S.barrier(['sp'])
    S.emit(nc, es)
    es.close()
    global LAST_S
    LAST_S = S
    return nc


LAST_S = None


_NC_CACHE = {}


def kernel(**inputs):
    B = 8
    SEQ = 2048
    if 'nc' not in _NC_CACHE:
        _NC_CACHE['nc'] = build(SEQ=SEQ, layers=(0, 1))
    nc = _NC_CACHE['nc']
    x = np.ascontiguousarray(inputs['x'], dtype=np.float32)
    c = np.ascontiguousarray(inputs['c'], dtype=np.float32)
    pos = np.ascontiguousarray(inputs['positions'], dtype=np.int32)
    wts = {n: np.ascontiguousarray(inputs[n], dtype=np.float32) for n in WNAMES}
    in_maps = []
    for b in range(B):
        m = {'x': x[b], 'c': c[b], 'positions': pos[b]}
        m.update(wts)
        in_maps.append(m)
    res = run_bass_kernel_spmd(nc, in_maps, core_ids=list(range(B)))
    return np.stack([np.asarray(r['y'], dtype=np.float32) for r in res.results], axis=0)
```
